# Optimizing a Trainium2 kernel written in Bass

```python
import math
import jax
import jax.numpy as jnp
from jax import lax
import numpy as np

D_MODEL = 1024
BATCH = 8
SEQ = 4096
DEPTH = 2

GRID_W = 64
CTX_LEN = 256
DA_HEADS = 4
DA_HEAD_DIM = 64
DA_WIDTH = DA_HEADS * 2 * DA_HEAD_DIM
HY_WIDTH = D_MODEL - DA_WIDTH
HY_ORDER = 2
HY_SHORT = 3
HY_EMB = 33
HY_BANDS = (HY_EMB - 1) // 2
HY_FFN = 64
HY_FILTERS = HY_ORDER * 2 * HY_WIDTH
HY_TARGET = 1e-2
HY_MIN_DECAY = math.log(HY_TARGET) / 0.3
HY_MAX_DECAY = math.log(HY_TARGET) / 1.5
IN0_WIDTH = 3 * DA_WIDTH + 3 * HY_WIDTH
CV_WIDTH = D_MODEL
CV_KERNEL = 31
N_GROUPS = 4
EXPERTS_PER_GROUP = 8
N_EXPERTS = N_GROUPS * EXPERTS_PER_GROUP
TOP_K = 2
D_EXPERT = 512
ROPE_THETA = 10000.0
EPS = 1e-6
Q_BLOCK = 128
N_EVEN = (DEPTH + 1) // 2
N_ODD = DEPTH // 2

kernel_name = 'hybrid_diffattn_hyena_conformer_hmoe'

F32 = jnp.float32


def rmsnorm(x, g):
    x32 = x.astype(F32)
    y = x32 * lax.rsqrt(jnp.mean(x32 * x32, axis=-1, keepdims=True) + EPS)
    return (y * g).astype(x.dtype)


def layernorm(x, g, b):
    x32 = x.astype(F32)
    mu = jnp.mean(x32, axis=-1, keepdims=True)
    var = jnp.mean(jnp.square(x32 - mu), axis=-1, keepdims=True)
    return ((x32 - mu) * lax.rsqrt(var + EPS) * g + b).astype(x.dtype)


def adaln(cvec, w, b):
    m = (jax.nn.silu(cvec) @ w + b).reshape((-1, 1, 6 * D_MODEL))
    return jnp.split(m, 6, axis=-1)


def modulate(h, shift, scale):
    return h * (1.0 + scale) + shift


def _heads(t, n_heads):
    bsz, n, _ = t.shape
    return t.reshape(bsz, n, n_heads, -1).transpose(0, 2, 1, 3)


def rope_2d(x):
    n, hd = x.shape[2], x.shape[3]
    rows = n // GRID_W
    row = jnp.repeat(jnp.arange(rows, dtype=F32), GRID_W)
    col = jnp.tile(jnp.arange(GRID_W, dtype=F32), rows)
    half = hd // 2
    inv = ROPE_THETA ** (-jnp.arange(0, half, 2, dtype=F32) / half)
    x32 = x.astype(F32)

    def rot(xp, pos):
        ang = pos[:, None] * inv
        cos, sin = jnp.cos(ang), jnp.sin(ang)
        a, b = jnp.split(xp, 2, axis=-1)
        return jnp.concatenate([a * cos - b * sin, a * sin + b * cos], axis=-1)

    out = jnp.concatenate([rot(x32[..., :half], row), rot(x32[..., half:], col)], axis=-1)
    return out.astype(x.dtype)


def diff_attention(q, k, v, lam, subln_g, lam_init):
    bsz, h2, lq, d = q.shape
    nh = h2 // 2
    nb = lq // Q_BLOCK
    qb = q.reshape(bsz, h2, nb, Q_BLOCK, d).transpose(2, 0, 1, 3, 4)
    scale = d ** -0.5

    def block(qi):
        s = jnp.einsum('bhqd,bhkd->bhqk', qi, k).astype(F32) * scale
        p = jax.nn.softmax(s, axis=-1).reshape(bsz, nh, 2, Q_BLOCK, -1)
        a = p[:, :, 0] - lam * p[:, :, 1]
        o = jnp.einsum('bhqk,bhkv->bhqv', a.astype(v.dtype), v)
        return rmsnorm(o, subln_g) * (1.0 - lam_init)

    o = lax.map(block, qb)
    return o.transpose(1, 0, 3, 2, 4).reshape(bsz, lq, nh * 2 * d)


def depthwise_conv(x, w, b):
    kw, ch = w.shape
    y = lax.conv_general_dilated(x, w[:, None, :].astype(x.dtype), window_strides=(1,),
                                 padding=[((kw - 1) // 2, kw // 2)],
                                 dimension_numbers=('NWC', 'WIO', 'NWC'),
                                 feature_group_count=ch)
    return y + b


def hyena_kernels(n, w1, b1, fr1, w2, b2, fr2, w3, b3):
    t = jnp.linspace(0.0, 1.0, n, dtype=F32)[:, None]
    ang = (2.0 * math.pi / n) * jnp.arange(n, dtype=F32)[:, None] * jnp.linspace(1e-4, HY_BANDS - 1, HY_BANDS, dtype=F32)[None, :]
    z = jnp.concatenate([t, jnp.cos(ang), -jnp.sin(ang)], axis=-1).astype(w1.dtype)
    h = jnp.sin(fr1 * (z @ w1 + b1))
    h = jnp.sin(fr2 * (h @ w2 + b2))
    h = (h @ w3 + b3).astype(F32).reshape(n, HY_ORDER, 2, HY_WIDTH)
    deltas = jnp.abs(jnp.linspace(HY_MIN_DECAY, HY_MAX_DECAY, HY_WIDTH, dtype=F32))
    h = h * jnp.exp(-t[:, :, None, None] * deltas)
    fwd, bwd = h[:, :, 0], h[:, :, 1]
    kc = jnp.concatenate([fwd, jnp.zeros_like(fwd[:1]), bwd[:0:-1]], axis=0)
    return kc / jnp.sum(jnp.abs(kc), axis=0, keepdims=True)


def fft_long_conv(z, kf, bias):
    n = z.shape[1]
    z32 = z.astype(F32)
    y = jnp.fft.irfft(jnp.fft.rfft(z32, n=2 * n, axis=1) * kf, n=2 * n, axis=1)[:, :n]
    return (y + z32 * bias.astype(F32)).astype(z.dtype)


def hyena_mixer(u, short_w, short_b, w1, b1, fr1, w2, b2, fr2, w3, b3, bias):
    n = u.shape[1]
    u = depthwise_conv(u, short_w, short_b)
    v, x1, x2 = jnp.split(u, 3, axis=-1)
    kf = jnp.fft.rfft(hyena_kernels(n, w1, b1, fr1, w2, b2, fr2, w3, b3), axis=0)
    z = x1 * fft_long_conv(v, kf[:, 0], bias[0])
    return x2 * fft_long_conv(z, kf[:, 1], bias[1])


def conformer_conv(h, w1, b1, dw_w, dw_b, ln_g, ln_b, w2, b2):
    a = h @ w1 + b1
    a, g = jnp.split(a, 2, axis=-1)
    a = a * jax.nn.sigmoid(g)
    a = depthwise_conv(a, dw_w, dw_b)
    a = jax.nn.silu(layernorm(a, ln_g, ln_b))
    return a @ w2 + b2


def hier_moe(h, wg, bg, we, be, w_gate, w_up, w_down):
    bsz, n, d = h.shape
    t = h.reshape(-1, d)
    ntok = t.shape[0]
    g_prob = jax.nn.softmax((t @ wg + bg).astype(F32), axis=-1)
    g_w, g_idx = lax.top_k(g_prob, 1)
    e_logits = (t @ we + be).astype(F32).reshape(ntok, N_GROUPS, EXPERTS_PER_GROUP)
    e_logits = e_logits[jnp.arange(ntok), g_idx[:, 0]]
    e_w, e_idx = lax.top_k(jax.nn.softmax(e_logits, axis=-1), TOP_K)
    e_w = e_w / jnp.sum(e_w, axis=-1, keepdims=True)
    wts = g_w * e_w
    gid = g_idx * EXPERTS_PER_GROUP + e_idx
    comb = jnp.sum(jax.nn.one_hot(gid, N_EXPERTS, dtype=F32) * wts[..., None], axis=1)
    y = jnp.zeros((ntok, d), F32)
    for e in range(N_EXPERTS):
        he = jax.nn.silu(t @ w_gate[e]) * (t @ w_up[e])
        y = y + comb[:, e:e + 1] * (he @ w_down[e]).astype(F32)
    return y.astype(h.dtype).reshape(bsz, n, d)


def setup_inputs(seed: int = 0) -> dict:
    key = jax.random.key(seed)
    ks = iter(jax.random.split(key, 48))

    def nrm(shape, scale):
        return jax.random.normal(next(ks), shape, jnp.float32) * scale

    def gain(shape, s=0.02):
        return 1.0 + nrm(shape, s)

    D = D_MODEL
    C3 = 3 * HY_WIDTH
    DO = DA_WIDTH + HY_WIDTH
    return dict(
        x=nrm((BATCH, SEQ, D), 1.0),
        c=nrm((BATCH, D), 1.0),
        ctx=nrm((BATCH, CTX_LEN, D), 1.0),
        c_ctx=nrm((D,), 1.0),
        ada_w=nrm((DEPTH, D, 6 * D), 0.5 * D ** -0.5),
        ada_b=nrm((DEPTH, 6 * D), 0.01),
        norm1_g=gain((DEPTH, D)),
        norm2_g=gain((DEPTH, D)),
        final_g=gain((D,)),
        w_in0=nrm((N_EVEN, D, IN0_WIDTH), D ** -0.5),
        w_out0=nrm((N_EVEN, DO, D), DO ** -0.5),
        lam_q1=nrm((N_EVEN, DA_HEAD_DIM), 0.1),
        lam_k1=nrm((N_EVEN, DA_HEAD_DIM), 0.1),
        lam_q2=nrm((N_EVEN, DA_HEAD_DIM), 0.1),
        lam_k2=nrm((N_EVEN, DA_HEAD_DIM), 0.1),
        subln_g=gain((N_EVEN, 2 * DA_HEAD_DIM)),
        hy_short_w=nrm((N_EVEN, HY_SHORT, C3), HY_SHORT ** -0.5),
        hy_short_b=nrm((N_EVEN, C3), 0.01),
        hy_w1=nrm((N_EVEN, HY_EMB, HY_FFN), HY_EMB ** -0.5),
        hy_b1=nrm((N_EVEN, HY_FFN), 0.1),
        hy_fr1=gain((N_EVEN, HY_FFN), 0.1),
        hy_w2=nrm((N_EVEN, HY_FFN, HY_FFN), HY_FFN ** -0.5),
        hy_b2=nrm((N_EVEN, HY_FFN), 0.1),
        hy_fr2=gain((N_EVEN, HY_FFN), 0.1),
        hy_w3=nrm((N_EVEN, HY_FFN, HY_FILTERS), HY_FFN ** -0.5),
        hy_b3=nrm((N_EVEN, HY_FILTERS), 0.01),
        hy_bias=nrm((N_EVEN, HY_ORDER, HY_WIDTH), 0.5),
        cv_w1=nrm((N_ODD, D, 2 * CV_WIDTH), D ** -0.5),
        cv_b1=nrm((N_ODD, 2 * CV_WIDTH), 0.01),
        cv_dw_w=nrm((N_ODD, CV_KERNEL, CV_WIDTH), CV_KERNEL ** -0.5),
        cv_dw_b=nrm((N_ODD, CV_WIDTH), 0.01),
        cv_ln_g=gain((N_ODD, CV_WIDTH)),
        cv_ln_b=nrm((N_ODD, CV_WIDTH), 0.01),
        cv_w2=nrm((N_ODD, CV_WIDTH, D), CV_WIDTH ** -0.5),
        cv_b2=nrm((N_ODD, D), 0.01),
        moe_wg=nrm((DEPTH, D, N_GROUPS), D ** -0.5),
        moe_bg=nrm((DEPTH, N_GROUPS), 0.01),
        moe_we=nrm((DEPTH, D, N_EXPERTS), D ** -0.5),
        moe_be=nrm((DEPTH, N_EXPERTS), 0.01),
        moe_w_gate=nrm((DEPTH, N_EXPERTS, D, D_EXPERT), D ** -0.5),
        moe_w_up=nrm((DEPTH, N_EXPERTS, D, D_EXPERT), D ** -0.5),
        moe_w_down=nrm((DEPTH, N_EXPERTS, D_EXPERT, D), D_EXPERT ** -0.5),
    )


def reference(x, c, ctx, c_ctx, ada_w, ada_b, norm1_g, norm2_g, final_g,
              w_in0, w_out0, lam_q1, lam_k1, lam_q2, lam_k2, subln_g,
              hy_short_w, hy_short_b, hy_w1, hy_b1, hy_fr1, hy_w2, hy_b2, hy_fr2, hy_w3, hy_b3, hy_bias,
              cv_w1, cv_b1, cv_dw_w, cv_dw_b, cv_ln_g, cv_ln_b, cv_w2, cv_b2,
              moe_wg, moe_bg, moe_we, moe_be, moe_w_gate, moe_w_up, moe_w_down):
    ctx_s = ctx
    H2 = 2 * DA_HEADS
    for i in range(DEPTH):
        j = i // 2
        ctx_live = any(m % 2 == 0 for m in range(i + 1, DEPTH))
        sh1, sc1, g1, sh2, sc2, g2 = adaln(c, ada_w[i], ada_b[i])
        if i % 2 == 0 or ctx_live:
            csh1, csc1, cg1, csh2, csc2, cg2 = adaln(c_ctx, ada_w[i], ada_b[i])
        hx = modulate(rmsnorm(x, norm1_g[i]), sh1, sc1)
        if i % 2 == 0:
            w_in = w_in0[j]
            hy = (hy_short_w[j], hy_short_b[j], hy_w1[j], hy_b1[j], hy_fr1[j], hy_w2[j], hy_b2[j],
                  hy_fr2[j], hy_w3[j], hy_b3[j], hy_bias[j])
            lam_init = 0.8 - 0.6 * math.exp(-0.3 * i)
            lam = (jnp.exp(jnp.sum(lam_q1[j] * lam_k1[j]).astype(F32))
                   - jnp.exp(jnp.sum(lam_q2[j] * lam_k2[j]).astype(F32)) + lam_init)
            hc = modulate(rmsnorm(ctx_s, norm1_g[i]), csh1, csc1)
            if ctx_live:
                pc = hc @ w_in
                pc_kv = pc[..., DA_WIDTH:3 * DA_WIDTH]
            else:
                pc_kv = hc @ w_in[:, DA_WIDTH:3 * DA_WIDTH]
            kc = _heads(pc_kv[..., :DA_WIDTH], H2)
            vc = _heads(pc_kv[..., DA_WIDTH:], DA_HEADS)
            px = hx @ w_in
            q = rope_2d(_heads(px[..., :DA_WIDTH], H2))
            k = rope_2d(_heads(px[..., DA_WIDTH:2 * DA_WIDTH], H2))
            v = _heads(px[..., 2 * DA_WIDTH:3 * DA_WIDTH], DA_HEADS)
            o_a = diff_attention(q, jnp.concatenate([kc, k], axis=2), jnp.concatenate([vc, v], axis=2),
                                 lam, subln_g[j], lam_init)
            o_b = hyena_mixer(px[..., 3 * DA_WIDTH:], *hy)
            x_new = x + g1 * (jnp.concatenate([o_a, o_b], axis=-1) @ w_out0[j])
            if ctx_live:
                qc = _heads(pc[..., :DA_WIDTH], H2)
                oc_a = diff_attention(qc, kc, vc, lam, subln_g[j], lam_init)
                oc_b = hyena_mixer(pc[..., 3 * DA_WIDTH:], *hy)
                ctx_s = ctx_s + cg1 * (jnp.concatenate([oc_a, oc_b], axis=-1) @ w_out0[j])
            x = x_new
        else:
            cv = (cv_w1[j], cv_b1[j], cv_dw_w[j], cv_dw_b[j], cv_ln_g[j], cv_ln_b[j], cv_w2[j], cv_b2[j])
            x = x + g1 * conformer_conv(hx, *cv)
            if ctx_live:
                hc = modulate(rmsnorm(ctx_s, norm1_g[i]), csh1, csc1)
                ctx_s = ctx_s + cg1 * conformer_conv(hc, *cv)
        moe_p = (moe_wg[i], moe_bg[i], moe_we[i], moe_be[i], moe_w_gate[i], moe_w_up[i], moe_w_down[i])
        x = x + g2 * hier_moe(modulate(rmsnorm(x, norm2_g[i]), sh2, sc2), *moe_p)
        if ctx_live:
            ctx_s = ctx_s + cg2 * hier_moe(modulate(rmsnorm(ctx_s, norm2_g[i]), csh2, csc2), *moe_p)
    return rmsnorm(x, final_g)
```

```python
import math
from contextlib import ExitStack

import numpy as np
import ml_dtypes

import concourse.bass as bass
import concourse.mybir as mybir
from concourse.bass_utils import run_bass_kernel_spmd

F32 = mybir.dt.float32
BF16 = mybir.dt.bfloat16
I32 = mybir.dt.int32
AF = mybir.ActivationFunctionType
ALU = mybir.AluOpType
AX = mybir.AxisListType

D = 1024
S = 4096
CTX = 256
LK = S + CTX
NCH = D // 128
EPS = 1e-6
N_EXP = 32
D_EXP = 512
N2 = 2 * S


class Tok:
    __slots__ = ("sem", "val", "eng", "dsem")

    def __init__(self, sem, val, eng, dsem=None):
        self.sem = sem
        self.val = val
        self.eng = eng
        self.dsem = dsem


class Buf:
    __slots__ = ("ap", "w", "r", "name")

    def __init__(self, ap, name=""):
        self.ap = ap
        self.w = None
        self.r = {}
        self.name = name

    def __getitem__(self, key):
        return self.ap[key]


class DSem:
    def __init__(self, sem):
        self.sem = sem
        self.count = 0


class K:
    def __init__(self, nc):
        self.nc = nc
        self.E = {"pe": nc.tensor, "act": nc.scalar, "dve": nc.vector, "pool": nc.gpsimd, "sp": nc.sync}
        self.sem = {}
        self.cnt = {}
        for e in ("pe", "act", "dve", "pool"):
            self.sem[e] = nc.alloc_semaphore("s_" + e)
            self.cnt[e] = 0
        self.seen = {}
        self.dsems = {}
        self.pe_pending = None
        self.n_instr = 0
        self.old_last = {}

    def dsem(self, name):
        if name not in self.dsems:
            self.dsems[name] = DSem(self.nc.alloc_semaphore("d_" + name))
        return self.dsems[name]

    def _wait(self, eng, tok):
        if tok is None:
            return
        if tok.eng == "pe" and eng == "pe":
            return
        if tok.val is None:
            raise RuntimeError("waiting on an un-signalled PE group")
        val = tok.val
        if tok.dsem is not None:
            val = tok.dsem.count
        key = (eng, tok.sem.num)
        if self.seen.get(key, 0) >= val:
            return
        self.E[eng].wait_ge(tok.sem, val)
        self.seen[key] = val

    def _hazards(self, eng, reads, writes):
        for b in reads:
            self._wait(eng, b.w)
        for b in writes:
            self._wait(eng, b.w)
            for t in b.r.values():
                self._wait(eng, t)

    def _update(self, tok, reads, writes):
        key = tok.eng if tok.dsem is None else ("d", tok.sem.num)
        for b in reads:
            b.r[key] = tok
        for b in writes:
            b.w = tok
            b.r = {}

    def op(self, eng, fn, reads=(), writes=(), signal=True):
        self._hazards(eng, reads, writes)
        ins = fn(self.E[eng])
        self.n_instr += 1
        if eng == "pe" and not signal:
            if self.pe_pending is None:
                self.pe_pending = Tok(self.sem["pe"], None, "pe")
            tok = self.pe_pending
        else:
            if self.cnt[eng] >= 15000:
                self.old_last[eng] = Tok(self.sem[eng], self.cnt[eng], eng)
                self.sem[eng] = self.nc.alloc_semaphore("s_%s_%d" % (eng, self.n_instr))
                self.cnt[eng] = 0
            self.cnt[eng] += 1
            ins.then_inc(self.sem[eng], 1)
            if eng == "pe" and self.pe_pending is not None:
                self.pe_pending.val = self.cnt[eng]
                tok = self.pe_pending
                self.pe_pending = None
            else:
                tok = Tok(self.sem[eng], self.cnt[eng], eng)
        self._update(tok, reads, writes)
        return tok

    def dma(self, out, in_, reads=(), writes=(), sem="ld", q="sp", wfree=()):
        ds = self.dsem(sem) if isinstance(sem, str) else sem
        self._hazards(q, reads, writes)
        for b in wfree:
            for t in b.r.values():
                self._wait(q, t)
        writes = list(writes) + list(wfree)
        ins = self.E[q].dma_start(out=out, in_=in_)
        ins.then_inc(ds.sem, 16)
        ds.count += 16
        self.n_instr += 1
        tok = Tok(ds.sem, ds.count, "dma", ds)
        self._update(tok, reads, writes)
        return tok

    def dma_raw(self, q, fn, reads=(), writes=(), wfree=(), sem="ld"):
        ds = self.dsem(sem) if isinstance(sem, str) else sem
        self._hazards(q, reads, writes)
        for b in wfree:
            for t in b.r.values():
                self._wait(q, t)
        writes = list(writes) + list(wfree)
        ins = fn(self.E[q])
        ins.then_inc(ds.sem, 16)
        ds.count += 16
        self.n_instr += 1
        tok = Tok(ds.sem, ds.count, "dma", ds)
        self._update(tok, reads, writes)
        return tok

    def barrier(self):
        toks = [Tok(self.sem[e], self.cnt[e], e) if self.cnt[e] > 0 else self.old_last[e]
                for e in ("pe", "act", "dve", "pool") if self.cnt[e] > 0 or e in self.old_last]
        dtoks = [Tok(d.sem, d.count, "dma", d) for d in self.dsems.values() if d.count > 0]
        if self.pe_pending is not None:
            raise RuntimeError("barrier with un-signalled PE group")
        for e in ("pe", "act", "dve", "pool", "sp"):
            for t in toks:
                if t.eng != e:
                    self._wait(e, t)
            for t in dtoks:
                self._wait(e, t)


class Prog:
    def __init__(self, dbg=(), feed=()):
        self.nc = bass.Bass("TRN2", target_bir_lowering=False)
        self.k = K(self.nc)
        self.dbg = set(dbg)
        self.feed = set(feed)
        self.inputs = {}
        self.outputs = []
        self.es = ExitStack()
        nc = self.nc
        self.psum = [Buf(self.es.enter_context(nc.psum_tensor("ps%d" % i, [128, 512], F32)), "ps%d" % i)
                     for i in range(8)]

    def inp(self, name, shape, dtype=F32):
        t = self.nc.dram_tensor(name, list(shape), dtype, kind="ExternalInput").ap()
        self.inputs[name] = (tuple(shape), dtype)
        return t

    def out(self, name, shape, dtype=F32):
        t = self.nc.dram_tensor(name, list(shape), dtype, kind="ExternalOutput").ap()
        self.outputs.append(name)
        return t

    def scratch(self, name, shape, dtype=F32):
        if name in self.feed:
            return Buf(self.inp(name, shape, dtype), name)
        if name in self.dbg:
            return Buf(self.out(name, shape, dtype), name)
        return Buf(self.nc.dram_tensor(name, list(shape), dtype).ap(), name)

    def sb(self, es, name, shape, dtype=F32):
        self._uid = getattr(self, "_uid", 0) + 1
        return Buf(es.enter_context(self.nc.sbuf_tensor("%s_%d" % (name, self._uid), list(shape), dtype)), name)


def _mm(k, ps, out_ap, lhsT, rhs, start, stop, reads, signal):
    return k.op("pe", lambda e: e.matmul(out_ap, lhsT, rhs, start=start, stop=stop),
                reads=reads, writes=[ps], signal=signal)


def _act(k, out_ap, in_ap, func, reads, writes, bias=None, scale=None):
    kw = {}
    if bias is not None:
        kw["bias"] = bias
    if scale is not None:
        kw["scale"] = scale
    return k.op("act", lambda e: e.activation(out=out_ap, in_=in_ap, func=func, **kw), reads=reads, writes=writes)


def _tt(k, eng, out_ap, a, b, op, reads, writes):
    return k.op(eng, lambda e: e.tensor_tensor(out=out_ap, in0=a, in1=b, op=op), reads=reads, writes=writes)


def _ts(k, eng, out_ap, a, s1, s2, op0, op1, reads, writes):
    if op1 is None:
        return k.op(eng, lambda e: e.tensor_scalar(out=out_ap, in0=a, scalar1=s1, scalar2=None, op0=op0),
                    reads=reads, writes=writes)
    return k.op(eng, lambda e: e.tensor_scalar(out=out_ap, in0=a, scalar1=s1, scalar2=s2, op0=op0, op1=op1),
                reads=reads, writes=writes)


def _stt(k, eng, out_ap, a, s, b, op0, op1, reads, writes):
    return k.op(eng, lambda e: e.scalar_tensor_tensor(out=out_ap, in0=a, scalar=s, in1=b, op0=op0, op1=op1),
                reads=reads, writes=writes)


def _copy(k, eng, out_ap, in_ap, reads, writes):
    if eng == "act":
        return k.op("act", lambda e: e.copy(out=out_ap, in_=in_ap), reads=reads, writes=writes)
    return k.op(eng, lambda e: e.tensor_copy(out=out_ap, in_=in_ap), reads=reads, writes=writes)


def phase_adaln(P):
    k, nc = P.k, P.nc
    ada_w = P.inp("ada_w", [2, D, 6 * D])
    ada_b = P.inp("ada_bT", [128, 2, 48])
    ccol_d = P.inp("ccol", [128, 8, 2])
    n1g_d = P.inp("norm1_gT", [128, 2, 8])
    n2g_d = P.inp("norm2_gT", [128, 2, 8])
    P.mv = P.sb(P.es, "mv", [128, 2, 8, 8])
    mv = P.mv
    with ExitStack() as es:
        ccol = P.sb(es, "ccol_s", [128, 8, 2])
        silc = P.sb(es, "silc", [128, 8, 2])
        ab = P.sb(es, "adab", [128, 2, 48])
        ng = P.sb(es, "ng", [128, 2, 2, 8])
        acc = P.sb(es, "adacc", [128, 48, 2])
        wb = [P.sb(es, "adaw%d" % i, [128, 6 * D]) for i in range(2)]
        k.dma(ccol.ap[:], ccol_d[:, :, :], writes=[ccol], sem="misc")
        k.dma(ab.ap[:], ada_b[:, :, :], writes=[ab], sem="misc")
        k.dma(ng.ap[:, 0], n1g_d[:, :, :], writes=[ng], sem="misc")
        k.dma(ng.ap[:, 1], n2g_d[:, :, :], writes=[ng], sem="misc")
        _act(k, silc.ap[:], ccol.ap[:], AF.Silu, [ccol], [silc])
        it = 0
        for layer in range(2):
            for kc in range(8):
                w = wb[it % 2]
                k.dma(w.ap[:], ada_w[layer, kc * 128:(kc + 1) * 128, :], writes=[w], sem="adaw%d" % (it % 2))
                ps = P.psum[it % 2]
                for n in range(48):
                    _mm(k, ps, ps.ap[:, 2 * n:2 * n + 2], w.ap[:, n * 128:(n + 1) * 128], silc.ap[:, kc, :],
                        True, True, [w, silc], n == 47)
                pv = ps.ap[:, 0:96].rearrange("p (n c) -> p n c", c=2)
                if kc == 0:
                    _tt(k, "dve", acc.ap[:], pv, ab.ap[:, layer, :].unsqueeze(2).to_broadcast([128, 48, 2]), ALU.add,
                        [ps, ab], [acc])
                else:
                    _tt(k, "dve", acc.ap[:], pv, acc.ap[:], ALU.add, [ps, acc], [acc])
                it += 1
            def m(j, col):
                return acc.ap[:, j * 8:(j + 1) * 8, col]
            _stt(k, "dve", mv.ap[:, layer, 0, :], m(1, 0), 1.0, ng.ap[:, 0, layer, :], ALU.add, ALU.mult, [acc, ng], [mv])
            _copy(k, "dve", mv.ap[:, layer, 1, :], m(0, 0), [acc], [mv])
            _copy(k, "dve", mv.ap[:, layer, 2, :], m(2, 0), [acc], [mv])
            _stt(k, "dve", mv.ap[:, layer, 3, :], m(4, 0), 1.0, ng.ap[:, 1, layer, :], ALU.add, ALU.mult, [acc, ng], [mv])
            _copy(k, "dve", mv.ap[:, layer, 4, :], m(3, 0), [acc], [mv])
            _copy(k, "dve", mv.ap[:, layer, 5, :], m(5, 0), [acc], [mv])
            _stt(k, "dve", mv.ap[:, layer, 6, :], m(1, 1), 1.0, ng.ap[:, 0, layer, :], ALU.add, ALU.mult, [acc, ng], [mv])
            _copy(k, "dve", mv.ap[:, layer, 7, :], m(0, 1), [acc], [mv])
        if "mv" in P.dbg:
            o = P.out("mv_o", [128, 2, 8, 8])
            k.dma(o[:, :, :, :], mv.ap[:], reads=[mv], sem="misc")
        k.barrier()


def rope_tables():
    p = np.arange(128)
    j = p % 64
    blk = j // 32
    r = j % 32
    i = r % 16
    inv = (10000.0 ** (-(np.arange(0, 32, 2, dtype=np.float32)) / np.float32(32))).astype(np.float32)
    tok = np.arange(S)
    row = (tok // 64).astype(np.float32)
    col = (tok % 64).astype(np.float32)
    pos = np.where(blk[:, None] == 0, row[None, :], col[None, :]).astype(np.float32)
    ang = (pos * inv[i][:, None]).astype(np.float32)
    cos = np.cos(ang).astype(np.float32)
    sin = np.sin(ang).astype(np.float32)
    sgn = np.where(r < 16, -1.0, 1.0).astype(np.float32)
    rt = np.zeros((128, 128), np.float32)
    for m in range(128):
        partner = m + 16 if (m % 32) < 16 else m - 16
        rt[partner, m] = 1.0
    return cos, (sin * sgn[:, None]).astype(np.float32), rt


def setup_consts(P):
    k = P.k
    P.ones_f = P.sb(P.es, "ones_f", [128, 128])
    k.op("dve", lambda e: e.memset(P.ones_f.ap[:], 1.0), writes=[P.ones_f])
    P.ones_b = P.sb(P.es, "ones_b", [128, 128], BF16)
    k.op("dve", lambda e: e.memset(P.ones_b.ap[:], 1.0), writes=[P.ones_b])
    P._psi = 0
    P.ident_in = P.inp("ident", [128, 128])


def next_ps(P):
    lst = getattr(P, "ps_list", None) or P.psum
    b = lst[P._psi % len(lst)]
    P._psi += 1
    return b


def phase_front0(P):
    k, nc = P.k, P.nc
    xT = P.inp("xT", [D, S])
    P.xT_d = xT
    ctxT = P.inp("ctxT", [D, CTX])
    w_in = P.inp("w_in0", [D, 3 * D])
    cos_d = P.inp("rope_cos", [128, S])
    sin_d = P.inp("rope_sin", [128, S])
    rt_d = P.inp("rope_rt", [128, 128])
    P.QT = P.scratch("QT", [512, S], BF16)
    P.KT = P.scratch("KT", [512, LK], BF16)
    P.V = P.scratch("V", [LK, 512], BF16)
    P.U = P.scratch("U", [1536, S], F32)
    mv = P.mv
    xTv = xT.rearrange("(c p) t -> p c t", p=128)
    ctxv = ctxT.rearrange("(c p) t -> p c t", p=128)
    with ExitStack() as es:
        W = P.sb(es, "w_in_s", [128, 8, 3 * D], BF16)
        cos = P.sb(es, "cos_s", [128, S])
        sin = P.sb(es, "sin_s", [128, S])
        rt = P.sb(es, "rt_s", [128, 128])
        xt = [P.sb(es, "xt%d" % i, [128, 8, 512]) for i in range(2)]
        sq = P.sb(es, "sq", [128, 8, 512])
        rstd = P.sb(es, "rstd", [128, 512])
        hx = [P.sb(es, "hx%d" % i, [128, 8, 512], BF16) for i in range(2)]
        qf = [P.sb(es, "qf%d" % i, [128, 512]) for i in range(2)]
        t1 = [P.sb(es, "t1_%d" % i, [128, 512]) for i in range(2)]
        qb = [P.sb(es, "qb%d" % i, [128, 512], BF16) for i in range(3)]
        uf = [P.sb(es, "uf%d" % i, [128, 512]) for i in range(3)]
        vb = [P.sb(es, "vb%d" % i, [128, 512], BF16) for i in range(2)]
        w_v = w_in.rearrange("(c p) n -> p c n", p=128)
        for c in range(8):
            k.dma(W.ap[:, c, :], w_v[:, c, :], writes=[], wfree=[W], sem="w_in", q="pool")
        k.dma(cos.ap[:], cos_d[:, :], writes=[cos], sem="misc")
        k.dma(sin.ap[:], sin_d[:, :], writes=[sin], sem="misc")
        k.dma(rt.ap[:], rt_d[:, :], writes=[rt], sem="misc")
        cnt = {"q": 0, "u": 0, "v": 0, "f": 0}
        tiles = [("ctx", 0, CTX)] + [("x", t * 512, 512) for t in range(8)]
        def load(i):
            kind, t0, T = tiles[i]
            buf = xt[i % 2]
            src = ctxv[:, :, 0:T] if kind == "ctx" else xTv[:, :, t0:t0 + T]
            k.dma(buf.ap[:, :, 0:T], src, writes=[buf], sem="xt%d" % (i % 2))
        load(0)
        for i, (kind, t0, T) in enumerate(tiles):
            if i + 1 < len(tiles):
                load(i + 1)
            xb = xt[i % 2]
            h = hx[i % 2]
            ia, ib = (6, 7) if kind == "ctx" else (0, 1)
            _act(k, sq.ap[:, :, 0:T], xb.ap[:, :, 0:T], AF.Square, [xb], [sq])
            ps = next_ps(P)
            for c in range(8):
                _mm(k, ps, ps.ap[:, 0:T], P.ones_f.ap[:], sq.ap[:, c, 0:T], c == 0, c == 7, [P.ones_f, sq], c == 7)
            _act(k, rstd.ap[:, 0:T], ps.ap[:, 0:T], AF.Sqrt, [ps], [rstd], bias=EPS, scale=1.0 / D)
            k.op("dve", lambda e: e.reciprocal(out=rstd.ap[:, 0:T], in_=rstd.ap[:, 0:T]), reads=[rstd], writes=[rstd])
            _tt(k, "dve", sq.ap[:, :, 0:T], xb.ap[:, :, 0:T], rstd.ap[:, 0:T].unsqueeze(1).to_broadcast([128, 8, T]),
                ALU.mult, [xb, rstd], [sq])
            for c in range(8):
                _act(k, h.ap[:, c, 0:T], sq.ap[:, c, 0:T], AF.Identity, [sq, mv], [h],
                     bias=mv.ap[:, 0, ib, c:c + 1], scale=mv.ap[:, 0, ia, c:c + 1])
            nlist = list(range(4, 8)) if kind == "ctx" else list(range(0, 8)) + list(range(12, 24))
            for n in nlist:
                ps = next_ps(P)
                for c in range(8):
                    _mm(k, ps, ps.ap[:, 0:T], W.ap[:, c, n * 128:(n + 1) * 128], h.ap[:, c, 0:T], c == 0, c == 7,
                        [W, h], c == 7)
                if n >= 12:
                    u = uf[cnt["u"] % 3]
                    cnt["u"] += 1
                    _copy(k, "act", u.ap[:, 0:T], ps.ap[:, 0:T], [ps], [u])
                    k.dma(P.U.ap[(n - 12) * 128:(n - 11) * 128, t0:t0 + T], u.ap[:, 0:T], reads=[u], wfree=[P.U], sem="st_U")
                elif kind == "ctx":
                    q = qb[cnt["q"] % 3]
                    cnt["q"] += 1
                    _copy(k, "act", q.ap[:, 0:T], ps.ap[:, 0:T], [ps], [q])
                    k.dma(P.KT.ap[(n - 4) * 128:(n - 3) * 128, 0:T], q.ap[:, 0:T], reads=[q], wfree=[P.KT], sem="st_KT")
                else:
                    f = qf[cnt["f"] % 2]
                    tt1 = t1[cnt["f"] % 2]
                    cnt["f"] += 1
                    q = qb[cnt["q"] % 3]
                    cnt["q"] += 1
                    _copy(k, "act", f.ap[:], ps.ap[:], [ps], [f])
                    ps2 = next_ps(P)
                    _mm(k, ps2, ps2.ap[:], rt.ap[:], f.ap[:], True, True, [rt, f], True)
                    _tt(k, "dve", tt1.ap[:], f.ap[:], cos.ap[:, t0:t0 + T], ALU.mult, [f, cos], [tt1])
                    _tt(k, "dve", f.ap[:], ps2.ap[:], sin.ap[:, t0:t0 + T], ALU.mult, [ps2, sin], [f])
                    _tt(k, "dve", q.ap[:], tt1.ap[:], f.ap[:], ALU.add, [tt1, f], [q])
                    if n < 4:
                        k.dma(P.QT.ap[n * 128:(n + 1) * 128, t0:t0 + T], q.ap[:], reads=[q], wfree=[P.QT], sem="st_QT")
                    else:
                        k.dma(P.KT.ap[(n - 4) * 128:(n - 3) * 128, CTX + t0:CTX + t0 + T], q.ap[:], reads=[q],
                              wfree=[P.KT], sem="st_KT")
            for s4 in range(T // 128):
                ps = next_ps(P)
                for c in range(8):
                    _mm(k, ps, ps.ap[:], h.ap[:, c, s4 * 128:(s4 + 1) * 128], W.ap[:, c, 1024:1536], c == 0, c == 7,
                        [W, h], c == 7)
                v = vb[cnt["v"] % 2]
                cnt["v"] += 1
                _copy(k, "dve", v.ap[:], ps.ap[:], [ps], [v])
                r0 = (0 if kind == "ctx" else CTX + t0) + s4 * 128
                k.dma(P.V.ap[r0:r0 + 128, :], v.ap[:], reads=[v], wfree=[P.V], sem="st_V")
        k.barrier()


LAM_INIT0 = 0.8 - 0.6 * math.exp(-0.3 * 0)


def phase_attn(P):
    k, nc = P.k, P.nc
    lam_d = P.inp("lamv", [4, 64])
    sg_d = P.inp("subln_gT", [128, 1])
    if not hasattr(P, "OAB"):
        P.OAB = P.scratch("OAB", [D, S], BF16)
    with ExitStack() as es:
        lamt = P.sb(es, "lamt", [128, 4, 64])
        lw = P.sb(es, "lamw", [128, 8])
        sg = P.sb(es, "sublng", [128, 1])
        KTs = [P.sb(es, "KTs%d" % i, [128, LK], BF16) for i in range(2)]
        QTs = [P.sb(es, "QTs%d" % i, [128, S], BF16) for i in range(2)]
        Vs = [P.sb(es, "Vs%d" % i, [128, LK // 128, 128], BF16) for i in range(2)]
        eT = [P.sb(es, "eT%d" % i, [128, 512], BF16) for i in range(4)]
        r = [P.sb(es, "rs%d" % i, [128, 512]) for i in range(2)]
        tA = P.sb(es, "tA", [128, 512])
        tB = P.sb(es, "tB", [128, 512])
        tC = P.sb(es, "tC", [128, 512])
        ob = [P.sb(es, "ob%d" % i, [128, 512], BF16) for i in range(2)]
        for i in range(4):
            k.dma(lamt.ap[:, i, :], lam_d[i:i + 1, :].partition_broadcast(128), writes=[], wfree=[lamt], sem="misc")
        k.dma(sg.ap[:], sg_d[:, :], writes=[sg], sem="misc")
        _tt(k, "dve", lamt.ap[:, 0, :], lamt.ap[:, 0, :], lamt.ap[:, 1, :], ALU.mult, [lamt], [lamt])
        _tt(k, "dve", lamt.ap[:, 2, :], lamt.ap[:, 2, :], lamt.ap[:, 3, :], ALU.mult, [lamt], [lamt])
        k.op("dve", lambda e: e.reduce_sum(out=lw.ap[:, 0:1], in_=lamt.ap[:, 0, :], axis=AX.X), reads=[lamt], writes=[lw])
        k.op("dve", lambda e: e.reduce_sum(out=lw.ap[:, 1:2], in_=lamt.ap[:, 2, :], axis=AX.X), reads=[lamt], writes=[lw])
        _act(k, lw.ap[:, 2:4], lw.ap[:, 0:2], AF.Exp, [lw], [lw])
        _tt(k, "dve", lw.ap[:, 4:5], lw.ap[:, 3:4], lw.ap[:, 2:3], ALU.subtract, [lw], [lw])
        _ts(k, "dve", lw.ap[:, 4:5], lw.ap[:, 4:5], -LAM_INIT0, None, ALU.add, None, [lw], [lw])
        _ts(k, "dve", lw.ap[:, 5:6], sg.ap[:, 0:1], 1.0 - LAM_INIT0, None, ALU.mult, None, [sg], [lw])
        neglam = lw.ap[:, 4:5]
        gsc = lw.ap[:, 5:6]

        sc_banks = [P.psum[0], P.psum[1], P.psum[2]]
        acc_o = [P.psum[3], P.psum[4]]
        acc_s = [P.psum[5], P.psum[6]]
        ms_b = P.psum[7]
        NKC = LK // 128

        def load_hp(hp):
            i = hp % 2
            k.dma(KTs[i].ap[:], P.KT.ap[hp * 128:(hp + 1) * 128, :], reads=[P.KT], writes=[KTs[i]], sem="ld_kt%d" % i)
            k.dma(QTs[i].ap[:], P.QT.ap[hp * 128:(hp + 1) * 128, :], reads=[P.QT], writes=[QTs[i]], sem="ld_qt%d" % i)
            k.dma(Vs[i].ap[:], P.V.ap[:, hp * 128:(hp + 1) * 128].rearrange("(c p) v -> p c v", p=128),
                  reads=[P.V], writes=[Vs[i]], sem="ld_v%d" % i)

        steps = [(hp, qt, e, kc) for hp in range(4) for qt in range(8) for e in range(2) for kc in range(NKC)]

        def score(i):
            hp, qt, e, kc = steps[i]
            ps = sc_banks[i % 3]
            Kt, Qt = KTs[hp % 2], QTs[hp % 2]
            _mm(k, ps, ps.ap[:], Kt.ap[e * 64:(e + 1) * 64, kc * 128:(kc + 1) * 128],
                Qt.ap[e * 64:(e + 1) * 64, qt * 512:(qt + 1) * 512], True, True, [Kt, Qt], True)

        load_hp(0)
        score(0)
        score(1)
        for i, (hp, qt, e, kc) in enumerate(steps):
            if qt == 0 and e == 0 and kc == 0 and hp + 1 < 4:
                load_hp(hp + 1)
            ps = sc_banks[i % 3]
            et = eT[i % 4]
            _act(k, et.ap[:], ps.ap[:], AF.Exp, [ps], [et], scale=0.125)
            if i + 2 < len(steps):
                score(i + 2)
            Vt = Vs[hp % 2]
            _mm(k, acc_o[e], acc_o[e].ap[:], Vt.ap[:, kc, :], et.ap[:], kc == 0, kc == NKC - 1, [Vt, et], False)
            _mm(k, acc_s[e], acc_s[e].ap[:], P.ones_b.ap[:], et.ap[:], kc == 0, kc == NKC - 1, [P.ones_b, et], True)
            if e == 1 and kc == NKC - 1:
                for ee in range(2):
                    k.op("dve", lambda e_, ee=ee: e_.reciprocal(out=r[ee].ap[:], in_=acc_s[ee].ap[:]),
                         reads=[acc_s[ee]], writes=[r[ee]])
                _tt(k, "dve", tA.ap[:], acc_o[0].ap[:], r[0].ap[:], ALU.mult, [acc_o[0], r[0]], [tA])
                _tt(k, "dve", tB.ap[:], acc_o[1].ap[:], r[1].ap[:], ALU.mult, [acc_o[1], r[1]], [tB])
                _stt(k, "dve", tA.ap[:], tB.ap[:], neglam, tA.ap[:], ALU.mult, ALU.add, [tB, tA, lw], [tA])
                _act(k, tB.ap[:], tA.ap[:], AF.Square, [tA], [tB])
                _mm(k, ms_b, ms_b.ap[:], P.ones_f.ap[:], tB.ap[:], True, True, [P.ones_f, tB], True)
                _act(k, tC.ap[:], ms_b.ap[:], AF.Sqrt, [ms_b], [tC], bias=EPS, scale=1.0 / 128)
                k.op("dve", lambda e_: e_.reciprocal(out=tC.ap[:], in_=tC.ap[:]), reads=[tC], writes=[tC])
                o = ob[qt % 2]
                _stt(k, "dve", o.ap[:], tA.ap[:], gsc, tC.ap[:], ALU.mult, ALU.mult, [tA, tC, lw], [o])
                k.dma(P.OAB.ap[hp * 128:(hp + 1) * 128, qt * 512:(qt + 1) * 512], o.ap[:], reads=[o], wfree=[P.OAB],
                      sem="st_OAB")
        k.barrier()


HY_W = 512
HY_MIN_DECAY = math.log(1e-2) / 0.3
HY_MAX_DECAY = math.log(1e-2) / 1.5


def hyena_consts():
    n = S
    j = np.arange(n + 1, dtype=np.float64)
    t = j / (n - 1)
    bands = np.linspace(1e-4, 15.0, 16)
    ang = (2.0 * math.pi / n) * j[:, None] * bands[None, :]
    z = np.concatenate([t[:, None], np.cos(ang), -np.sin(ang)], axis=1)
    zpos = np.ascontiguousarray(z.T).astype(np.float32)
    delta = np.abs(np.linspace(HY_MIN_DECAY, HY_MAX_DECAY, HY_W)).astype(np.float32).reshape(1, HY_W)
    p = np.arange(128)[:, None]
    jc = np.arange(32)[None, :]
    jj = jc * 128 + p
    tf = -(jj / (n - 1.0))
    tb = -((jj + 1) / (n - 1.0))
    tb[jj == n - 1] = -1.0e4
    tcol = np.stack([tf, tb], axis=1).astype(np.float32)
    alpha = math.pi * (jj + 0.5) / N2
    rot = np.stack([np.cos(alpha), np.sin(alpha)], axis=1).astype(np.float32)
    return zpos, delta, tcol, rot


def dft_mats():
    f = np.arange(S, dtype=np.float64) + 0.5
    m = np.outer((2 * np.arange(S) + 1), (2 * np.arange(S) + 1)) % (4 * N2)
    ang = (2.0 * math.pi / (4 * N2)) * m
    c = np.cos(ang).astype(np.float32).astype(ml_dtypes.bfloat16)
    s = np.sin(ang).astype(np.float32).astype(ml_dtypes.bfloat16)
    return c, s


def _sin_reduced(k, out_ap, arg, tmp, reads_arg, writes_out):
    MAGIC = 12582912.0
    TWO_PI = 2.0 * math.pi
    a_ap, a_buf = arg
    t_ap, t_buf = tmp
    _ts(k, "dve", t_ap, a_ap, 1.0 / TWO_PI, MAGIC, ALU.mult, ALU.add, [a_buf], [t_buf])
    _ts(k, "dve", t_ap, t_ap, -MAGIC, -TWO_PI, ALU.add, ALU.mult, [t_buf], [t_buf])
    _tt(k, "dve", t_ap, t_ap, a_ap, ALU.add, [t_buf, a_buf], [t_buf])
    _act(k, out_ap, t_ap, AF.Sin, [t_buf], writes_out)


def dft_stream(P, es, n_groups=16):
    gw = S // n_groups
    P.dft_gw = gw
    P.dftC = [P.sb(es, "dftC%d" % i, [128, 32, gw], BF16) for i in range(2)]
    P.dftS = [P.sb(es, "dftS%d" % i, [128, 32, gw], BF16) for i in range(2)]
    P.dft_it = 0


def dft_load(P, g):
    k = P.k
    gw = P.dft_gw
    i = P.dft_it % 2
    P.dft_it += 1
    cv = P.dftC_d[:, g * gw:(g + 1) * gw].rearrange("(jc p) f -> p jc f", p=128)
    sv = P.dftS_d[:, g * gw:(g + 1) * gw].rearrange("(jc p) f -> p jc f", p=128)
    k.dma(P.dftC[i].ap[:], cv, writes=[P.dftC[i]], sem="dftC%d" % i)
    k.dma(P.dftS[i].ap[:], sv, writes=[P.dftS[i]], sem="dftS%d" % i)
    return P.dftC[i], P.dftS[i]


def phase_hy_filters(P):
    k, nc = P.k, P.nc
    zpos_d = P.inp("hy_zpos", [33, S + 1])
    delta_d = P.inp("hy_delta", [1, HY_W])
    tcol_d = P.inp("hy_tcol", [128, 2, 32])
    rot_d = P.inp("hy_rot", [128, 2, 32])
    w1_d = P.inp("hy_w1", [33, 64])
    w2_d = P.inp("hy_w2", [64, 64])
    w3_d = P.inp("hy_w3", [64, 2048])
    b3_d = P.inp("hy_b3", [1, 2048])
    vec_d = P.inp("hy_vec", [64, 4])
    bias_d = P.inp("hy_bias", [2, HY_W])
    P.dftC_d = P.inp("dft_c", [S, S], BF16)
    P.dftS_d = P.inp("dft_s", [S, S], BF16)
    P.KS = P.scratch("KS", [2, 2, S, HY_W], F32)
    NT = S + 1
    with ExitStack() as es:
        h2 = P.sb(es, "hyh2", [64, NT])
        es1 = ExitStack()
        zpos = P.sb(es1, "zpos", [33, NT])
        h1 = P.sb(es1, "hyh1", [64, NT])
        arg = P.sb(es1, "hyarg", [64, 512])
        tmp = P.sb(es1, "hytmp", [64, 512])
        w1 = P.sb(es1, "hyw1", [33, 64])
        w2 = P.sb(es1, "hyw2", [64, 64])
        vec = P.sb(es1, "hyvec", [64, 8])
        k.dma(zpos.ap[:], zpos_d[:, :], writes=[zpos], sem="misc")
        k.dma(w1.ap[:], w1_d[:, :], writes=[w1], sem="misc")
        k.dma(w2.ap[:], w2_d[:, :], writes=[w2], sem="misc")
        k.dma(vec.ap[:, 0:4], vec_d[:, :], writes=[vec], sem="misc")
        _tt(k, "dve", vec.ap[:, 4:5], vec.ap[:, 0:1], vec.ap[:, 1:2], ALU.mult, [vec], [vec])
        _tt(k, "dve", vec.ap[:, 5:6], vec.ap[:, 2:3], vec.ap[:, 3:4], ALU.mult, [vec], [vec])
        ntile = (NT + 511) // 512
        for layer, (wt, src, dst, kdim, fcol, bcol) in enumerate(((w1, zpos, h1, 33, 1, 4), (w2, h1, h2, 64, 3, 5))):
            for ti in range(ntile):
                c0 = ti * 512
                T = min(512, NT - c0)
                ps = next_ps(P)
                _mm(k, ps, ps.ap[0:64, 0:T], wt.ap[0:kdim, :], src.ap[0:kdim, c0:c0 + T], True, True, [wt, src], True)
                _ts(k, "dve", arg.ap[:, 0:T], ps.ap[0:64, 0:T], vec.ap[:, fcol:fcol + 1], vec.ap[:, bcol:bcol + 1],
                    ALU.mult, ALU.add, [ps, vec], [arg])
                _sin_reduced(k, dst.ap[:, c0:c0 + T], (arg.ap[:, 0:T], arg), (tmp.ap[:, 0:T], tmp), None, [dst])
        k.barrier()
        es1.close()
        w3 = P.sb(es, "hyw3", [64, 2048])
        b3 = P.sb(es, "hyb3", [128, 2048])
        delt = P.sb(es, "hydelta", [128, HY_W])
        tcol = P.sb(es, "hytcol", [128, 2, 32])
        rot = P.sb(es, "hyrot", [128, 4, 32])
        biasb = P.sb(es, "hybias", [128, 2, HY_W])
        inv = P.sb(es, "hyinv", [128, HY_W])
        A = P.sb(es, "hyA", [128, 32, HY_W], BF16)
        Bm = P.sb(es, "hyBm", [128, 32, HY_W], BF16)
        dec = [P.sb(es, "hydec%d" % i, [128, HY_W]) for i in range(2)]
        hf = P.sb(es, "hyhf", [128, HY_W])
        hb = P.sb(es, "hyhb", [128, HY_W])
        ab = [P.sb(es, "hyab%d" % i, [128, HY_W], BF16) for i in range(2)]
        t1 = P.sb(es, "hyt1", [128, HY_W])
        t2 = P.sb(es, "hyt2", [128, HY_W])
        ko = [P.sb(es, "hyko%d" % i, [128, 2, HY_W]) for i in range(2)]
        dft_stream(P, es)
        k.dma(w3.ap[:], w3_d[:, :], writes=[w3], sem="misc")
        k.dma(b3.ap[:], b3_d[0:1, :].partition_broadcast(128), writes=[b3], sem="misc")
        k.dma(delt.ap[:], delta_d[0:1, :].partition_broadcast(128), writes=[delt], sem="misc")
        k.dma(tcol.ap[:], tcol_d[:, :, :], writes=[tcol], sem="misc")
        k.dma(rot.ap[:, 0:2, :], rot_d[:, :, :], writes=[rot], sem="misc")
        for o in range(2):
            k.dma(biasb.ap[:, o, :], bias_d[o:o + 1, :].partition_broadcast(128), writes=[], wfree=[biasb], sem="misc")
        _ts(k, "dve", rot.ap[:, 2, :], rot.ap[:, 1, :], -1.0, None, ALU.mult, None, [rot], [rot])
        _ts(k, "dve", biasb.ap[:], biasb.ap[:], 2.0 / N2, None, ALU.mult, None, [biasb], [biasb])
        for o in range(2):
            nb = next_ps(P)
            for jc in range(32):
                psf = next_ps(P)
                psb = next_ps(P)
                if psf is nb or psb is nb:
                    psf = next_ps(P) if psf is nb else psf
                    psb = next_ps(P) if psb is nb else psb
                _mm(k, psf, psf.ap[:], h2.ap[:, jc * 128:jc * 128 + 128], w3.ap[:, o * 1024:o * 1024 + 512], True, True,
                    [h2, w3], True)
                _mm(k, psb, psb.ap[:], h2.ap[:, jc * 128 + 1:jc * 128 + 129], w3.ap[:, o * 1024 + 512:o * 1024 + 1024],
                    True, True, [h2, w3], True)
                _act(k, dec[0].ap[:], delt.ap[:], AF.Exp, [delt, tcol], [dec[0]], scale=tcol.ap[:, 0, jc:jc + 1])
                _act(k, dec[1].ap[:], delt.ap[:], AF.Exp, [delt, tcol], [dec[1]], scale=tcol.ap[:, 1, jc:jc + 1])
                _tt(k, "dve", hf.ap[:], psf.ap[:], b3.ap[:, o * 1024:o * 1024 + 512], ALU.add, [psf, b3], [hf])
                _tt(k, "dve", hb.ap[:], psb.ap[:], b3.ap[:, o * 1024 + 512:o * 1024 + 1024], ALU.add, [psb, b3], [hb])
                _tt(k, "pool", hf.ap[:], hf.ap[:], dec[0].ap[:], ALU.mult, [hf, dec[0]], [hf])
                _tt(k, "pool", hb.ap[:], hb.ap[:], dec[1].ap[:], ALU.mult, [hb, dec[1]], [hb])
                _tt(k, "pool", A.ap[:, jc, :], hf.ap[:], hb.ap[:], ALU.add, [hf, hb], [A])
                _tt(k, "pool", Bm.ap[:, jc, :], hb.ap[:], hf.ap[:], ALU.subtract, [hf, hb], [Bm])
                a = ab[jc % 2]
                _stt(k, "dve", t1.ap[:], hf.ap[:], -1.0, hf.ap[:], ALU.mult, ALU.max, [hf], [t1])
                _stt(k, "dve", t2.ap[:], hb.ap[:], -1.0, hb.ap[:], ALU.mult, ALU.max, [hb], [t2])
                _tt(k, "dve", a.ap[:], t1.ap[:], t2.ap[:], ALU.add, [t1, t2], [a])
                _mm(k, nb, nb.ap[:], P.ones_b.ap[:], a.ap[:], jc == 0, jc == 31, [P.ones_b, a], True)
            k.op("dve", lambda e: e.reciprocal(out=inv.ap[:], in_=nb.ap[:]), reads=[nb], writes=[inv])
            _ts(k, "dve", inv.ap[:], inv.ap[:], 2.0 / N2, None, ALU.mult, None, [inv], [inv])
            ng = S // P.dft_gw
            cpg = P.dft_gw // 128
            nxt = dft_load(P, 0)
            for g in range(ng):
                Cb, Sb = nxt
                if g + 1 < ng:
                    nxt = dft_load(P, g + 1)
                for ci in range(cpg):
                    fc = g * cpg + ci
                    pc = next_ps(P)
                    pss = next_ps(P)
                    for jc in range(32):
                        _mm(k, pc, pc.ap[:], Cb.ap[:, jc, ci * 128:(ci + 1) * 128], A.ap[:, jc, :], jc == 0, jc == 31,
                            [Cb, A], jc == 31)
                    for jc in range(32):
                        _mm(k, pss, pss.ap[:], Sb.ap[:, jc, ci * 128:(ci + 1) * 128], Bm.ap[:, jc, :], jc == 0, jc == 31,
                            [Sb, Bm], jc == 31)
                    kk = ko[fc % 2]
                    cosa = rot.ap[:, 0, fc:fc + 1]
                    sina = rot.ap[:, 1, fc:fc + 1]
                    nsina = rot.ap[:, 2, fc:fc + 1]
                    _ts(k, "dve", t1.ap[:], pss.ap[:], nsina, None, ALU.mult, None, [pss, rot], [t1])
                    _stt(k, "dve", t1.ap[:], pc.ap[:], cosa, t1.ap[:], ALU.mult, ALU.add, [pc, rot, t1], [t1])
                    _tt(k, "pool", t1.ap[:], t1.ap[:], inv.ap[:], ALU.mult, [t1, inv], [t1])
                    _tt(k, "pool", kk.ap[:, 0, :], t1.ap[:], biasb.ap[:, o, :], ALU.add, [t1, biasb], [kk])
                    _ts(k, "dve", t2.ap[:], pss.ap[:], cosa, None, ALU.mult, None, [pss, rot], [t2])
                    _stt(k, "dve", t2.ap[:], pc.ap[:], sina, t2.ap[:], ALU.mult, ALU.add, [pc, rot, t2], [t2])
                    _tt(k, "pool", kk.ap[:, 1, :], t2.ap[:], inv.ap[:], ALU.mult, [t2, inv], [kk])
                    k.dma(P.KS.ap[o, :, fc * 128:(fc + 1) * 128, :].rearrange("r p c -> p r c"), kk.ap[:], reads=[kk],
                          wfree=[P.KS], sem="st_KS")
        k.barrier()


def phase_hy_conv(P):
    k, nc = P.k, P.nc
    sw_d = P.inp("hy_swT", [128, 3, 12])
    sb_d = P.inp("hy_sbT", [128, 12])
    id_d = P.ident_in
    if not hasattr(P, "OAB"):
        P.OAB = P.scratch("OAB", [D, S], BF16)
    X1T = P.scratch("X1T", [S, HY_W], BF16)
    X2C = P.scratch("X2C", [HY_W, S], F32)
    with ExitStack() as es:
        sw = P.sb(es, "hysw", [128, 3, 12])
        sbb = P.sb(es, "hysb", [128, 12])
        ident = P.sb(es, "ident_s", [128, 128])
        vT = P.sb(es, "hyvT", [128, 32, HY_W], BF16)
        k.dma(sw.ap[:], sw_d[:, :, :], writes=[sw], sem="misc")
        k.dma(sbb.ap[:], sb_d[:, :], writes=[sbb], sem="misc")
        k.dma(ident.ap[:], id_d[:, :], writes=[ident], sem="misc")
        es1 = ExitStack()
        ucv = P.sb(es1, "hyucv", [128, 4, S])
        ub = [P.sb(es1, "hyub%d" % i, [128, S]) for i in range(2)]
        x1s = [P.sb(es1, "hyx1s%d" % i, [128, HY_W], BF16) for i in range(2)]

        def load_u(ch):
            k.dma(ub[ch % 2].ap[:], P.U.ap[ch * 128:(ch + 1) * 128, :], reads=[P.U], writes=[ub[ch % 2]], sem="ld_u%d" % (ch % 2))

        load_u(0)
        for ch in range(12):
            if ch + 1 < 12:
                load_u(ch + 1)
            u = ub[ch % 2]
            dst = ucv.ap[:, ch % 4, :]
            _ts(k, "dve", dst, u.ap[:], sw.ap[:, 1, ch:ch + 1], sbb.ap[:, ch:ch + 1], ALU.mult, ALU.add, [u, sw, sbb], [ucv])
            _stt(k, "dve", ucv.ap[:, ch % 4, 1:S], u.ap[:, 0:S - 1], sw.ap[:, 0, ch:ch + 1], ucv.ap[:, ch % 4, 1:S],
                 ALU.mult, ALU.add, [u, sw, ucv], [ucv])
            _stt(k, "dve", ucv.ap[:, ch % 4, 0:S - 1], u.ap[:, 1:S], sw.ap[:, 2, ch:ch + 1], ucv.ap[:, ch % 4, 0:S - 1],
                 ALU.mult, ALU.add, [u, sw, ucv], [ucv])
            if ch >= 8:
                k.dma(X2C.ap[(ch - 8) * 128:(ch - 7) * 128, :], ucv.ap[:, ch % 4, :], reads=[ucv], wfree=[X2C], sem="st_X2C")
            if ch == 3 or ch == 7:
                for sc in range(32):
                    ps = next_ps(P)
                    for c4 in range(4):
                        k.op("pe", lambda e, c4=c4, ps=ps, sc=sc: e.transpose(out=ps.ap[:, c4 * 128:(c4 + 1) * 128],
                                                                         in_=ucv.ap[:, c4, sc * 128:(sc + 1) * 128],
                                                                         identity=ident.ap[:]),
                             reads=[ucv, ident], writes=[ps], signal=(c4 == 3))
                    if ch == 3:
                        _copy(k, "act", vT.ap[:, sc, :], ps.ap[:], [ps], [vT])
                    else:
                        xs = x1s[sc % 2]
                        _copy(k, "act", xs.ap[:], ps.ap[:], [ps], [xs])
                        k.dma(X1T.ap[sc * 128:(sc + 1) * 128, :], xs.ap[:], reads=[xs], wfree=[X1T], sem="st_X1T")
        k.barrier()
        es1.close()
        Yr = P.sb(es, "hyYr", [128, 32, HY_W], BF16)
        Yn = P.sb(es, "hyYn", [128, 32, HY_W], BF16)
        kt = [P.sb(es, "hykt%d" % i, [128, 2, HY_W]) for i in range(3)]
        ta = P.sb(es, "hyta", [128, HY_W])
        tb = P.sb(es, "hytb", [128, HY_W])
        tc_ = P.sb(es, "hytc", [128, HY_W])
        td = P.sb(es, "hytd", [128, HY_W])
        xg = [P.sb(es, "hyxg%d" % i, [128, HY_W], BF16) for i in range(2)]
        x2g = [P.sb(es, "hyx2g%d" % i, [128, 4, 256]) for i in range(2)]
        obs = [P.sb(es, "hyob%d" % i, [128, 256], BF16) for i in range(3)]
        dft_stream(P, es)
        ng = S // P.dft_gw
        cpg = P.dft_gw // 128
        gw = P.dft_gw

        def forward(o):
            def load_k(fc):
                kk = kt[fc % 3]
                k.dma(kk.ap[:], P.KS.ap[o, :, fc * 128:(fc + 1) * 128, :].rearrange("r p c -> p r c"), reads=[P.KS],
                      writes=[kk], sem="ld_kt%d" % (fc % 3))
            load_k(0)
            nxt = dft_load(P, 0)
            for g in range(ng):
                Cb, Sb = nxt
                if g + 1 < ng:
                    nxt = dft_load(P, g + 1)
                for ci in range(cpg):
                    fc = g * cpg + ci
                    if fc + 1 < 32:
                        load_k(fc + 1)
                    kk = kt[fc % 3]
                    pc = next_ps(P)
                    pss = next_ps(P)
                    for sc in range(32):
                        _mm(k, pc, pc.ap[:], Cb.ap[:, sc, ci * 128:(ci + 1) * 128], vT.ap[:, sc, :], sc == 0, sc == 31,
                            [Cb, vT], sc == 31)
                    for sc in range(32):
                        _mm(k, pss, pss.ap[:], Sb.ap[:, sc, ci * 128:(ci + 1) * 128], vT.ap[:, sc, :], sc == 0, sc == 31,
                            [Sb, vT], sc == 31)
                    _tt(k, "dve", ta.ap[:], pc.ap[:], kk.ap[:, 0, :], ALU.mult, [pc, kk], [ta])
                    _tt(k, "dve", tb.ap[:], pss.ap[:], kk.ap[:, 1, :], ALU.mult, [pss, kk], [tb])
                    _tt(k, "pool", Yr.ap[:, fc, :], ta.ap[:], tb.ap[:], ALU.add, [ta, tb], [Yr])
                    _tt(k, "dve", tc_.ap[:], pss.ap[:], kk.ap[:, 0, :], ALU.mult, [pss, kk], [tc_])
                    _tt(k, "dve", td.ap[:], pc.ap[:], kk.ap[:, 1, :], ALU.mult, [pc, kk], [td])
                    _tt(k, "pool", Yn.ap[:, fc, :], tc_.ap[:], td.ap[:], ALU.subtract, [tc_, td], [Yn])

        forward(0)
        nxt = dft_load(P, 0)
        for g in range(ng):
            Cb, Sb = nxt
            if g + 1 < ng:
                nxt = dft_load(P, g + 1)
            for ci in range(cpg):
                tci = g * cpg + ci
                x1t = xg[tci % 2]
                k.dma(x1t.ap[:], X1T.ap[tci * 128:(tci + 1) * 128, :], reads=[X1T], writes=[x1t], sem="ld_xg%d" % (tci % 2))
                ps = next_ps(P)
                for fc in range(32):
                    _mm(k, ps, ps.ap[:], Cb.ap[:, fc, ci * 128:(ci + 1) * 128], Yr.ap[:, fc, :], fc == 0, False, [Cb, Yr], False)
                for fc in range(32):
                    _mm(k, ps, ps.ap[:], Sb.ap[:, fc, ci * 128:(ci + 1) * 128], Yn.ap[:, fc, :], False, fc == 31, [Sb, Yn],
                        fc == 31)
                _tt(k, "dve", vT.ap[:, tci, :], ps.ap[:], x1t.ap[:], ALU.mult, [ps, x1t], [vT])
        forward(1)
        nxt = dft_load(P, 0)
        oc = 0
        for g in range(ng):
            Cb, Sb = nxt
            if g + 1 < ng:
                nxt = dft_load(P, g + 1)
            x2t = x2g[g % 2]
            k.dma(x2t.ap[:, :, 0:gw], X2C.ap[:, g * gw:(g + 1) * gw].rearrange("(c p) t -> p c t", p=128), reads=[X2C],
                  writes=[x2t], sem="ld_x2g%d" % (g % 2))
            for cc in range(4):
                ps = next_ps(P)
                for fc in range(32):
                    _mm(k, ps, ps.ap[:, 0:gw], Yr.ap[:, fc, cc * 128:(cc + 1) * 128], Cb.ap[:, fc, :], fc == 0, False,
                        [Cb, Yr], False)
                for fc in range(32):
                    _mm(k, ps, ps.ap[:, 0:gw], Yn.ap[:, fc, cc * 128:(cc + 1) * 128], Sb.ap[:, fc, :], False, fc == 31,
                        [Sb, Yn], fc == 31)
                ob = obs[oc % 3]
                oc += 1
                _tt(k, "dve", ob.ap[:, 0:gw], ps.ap[:, 0:gw], x2t.ap[:, cc, 0:gw], ALU.mult, [ps, x2t], [ob])
                k.dma(P.OAB.ap[512 + cc * 128:512 + (cc + 1) * 128, g * gw:(g + 1) * gw], ob.ap[:, 0:gw], reads=[ob],
                      wfree=[P.OAB], sem="st_OAB")
        k.barrier()


def rms_rstd(P, xap, xbuf, T, sq, rstd):
    k = P.k
    _act(k, sq.ap[:, :, 0:T], xap, AF.Square, [xbuf], [sq])
    ps = next_ps(P)
    for c in range(8):
        _mm(k, ps, ps.ap[:, 0:T], P.ones_f.ap[:], sq.ap[:, c, 0:T], c == 0, c == 7, [P.ones_f, sq], c == 7)
    _act(k, rstd.ap[:, 0:T], ps.ap[:, 0:T], AF.Sqrt, [ps], [rstd], bias=EPS, scale=1.0 / D)
    k.op("dve", lambda e: e.reciprocal(out=rstd.ap[:, 0:T], in_=rstd.ap[:, 0:T]), reads=[rstd], writes=[rstd])


def phase_outproj0(P):
    k, nc = P.k, P.nc
    w_out = P.inp("w_out0", [D, D])
    xT = P.xT_d
    P.X1 = P.scratch("X1", [D, S], F32)
    mv = P.mv
    xTv = xT.rearrange("(c p) t -> p c t", p=128)
    with ExitStack() as es:
        W = P.sb(es, "w_out_s", [128, 8, D], BF16)
        xt = [P.sb(es, "opx%d" % i, [128, 8, 512]) for i in range(2)]
        ot = [P.sb(es, "opo%d" % i, [128, 8, 512], BF16) for i in range(2)]
        k.dma(W.ap[:], w_out.rearrange("(c p) n -> p c n", p=128), writes=[W], sem="w_out", q="pool")
        for t in range(8):
            xb, ob = xt[t % 2], ot[t % 2]
            k.dma(xb.ap[:], xTv[:, :, t * 512:(t + 1) * 512], writes=[xb], sem="opx%d" % (t % 2))
            k.dma(ob.ap[:], P.OAB.ap[:, t * 512:(t + 1) * 512].rearrange("(c p) t -> p c t", p=128), reads=[P.OAB],
                  writes=[ob], sem="opo%d" % (t % 2))
            for dc in range(8):
                ps = next_ps(P)
                for c in range(8):
                    _mm(k, ps, ps.ap[:], W.ap[:, c, dc * 128:(dc + 1) * 128], ob.ap[:, c, :], c == 0, c == 7, [W, ob], c == 7)
                _stt(k, "dve", xb.ap[:, dc, :], ps.ap[:], mv.ap[:, 0, 2, dc:dc + 1], xb.ap[:, dc, :], ALU.mult, ALU.add,
                     [ps, mv, xb], [xb])
            k.dma(P.X1.ap[:, t * 512:(t + 1) * 512].rearrange("(c p) t -> p c t", p=128), xb.ap[:], reads=[xb], wfree=[P.X1],
                  sem="st_X1")
        k.barrier()


def phase_moe(P, layer, Xin, Xout, final=False, experts=range(N_EXP)):
    k, nc = P.k, P.nc
    L = layer
    wr_d = P.inp("moe_wr%d" % L, [D, 36])
    br_d = P.inp("moe_br%d" % L, [1, 36])
    wg_d = P.inp("moe_w_gate%d" % L, [N_EXP, D, D_EXP])
    wu_d = P.inp("moe_w_up%d" % L, [N_EXP, D, D_EXP])
    wd_d = P.inp("moe_w_down%d" % L, [N_EXP, D_EXP, D])
    if final:
        fg_d = P.inp("final_gT", [128, 8])
    CT = P.scratch("CT%d" % L, [N_EXP, S], F32)
    mv = P.mv
    TH = 2048
    with ExitStack() as es:
        xh = P.sb(es, "mxh", [128, 8, TH])
        h = P.sb(es, "mh", [128, 8, TH], BF16)
        wgs = [P.sb(es, "mwg%d" % i, [128, 8, D_EXP], BF16) for i in range(2)]
        wus = [P.sb(es, "mwu%d" % i, [128, 8, D_EXP], BF16) for i in range(2)]
        wds = [P.sb(es, "mwd%d" % i, [128, 4, D], BF16) for i in range(2)]
        ident = P.sb(es, "mident", [128, 128])
        wr = P.sb(es, "mwr", [128, 8, 36])
        brb = P.sb(es, "mbr", [128, 36])
        k.dma(ident.ap[:], P.ident_in[:, :], writes=[ident], sem="misc")
        k.dma(wr.ap[:], wr_d.rearrange("(c p) n -> p c n", p=128), writes=[wr], sem="misc")
        k.dma(brb.ap[:], br_d[0:1, :].partition_broadcast(128), writes=[brb], sem="misc")
        if final:
            fg = P.sb(es, "mfg", [128, 8])
            k.dma(fg.ap[:], fg_d[:, :], writes=[fg], sem="misc")

        def load_w(e, i):
            k.dma(wgs[i].ap[:], wg_d[e].rearrange("(c p) n -> p c n", p=128), writes=[wgs[i]], sem="mwg%d" % i, q="pool")
            k.dma(wus[i].ap[:], wu_d[e].rearrange("(c p) n -> p c n", p=128), writes=[wus[i]], sem="mwu%d" % i, q="pool")
            k.dma(wds[i].ap[:], wd_d[e].rearrange("(c p) n -> p c n", p=128), writes=[wds[i]], sem="mwd%d" % i, q="pool")

        elist = list(experts)
        witer = 0
        for hh in range(S // TH):
            tok0 = hh * TH
            k.dma(xh.ap[:], Xin.ap[:, tok0:tok0 + TH].rearrange("(c p) t -> p c t", p=128), reads=[Xin], writes=[xh],
                  sem="mxh")
            load_w(elist[0], witer % 2)
            with ExitStack() as es2:
                sq = P.sb(es2, "msq", [128, 8, 512])
                hf = P.sb(es2, "mhf", [128, 8, 512])
                rstd = P.sb(es2, "mrstd", [128, 512])
                lg = P.sb(es2, "mlg", [128, 36])
                sm = P.sb(es2, "msm", [128, 16])
                gm = P.sb(es2, "mgm", [128, 4])
                e1 = P.sb(es2, "me1", [128, 8])
                e2 = P.sb(es2, "me2", [128, 8])
                k1 = P.sb(es2, "mk1", [128, 8])
                k2 = P.sb(es2, "mk2", [128, 8])
                c8 = P.sb(es2, "mc8", [128, 8])
                cmb = P.sb(es2, "mcmb", [128, 32])
                cT = P.sb(es2, "mcT", [32, 512])
                for tt in range(TH // 512):
                    c0 = tt * 512
                    rms_rstd(P, xh.ap[:, :, c0:c0 + 512], xh, 512, sq, rstd)
                    _tt(k, "dve", sq.ap[:], xh.ap[:, :, c0:c0 + 512], rstd.ap[:].unsqueeze(1).to_broadcast([128, 8, 512]),
                        ALU.mult, [xh, rstd], [sq])
                    for c in range(8):
                        _act(k, hf.ap[:, c, :], sq.ap[:, c, :], AF.Identity, [sq, mv], [hf],
                             bias=mv.ap[:, L, 4, c:c + 1], scale=mv.ap[:, L, 3, c:c + 1])
                    _copy(k, "dve", h.ap[:, :, c0:c0 + 512], hf.ap[:], [hf], [h])
                    pT = next_ps(P)
                    for s4 in range(4):
                        ps = next_ps(P)
                        if ps is pT:
                            ps = next_ps(P)
                        for c in range(8):
                            _mm(k, ps, ps.ap[:, 0:36], hf.ap[:, c, s4 * 128:(s4 + 1) * 128], wr.ap[:, c, :], c == 0, c == 7,
                                [hf, wr], c == 7)
                        _tt(k, "dve", lg.ap[:], ps.ap[:, 0:36], brb.ap[:], ALU.add, [ps, brb], [lg])
                        lge = lg.ap[:, 4:36].rearrange("p (g e) -> p g e", e=8)
                        k.op("dve", lambda e: e.reduce_max(out=sm.ap[:, 0:1], in_=lg.ap[:, 0:4], axis=AX.X), reads=[lg], writes=[sm])
                        _ts(k, "dve", gm.ap[:], lg.ap[:, 0:4], sm.ap[:, 0:1], None, ALU.is_ge, None, [lg, sm], [gm])
                        _ts(k, "dve", sm.ap[:, 1:2], sm.ap[:, 0:1], -1.0, None, ALU.mult, None, [sm], [sm])
                        k.op("act", lambda e: e.activation(out=e2.ap[:, 0:4], in_=lg.ap[:, 0:4], func=AF.Exp, bias=sm.ap[:, 1:2],
                                                           scale=1.0, accum_out=sm.ap[:, 2:3]), reads=[lg, sm], writes=[e2, sm])
                        k.op("dve", lambda e: e.reciprocal(out=sm.ap[:, 3:4], in_=sm.ap[:, 2:3]), reads=[sm], writes=[sm])
                        _ts(k, "dve", e1.ap[:], lge[:, 0, :], gm.ap[:, 0:1], None, ALU.mult, None, [lg, gm], [e1])
                        for g in range(1, 4):
                            _stt(k, "dve", e1.ap[:], lge[:, g, :], gm.ap[:, g:g + 1], e1.ap[:], ALU.mult, ALU.add, [lg, gm, e1], [e1])
                        k.op("dve", lambda e: e.reduce_max(out=sm.ap[:, 4:5], in_=e1.ap[:], axis=AX.X), reads=[e1], writes=[sm])
                        _ts(k, "dve", k1.ap[:], e1.ap[:], sm.ap[:, 4:5], None, ALU.is_ge, None, [e1, sm], [k1])
                        _stt(k, "dve", e2.ap[:], k1.ap[:], -1.0e30, e1.ap[:], ALU.mult, ALU.add, [k1, e1], [e2])
                        k.op("dve", lambda e: e.reduce_max(out=sm.ap[:, 5:6], in_=e2.ap[:], axis=AX.X), reads=[e2], writes=[sm])
                        _ts(k, "dve", k2.ap[:], e2.ap[:], sm.ap[:, 5:6], None, ALU.is_ge, None, [e2, sm], [k2])
                        _ts(k, "dve", sm.ap[:, 6:7], sm.ap[:, 4:5], -1.0, None, ALU.mult, None, [sm], [sm])
                        _act(k, sm.ap[:, 7:8], sm.ap[:, 5:6], AF.Exp, [sm], [sm], bias=sm.ap[:, 6:7], scale=1.0)
                        _ts(k, "dve", sm.ap[:, 7:8], sm.ap[:, 7:8], 1.0, None, ALU.add, None, [sm], [sm])
                        k.op("dve", lambda e: e.reciprocal(out=sm.ap[:, 8:9], in_=sm.ap[:, 7:8]), reads=[sm], writes=[sm])
                        _ts(k, "dve", sm.ap[:, 9:10], sm.ap[:, 8:9], -1.0, 1.0, ALU.mult, ALU.add, [sm], [sm])
                        _ts(k, "dve", c8.ap[:], k1.ap[:], sm.ap[:, 8:9], None, ALU.mult, None, [k1, sm], [c8])
                        _stt(k, "dve", c8.ap[:], k2.ap[:], sm.ap[:, 9:10], c8.ap[:], ALU.mult, ALU.add, [k2, sm, c8], [c8])
                        _ts(k, "dve", c8.ap[:], c8.ap[:], sm.ap[:, 3:4], None, ALU.mult, None, [c8, sm], [c8])
                        for g in range(4):
                            _ts(k, "dve", cmb.ap[:, g * 8:(g + 1) * 8], c8.ap[:], gm.ap[:, g:g + 1], None, ALU.mult, None,
                                [c8, gm], [cmb])
                        k.op("pe", lambda e, s4=s4: e.transpose(out=pT.ap[0:32, s4 * 128:(s4 + 1) * 128], in_=cmb.ap[:, :],
                                                          identity=ident.ap[:]), reads=[cmb, ident], writes=[pT], signal=True)
                    _copy(k, "dve", cT.ap[:], pT.ap[0:32, :], [pT], [cT])
                    k.dma(CT.ap[:, tok0 + c0:tok0 + c0 + 512], cT.ap[:], reads=[cT], wfree=[CT], sem="st_CT%d" % L)
                k.barrier()
            es3 = ExitStack()
            cB = [P.sb(es3, "mcb%d" % i, [128, TH]) for i in range(2)]
            he = [P.sb(es3, "mhe%d" % i, [128, 4, 512], BF16) for i in range(2)]
            asb = [P.sb(es3, "masb%d" % i, [128, 512]) for i in range(2)]
            bsb = [P.sb(es3, "mbsb%d" % i, [128, 512]) for i in range(2)]
            for ei, e in enumerate(elist):
                wi = witer % 2
                witer += 1
                if ei + 1 < len(elist):
                    load_w(elist[ei + 1], witer % 2)
                cb = cB[ei % 2]
                k.dma(cb.ap[:], CT.ap[e:e + 1, tok0:tok0 + TH].partition_broadcast(128), reads=[CT], writes=[cb],
                      sem="mcb%d" % (ei % 2))
                Wg, Wu, Wd = wgs[wi], wus[wi], wds[wi]
                for tt in range(TH // 512):
                    c0 = tt * 512
                    hb = he[tt % 2]
                    for f in range(4):
                        pg = next_ps(P)
                        pu = next_ps(P)
                        for c in range(8):
                            _mm(k, pg, pg.ap[:], Wg.ap[:, c, f * 128:(f + 1) * 128], h.ap[:, c, c0:c0 + 512], c == 0, c == 7,
                                [Wg, h], c == 7)
                        for c in range(8):
                            _mm(k, pu, pu.ap[:], Wu.ap[:, c, f * 128:(f + 1) * 128], h.ap[:, c, c0:c0 + 512], c == 0, c == 7,
                                [Wu, h], c == 7)
                        a = asb[f % 2]
                        b = bsb[f % 2]
                        _act(k, a.ap[:], pg.ap[:], AF.Silu, [pg], [a])
                        _tt(k, "dve", b.ap[:], pu.ap[:], a.ap[:], ALU.mult, [pu, a], [b])
                        _tt(k, "dve", hb.ap[:, f, :], b.ap[:], cb.ap[:, c0:c0 + 512], ALU.mult, [b, cb], [hb])
                    for dc in range(8):
                        pd = next_ps(P)
                        for f in range(4):
                            _mm(k, pd, pd.ap[:], Wd.ap[:, f, dc * 128:(dc + 1) * 128], hb.ap[:, f, :], f == 0, f == 3, [Wd, hb],
                                f == 3)
                        _stt(k, "dve", xh.ap[:, dc, c0:c0 + 512], pd.ap[:], mv.ap[:, L, 5, dc:dc + 1], xh.ap[:, dc, c0:c0 + 512],
                             ALU.mult, ALU.add, [pd, mv, xh], [xh])
            k.barrier()
            es3.close()
            if not final:
                k.dma(Xout.ap[:, tok0:tok0 + TH].rearrange("(c p) t -> p c t", p=128), xh.ap[:], reads=[xh], wfree=[Xout],
                      sem="st_" + Xout.name)
            else:
                with ExitStack() as es2:
                    sq = P.sb(es2, "fsq", [128, 8, 512])
                    rstd = P.sb(es2, "frstd", [128, 512])
                    for tt in range(TH // 512):
                        c0 = tt * 512
                        rms_rstd(P, xh.ap[:, :, c0:c0 + 512], xh, 512, sq, rstd)
                        _tt(k, "dve", sq.ap[:], xh.ap[:, :, c0:c0 + 512], rstd.ap[:].unsqueeze(1).to_broadcast([128, 8, 512]),
                            ALU.mult, [xh, rstd], [sq])
                        for c in range(8):
                            _act(k, xh.ap[:, c, c0:c0 + 512], sq.ap[:, c, :], AF.Identity, [sq, fg], [xh], scale=fg.ap[:, c:c + 1])
                    k.dma(Xout.ap[:, tok0:tok0 + TH].rearrange("(c p) t -> p c t", p=128), xh.ap[:], reads=[xh], wfree=[Xout],
                          sem="st_" + Xout.name)
                    k.barrier()
        k.barrier()


CV_K = 31
CV_PAD = 15


def phase_conformer(P, Xin, Xout):
    k, nc = P.k, P.nc
    L = 1
    w1_d = P.inp("cv_w1", [D, 2 * D])
    w2_d = P.inp("cv_w2", [D, D])
    b1_d = P.inp("cv_b1T", [128, 16])
    dww_d = P.inp("cv_dw_wT", [128, CV_K, 8])
    vec_d = P.inp("cv_vecT", [128, 4, 8])
    mv = P.mv
    with ExitStack() as es:
        GLU = P.scratch("GLU", [D, S + 2 * CV_PAD], BF16)
        GLUv = GLU.ap.rearrange("(c p) t -> p c t", p=128)
        zpad = P.sb(es, "cvzpad", [128, 8, CV_PAD], BF16)
        b1 = P.sb(es, "cvb1", [128, 16])
        dww = P.sb(es, "cvdww", [128, CV_K, 8])
        vec = P.sb(es, "cvvec", [128, 5, 8])
        identb = P.sb(es, "cvidb", [128, 128], BF16)
        identf = P.sb(es, "cvidf", [128, 128])
        k.dma(b1.ap[:], b1_d[:, :], writes=[b1], sem="misc")
        k.dma(dww.ap[:], dww_d[:, :, :], writes=[dww], sem="misc")
        k.dma(vec.ap[:, 0:4, :], vec_d[:, :, :], writes=[vec], sem="misc")
        k.dma(identf.ap[:], P.ident_in[:, :], writes=[identf], sem="misc")
        _copy(k, "dve", identb.ap[:], identf.ap[:], [identf], [identb])
        _tt(k, "dve", vec.ap[:, 4, :], vec.ap[:, 3, :], mv.ap[:, L, 2, :], ALU.mult, [vec, mv], [vec])
        k.op("dve", lambda e: e.memset(zpad.ap[:], 0.0), writes=[zpad])
        k.dma(GLUv[:, :, 0:CV_PAD], zpad.ap[:], reads=[zpad], wfree=[GLU], sem="st_GLU")
        k.dma(GLUv[:, :, S + CV_PAD:S + 2 * CV_PAD], zpad.ap[:], reads=[zpad], wfree=[GLU], sem="st_GLU")
        with ExitStack() as es1:
            W1 = P.sb(es1, "cvw1", [128, 8, 2 * D], BF16)
            xt = [P.sb(es1, "cvx%d" % i, [128, 8, 512]) for i in range(2)]
            sq = P.sb(es1, "cvsq", [128, 8, 512])
            rstd = P.sb(es1, "cvrstd", [128, 512])
            hx = [P.sb(es1, "cvhx%d" % i, [128, 8, 512], BF16) for i in range(2)]
            sg = [P.sb(es1, "cvsg%d" % i, [128, 512]) for i in range(2)]
            gt = [P.sb(es1, "cvgt%d" % i, [128, 8, 512], BF16) for i in range(2)]
            w1v = w1_d.rearrange("(c p) n -> p c n", p=128)
            for c in range(8):
                k.dma(W1.ap[:, c, :], w1v[:, c, :], writes=[], wfree=[W1], sem="cvw1", q="pool")
            for t in range(8):
                xb, h = xt[t % 2], hx[t % 2]
                k.dma(xb.ap[:], Xin.ap[:, t * 512:(t + 1) * 512].rearrange("(c p) t -> p c t", p=128), reads=[Xin], writes=[xb],
                      sem="cvx%d" % (t % 2))
                rms_rstd(P, xb.ap[:], xb, 512, sq, rstd)
                _tt(k, "dve", sq.ap[:], xb.ap[:], rstd.ap[:].unsqueeze(1).to_broadcast([128, 8, 512]), ALU.mult, [xb, rstd], [sq])
                for c in range(8):
                    _act(k, h.ap[:, c, :], sq.ap[:, c, :], AF.Identity, [sq, mv], [h], bias=mv.ap[:, L, 1, c:c + 1],
                         scale=mv.ap[:, L, 0, c:c + 1])
                for c in range(8):
                    pa = next_ps(P)
                    pg = next_ps(P)
                    for kc in range(8):
                        _mm(k, pa, pa.ap[:], W1.ap[:, kc, c * 128:(c + 1) * 128], h.ap[:, kc, :], kc == 0, kc == 7, [W1, h], kc == 7)
                    for kc in range(8):
                        _mm(k, pg, pg.ap[:], W1.ap[:, kc, D + c * 128:D + (c + 1) * 128], h.ap[:, kc, :], kc == 0, kc == 7, [W1, h],
                            kc == 7)
                    s_ = sg[c % 2]
                    _act(k, s_.ap[:], pg.ap[:], AF.Sigmoid, [pg, b1], [s_], bias=b1.ap[:, 8 + c:9 + c])
                    _stt(k, "dve", gt[t % 2].ap[:, c, :], pa.ap[:], b1.ap[:, c:c + 1], s_.ap[:],
                         ALU.add, ALU.mult, [pa, b1, s_], [gt[t % 2]])
                k.dma(GLUv[:, :, CV_PAD + t * 512:CV_PAD + (t + 1) * 512], gt[t % 2].ap[:], reads=[gt[t % 2]], wfree=[GLU],
                      sem="st_GLU")
            k.barrier()
        with ExitStack() as es2:
            W2 = P.sb(es2, "cvw2", [128, 8, D], BF16)
            dg = P.sb(es2, "cvdg", [128, 8, CV_K, 128], BF16)
            xt = [P.sb(es2, "cvx2%d" % i, [128, 8, 512]) for i in range(2)]
            gl = [P.sb(es2, "cvgl%d" % i, [128, 8, 512 + 2 * CV_PAD], BF16) for i in range(2)]
            cvt = P.sb(es2, "cvcvt", [128, 8, 512])
            sq = P.sb(es2, "cvsq2", [128, 8, 512])
            mean = P.sb(es2, "cvmean", [128, 512])
            rstd = P.sb(es2, "cvrstd2", [128, 512])
            act = [P.sb(es2, "cvact%d" % i, [128, 8, 512], BF16) for i in range(2)]
            k.dma(W2.ap[:], w2_d.rearrange("(c p) n -> p c n", p=128), writes=[W2], sem="cvw2", q="pool")
            for c in range(8):
                for j in range(CV_K):
                    _ts(k, "pool", dg.ap[:, c, j, :], identf.ap[:], dww.ap[:, j, c:c + 1], None, ALU.mult, None, [identf, dww], [dg])
            for t in range(8):
                xb, ab = xt[t % 2], act[t % 2]
                k.dma(xb.ap[:], Xin.ap[:, t * 512:(t + 1) * 512].rearrange("(c p) t -> p c t", p=128), reads=[Xin], writes=[xb],
                      sem="cvx2%d" % (t % 2))
                glu = gl[t % 2]
                k.dma(glu.ap[:], GLUv[:, :, t * 512:t * 512 + 512 + 2 * CV_PAD], reads=[GLU], writes=[glu], sem="cvgl%d" % (t % 2))
                for c in range(8):
                    ps = next_ps(P)
                    for j in range(CV_K):
                        _mm(k, ps, ps.ap[:], dg.ap[:, c, j, :], glu.ap[:, c, j:j + 512], j == 0, j == CV_K - 1,
                            [dg, glu], j == CV_K - 1)
                    _act(k, cvt.ap[:, c, :], ps.ap[:], AF.Identity, [ps, vec], [cvt], bias=vec.ap[:, 0, c:c + 1])
                _act(k, sq.ap[:], cvt.ap[:], AF.Square, [cvt], [sq])
                p1 = next_ps(P)
                p2 = next_ps(P)
                for c in range(8):
                    _mm(k, p1, p1.ap[:], P.ones_f.ap[:], cvt.ap[:, c, :], c == 0, c == 7, [P.ones_f, cvt], c == 7)
                for c in range(8):
                    _mm(k, p2, p2.ap[:], P.ones_f.ap[:], sq.ap[:, c, :], c == 0, c == 7, [P.ones_f, sq], c == 7)
                _ts(k, "dve", mean.ap[:], p1.ap[:], 1.0 / D, None, ALU.mult, None, [p1], [mean])
                _tt(k, "dve", rstd.ap[:], mean.ap[:], mean.ap[:], ALU.mult, [mean], [rstd])
                _stt(k, "dve", rstd.ap[:], p2.ap[:], 1.0 / D, rstd.ap[:], ALU.mult, ALU.subtract, [p2, rstd], [rstd])
                _act(k, rstd.ap[:], rstd.ap[:], AF.Sqrt, [rstd], [rstd], bias=EPS)
                k.op("dve", lambda e: e.reciprocal(out=rstd.ap[:], in_=rstd.ap[:]), reads=[rstd], writes=[rstd])
                _tt(k, "dve", cvt.ap[:], cvt.ap[:], mean.ap[:].unsqueeze(1).to_broadcast([128, 8, 512]), ALU.subtract,
                    [cvt, mean], [cvt])
                _tt(k, "pool", cvt.ap[:], cvt.ap[:], rstd.ap[:].unsqueeze(1).to_broadcast([128, 8, 512]), ALU.mult,
                    [cvt, rstd], [cvt])
                for c in range(8):
                    _act(k, ab.ap[:, c, :], cvt.ap[:, c, :], AF.Silu, [cvt, vec], [ab], bias=vec.ap[:, 2, c:c + 1],
                         scale=vec.ap[:, 1, c:c + 1])
                for dc in range(8):
                    ps = next_ps(P)
                    for kc in range(8):
                        _mm(k, ps, ps.ap[:], W2.ap[:, kc, dc * 128:(dc + 1) * 128], ab.ap[:, kc, :], kc == 0, kc == 7, [W2, ab], kc == 7)
                    _ts(k, "pool", xb.ap[:, dc, :], xb.ap[:, dc, :], vec.ap[:, 4, dc:dc + 1], None, ALU.add, None, [xb, vec], [xb])
                    _stt(k, "dve", xb.ap[:, dc, :], ps.ap[:], mv.ap[:, L, 2, dc:dc + 1], xb.ap[:, dc, :], ALU.mult, ALU.add,
                         [ps, mv, xb], [xb])
                k.dma(Xout.ap[:, t * 512:(t + 1) * 512].rearrange("(c p) t -> p c t", p=128), xb.ap[:], reads=[xb], wfree=[Xout],
                      sem="st_" + Xout.name)
            k.barrier()


def build_program():
    P = Prog()
    setup_consts(P)
    phase_adaln(P)
    phase_front0(P)
    phase_attn(P)
    phase_hy_filters(P)
    phase_hy_conv(P)
    phase_outproj0(P)
    X2 = P.scratch("X2", [D, S], F32)
    phase_moe(P, 0, P.X1, X2)
    X3 = P.scratch("X3", [D, S], F32)
    phase_conformer(P, X2, X3)
    outT = Buf(P.out("outT", [D, S]), "outT")
    phase_moe(P, 1, X3, outT, final=True)
    return P


def _colT(v):
    return np.ascontiguousarray(np.asarray(v, np.float32).reshape(-1, 128).T)


_CONST_CACHE = {}


def _consts():
    if not _CONST_CACHE:
        cos, sin, rt = rope_tables()
        zpos, delta, tcol, rot = hyena_consts()
        c, s = dft_mats()
        _CONST_CACHE.update(dict(rope_cos=cos, rope_sin=sin, rope_rt=rt, hy_zpos=zpos, hy_delta=delta, hy_tcol=tcol,
                                 hy_rot=rot, dft_c=c, dft_s=s, ident=np.eye(128, dtype=np.float32)))
    return _CONST_CACHE


def kernel(x, c, ctx, c_ctx, ada_w, ada_b, norm1_g, norm2_g, final_g,
           w_in0, w_out0, lam_q1, lam_k1, lam_q2, lam_k2, subln_g,
           hy_short_w, hy_short_b, hy_w1, hy_b1, hy_fr1, hy_w2, hy_b2, hy_fr2, hy_w3, hy_b3, hy_bias,
           cv_w1, cv_b1, cv_dw_w, cv_dw_b, cv_ln_g, cv_ln_b, cv_w2, cv_b2,
           moe_wg, moe_bg, moe_we, moe_be, moe_w_gate, moe_w_up, moe_w_down):
    f32 = np.float32
    A = lambda v: np.ascontiguousarray(np.asarray(v, f32))
    P = build_program()
    shared = dict(_consts())
    shared.update({
        "ada_w": A(ada_w),
        "ada_bT": np.ascontiguousarray(np.stack([_colT(ada_b[l]) for l in range(2)], axis=1)),
        "norm1_gT": np.ascontiguousarray(np.stack([_colT(norm1_g[l]) for l in range(2)], axis=1)),
        "norm2_gT": np.ascontiguousarray(np.stack([_colT(norm2_g[l]) for l in range(2)], axis=1)),
        "final_gT": _colT(final_g),
        "w_in0": A(w_in0[0]), "w_out0": A(w_out0[0]),
        "lamv": A(np.stack([lam_q1[0], lam_k1[0], lam_q2[0], lam_k2[0]])),
        "subln_gT": A(subln_g[0]).reshape(128, 1),
        "hy_swT": np.ascontiguousarray(np.stack([_colT(hy_short_w[0][j]) for j in range(3)], axis=1)),
        "hy_sbT": _colT(hy_short_b[0]),
        "hy_w1": A(hy_w1[0]), "hy_w2": A(hy_w2[0]), "hy_w3": A(hy_w3[0]), "hy_b3": A(hy_b3[0]).reshape(1, 2048),
        "hy_vec": np.ascontiguousarray(np.stack([A(hy_b1[0]), A(hy_fr1[0]), A(hy_b2[0]), A(hy_fr2[0])], axis=1)),
        "hy_bias": A(hy_bias[0]),
        "cv_w1": A(cv_w1[0]), "cv_w2": A(cv_w2[0]), "cv_b1T": _colT(cv_b1[0]),
        "cv_dw_wT": np.ascontiguousarray(np.stack([_colT(cv_dw_w[0][j]) for j in range(CV_K)], axis=1)),
        "cv_vecT": np.ascontiguousarray(np.stack([_colT(cv_dw_b[0]), _colT(cv_ln_g[0]), _colT(cv_ln_b[0]), _colT(cv_b2[0])], axis=1)),
    })
    for L in range(2):
        shared["moe_wr%d" % L] = np.ascontiguousarray(np.concatenate([A(moe_wg[L]), A(moe_we[L])], axis=1))
        shared["moe_br%d" % L] = np.concatenate([A(moe_bg[L]), A(moe_be[L])]).reshape(1, 36)
        shared["moe_w_gate%d" % L] = A(moe_w_gate[L])
        shared["moe_w_up%d" % L] = A(moe_w_up[L])
        shared["moe_w_down%d" % L] = A(moe_w_down[L])
    x = np.asarray(x, f32)
    ctx = np.asarray(ctx, f32)
    c = np.asarray(c, f32)
    c_ctx = np.asarray(c_ctx, f32)
    in_maps = []
    for b in range(8):
        m = dict(shared)
        m["xT"] = np.ascontiguousarray(x[b].T)
        m["ctxT"] = np.ascontiguousarray(ctx[b].T)
        m["ccol"] = np.ascontiguousarray(np.stack([_colT(c[b]), _colT(c_ctx)], axis=-1))
        in_maps.append({k_: m[k_] for k_ in P.inputs})
    res = run_bass_kernel_spmd(P.nc, in_maps, core_ids=list(range(8)))
    out = np.stack([np.ascontiguousarray(res.results[b]["outT"].T) for b in range(8)], axis=0)
    return out.astype(f32)


NT_TILES = (2 * S) // 512 + N_EXP
NSLOT = NT_TILES * 512


def moe_consts():
    lt = np.triu(np.ones((128, 128), np.float32), 1)
    tstart = np.tile((512.0 * np.arange(NT_TILES, dtype=np.float32))[None, :], (32, 1)).astype(np.float32)
    p = np.arange(128)[:, None, None]
    c = np.arange(32)[None, :, None]
    kk = np.arange(2)[None, None, :]
    src = np.broadcast_to(c * 128 + p, (128, 32, 2))
    dst = kk * S + c * 128 + p
    slotvals = np.stack([src, np.broadcast_to(dst, (128, 32, 2))], axis=-1).astype(np.int32)
    init = np.zeros((128, NSLOT // 128, 2), np.int32)
    init[..., 0] = S
    init[..., 1] = 1 << 30
    return lt, tstart, slotvals, init


def phase_moe_sparse(P, layer, Xin, Xout, final=False):
    k, nc = P.k, P.nc
    L = layer
    wr_d = P.inp("moe_wr%d" % L, [D, 36])
    br_d = P.inp("moe_br%d" % L, [1, 36])
    wg_d = P.inp("moe_w_gate%d" % L, [N_EXP, D, D_EXP])
    wu_d = P.inp("moe_w_up%d" % L, [N_EXP, D, D_EXP])
    wd_d = P.inp("moe_w_down%d" % L, [N_EXP, D_EXP, D])
    if not hasattr(P, "moe_c"):
        P.moe_c = (P.inp("moe_lt", [128, 128]), P.inp("moe_tstart", [32, NT_TILES]),
                   P.inp("moe_slotvals", [128, 32, 2, 2], I32), P.inp("moe_idxinit", [128, NSLOT // 128, 2], I32))
    lt_d, ts_d, sv_d, ii_d = P.moe_c
    if final:
        fg_d = P.inp("final_gT", [128, 8])
    HT = P.scratch("HT%d" % L, [S + 128, D], BF16)
    IDX = P.scratch("IDX%d" % L, [NSLOT, 2], I32)
    Y2 = P.scratch("Y2_%d" % L, [2 * S, D], F32)
    mv = P.mv
    ps_all = P.psum
    with ExitStack() as es:
        ident = P.sb(es, "sident", [128, 128])
        identb = P.sb(es, "sidentb", [128, 128], BF16)
        ltri = P.sb(es, "sltri", [128, 128])
        wr = P.sb(es, "swr", [128, 8, 36])
        brb = P.sb(es, "sbr", [128, 36])
        Mst = P.sb(es, "sMst", [128, 32, 2, 32])
        rank = P.sb(es, "srank", [128, 32, 32])
        cum = P.sb(es, "scum", [128, 32])
        wts = P.sb(es, "swts", [128, 32, 2])
        eid_i = P.sb(es, "seid", [128, NT_TILES], I32)
        k.dma(ident.ap[:], P.ident_in[:, :], writes=[ident], sem="misc")
        k.dma(ltri.ap[:], lt_d[:, :], writes=[ltri], sem="misc")
        k.dma(wr.ap[:], wr_d.rearrange("(c p) n -> p c n", p=128), writes=[wr], sem="misc")
        k.dma(brb.ap[:], br_d[0:1, :].partition_broadcast(128), writes=[brb], sem="misc")
        _copy(k, "dve", identb.ap[:], ident.ap[:], [ident], [identb])
        k.op("dve", lambda e: e.memset(cum.ap[:], 0.0), writes=[cum])
        with ExitStack() as es0:
            ii = P.sb(es0, "sii", [128, NSLOT // 128, 2], I32)
            zr = P.sb(es0, "szr", [128, D], BF16)
            k.dma(ii.ap[:], ii_d[:, :, :], writes=[ii], sem="misc")
            k.dma(IDX.ap.rearrange("(p r) c -> p r c", p=128), ii.ap[:], reads=[ii], writes=[IDX], sem="st_IDX%d" % L)
            k.op("dve", lambda e: e.memset(zr.ap[:], 0.0), writes=[zr])
            k.dma(HT.ap[S:S + 128, :], zr.ap[:], reads=[zr], wfree=[HT], sem="st_HT%d" % L)
            k.barrier()
        with ExitStack() as es2:
            xt = [P.sb(es2, "sx%d" % i, [128, 8, 512]) for i in range(2)]
            sq = P.sb(es2, "ssq", [128, 8, 512])
            hf = P.sb(es2, "shf", [128, 8, 512])
            rstd = P.sb(es2, "srstd", [128, 512])
            lg = P.sb(es2, "slg", [128, 36])
            sm = P.sb(es2, "ssm", [128, 16])
            gm = P.sb(es2, "sgm", [128, 4])
            e1 = P.sb(es2, "se1", [128, 8])
            e2 = P.sb(es2, "se2", [128, 8])
            k1 = P.sb(es2, "sk1", [128, 8])
            k2 = P.sb(es2, "sk2", [128, 8])
            msum = P.sb(es2, "smsum", [128, 32])
            htk = [P.sb(es2, "shtk%d" % i, [128, D], BF16) for i in range(2)]
            P.ps_list = ps_all
            for tt in range(8):
                xb = xt[tt % 2]
                k.dma(xb.ap[:], Xin.ap[:, tt * 512:(tt + 1) * 512].rearrange("(c p) t -> p c t", p=128), reads=[Xin],
                      writes=[xb], sem="sx%d" % (tt % 2))
                rms_rstd(P, xb.ap[:], xb, 512, sq, rstd)
                _tt(k, "dve", sq.ap[:], xb.ap[:], rstd.ap[:].unsqueeze(1).to_broadcast([128, 8, 512]), ALU.mult, [xb, rstd], [sq])
                for c in range(8):
                    _act(k, hf.ap[:, c, :], sq.ap[:, c, :], AF.Identity, [sq, mv], [hf],
                         bias=mv.ap[:, L, 4, c:c + 1], scale=mv.ap[:, L, 3, c:c + 1])
                for s4 in range(4):
                    cg = tt * 4 + s4
                    pa, pb = next_ps(P), next_ps(P)
                    for c in range(8):
                        pp = pa if c < 4 else pb
                        k.op("pe", lambda e, c=c, pp=pp, s4=s4: e.transpose(out=pp.ap[:, (c % 4) * 128:(c % 4 + 1) * 128],
                                                                            in_=hf.ap[:, c, s4 * 128:(s4 + 1) * 128],
                                                                            identity=ident.ap[:]),
                             reads=[hf, ident], writes=[pp], signal=(c % 4 == 3))
                    hk = htk[cg % 2]
                    _copy(k, "act", hk.ap[:, 0:512], pa.ap[:], [pa], [hk])
                    _copy(k, "act", hk.ap[:, 512:1024], pb.ap[:], [pb], [hk])
                    k.dma(HT.ap[cg * 128:(cg + 1) * 128, :], hk.ap[:], reads=[hk], wfree=[HT], sem="st_HT%d" % L)
                    ps = next_ps(P)
                    for c in range(8):
                        _mm(k, ps, ps.ap[:, 0:36], hf.ap[:, c, s4 * 128:(s4 + 1) * 128], wr.ap[:, c, :], c == 0, c == 7,
                            [hf, wr], c == 7)
                    _tt(k, "dve", lg.ap[:], ps.ap[:, 0:36], brb.ap[:], ALU.add, [ps, brb], [lg])
                    lge = lg.ap[:, 4:36].rearrange("p (g e) -> p g e", e=8)
                    k.op("dve", lambda e: e.reduce_max(out=sm.ap[:, 0:1], in_=lg.ap[:, 0:4], axis=AX.X), reads=[lg], writes=[sm])
                    _ts(k, "dve", gm.ap[:], lg.ap[:, 0:4], sm.ap[:, 0:1], None, ALU.is_ge, None, [lg, sm], [gm])
                    _ts(k, "dve", sm.ap[:, 1:2], sm.ap[:, 0:1], -1.0, None, ALU.mult, None, [sm], [sm])
                    k.op("act", lambda e: e.activation(out=e2.ap[:, 0:4], in_=lg.ap[:, 0:4], func=AF.Exp, bias=sm.ap[:, 1:2],
                                                       scale=1.0, accum_out=sm.ap[:, 2:3]), reads=[lg, sm], writes=[e2, sm])
                    k.op("dve", lambda e: e.reciprocal(out=sm.ap[:, 3:4], in_=sm.ap[:, 2:3]), reads=[sm], writes=[sm])
                    _ts(k, "dve", e1.ap[:], lge[:, 0, :], gm.ap[:, 0:1], None, ALU.mult, None, [lg, gm], [e1])
                    for g in range(1, 4):
                        _stt(k, "dve", e1.ap[:], lge[:, g, :], gm.ap[:, g:g + 1], e1.ap[:], ALU.mult, ALU.add, [lg, gm, e1], [e1])
                    k.op("dve", lambda e: e.reduce_max(out=sm.ap[:, 4:5], in_=e1.ap[:], axis=AX.X), reads=[e1], writes=[sm])
                    _ts(k, "dve", k1.ap[:], e1.ap[:], sm.ap[:, 4:5], None, ALU.is_ge, None, [e1, sm], [k1])
                    _stt(k, "dve", e2.ap[:], k1.ap[:], -1.0e30, e1.ap[:], ALU.mult, ALU.add, [k1, e1], [e2])
                    k.op("dve", lambda e: e.reduce_max(out=sm.ap[:, 5:6], in_=e2.ap[:], axis=AX.X), reads=[e2], writes=[sm])
                    _ts(k, "dve", k2.ap[:], e2.ap[:], sm.ap[:, 5:6], None, ALU.is_ge, None, [e2, sm], [k2])
                    _ts(k, "dve", sm.ap[:, 6:7], sm.ap[:, 4:5], -1.0, None, ALU.mult, None, [sm], [sm])
                    _act(k, sm.ap[:, 7:8], sm.ap[:, 5:6], AF.Exp, [sm], [sm], bias=sm.ap[:, 6:7], scale=1.0)
                    _ts(k, "dve", sm.ap[:, 7:8], sm.ap[:, 7:8], 1.0, None, ALU.add, None, [sm], [sm])
                    k.op("dve", lambda e: e.reciprocal(out=sm.ap[:, 8:9], in_=sm.ap[:, 7:8]), reads=[sm], writes=[sm])
                    _ts(k, "dve", sm.ap[:, 9:10], sm.ap[:, 8:9], -1.0, 1.0, ALU.mult, ALU.add, [sm], [sm])
                    _ts(k, "dve", wts.ap[:, cg, :], sm.ap[:, 8:10], sm.ap[:, 3:4], None, ALU.mult, None, [sm], [wts])
                    for g in range(4):
                        _ts(k, "dve", Mst.ap[:, cg, 0, g * 8:(g + 1) * 8], k1.ap[:], gm.ap[:, g:g + 1], None, ALU.mult, None,
                            [k1, gm], [Mst])
                        _ts(k, "dve", Mst.ap[:, cg, 1, g * 8:(g + 1) * 8], k2.ap[:], gm.ap[:, g:g + 1], None, ALU.mult, None,
                            [k2, gm], [Mst])
                    _tt(k, "dve", msum.ap[:], Mst.ap[:, cg, 0, :], Mst.ap[:, cg, 1, :], ALU.add, [Mst], [msum])
                    pr = next_ps(P)
                    pt = next_ps(P)
                    _mm(k, pr, pr.ap[:, 0:32], ltri.ap[:], msum.ap[:], True, True, [ltri, msum], True)
                    _mm(k, pt, pt.ap[:, 0:32], P.ones_f.ap[:], msum.ap[:], True, True, [P.ones_f, msum], True)
                    _tt(k, "dve", rank.ap[:, cg, :], pr.ap[:, 0:32], cum.ap[:], ALU.add, [pr, cum], [rank])
                    _tt(k, "dve", cum.ap[:], pt.ap[:, 0:32], cum.ap[:], ALU.add, [pt, cum], [cum])
            k.barrier()
        with ExitStack() as es3:
            ncol = P.sb(es3, "sncol", [32, 2])
            pcol = P.sb(es3, "spcol", [32, 2])
            Up = P.sb(es3, "sUp", [32, 32])
            Ui = P.sb(es3, "sUi", [32, 32])
            offs = P.sb(es3, "soffs", [128, 32])
            endc = P.sb(es3, "sendc", [32, 2])
            tst = P.sb(es3, "ststart", [32, NT_TILES])
            cmpm = P.sb(es3, "scmp", [32, NT_TILES])
            eidf = P.sb(es3, "seidf", [128, NT_TILES])
            tmp3 = P.sb(es3, "stmp3", [128, 32, 32])
            prod = P.sb(es3, "sprod", [128, 32, 32])
            posf = P.sb(es3, "sposf", [128, 32, 2])
            posi = P.sb(es3, "sposi", [128, 32, 2], I32)
            sv = P.sb(es3, "ssv", [128, 32, 2, 2], I32)
            k.dma(tst.ap[:], ts_d[:, :], writes=[tst], sem="misc")
            k.dma(sv.ap[:], sv_d[:, :, :, :], writes=[sv], sem="misc")
            MAGIC = 12582912.0
            p0 = next_ps(P)
            _mm(k, p0, p0.ap[0:32, 0:2], cum.ap[0:1, :], P.ones_f.ap[0:1, 0:2], True, True, [cum, P.ones_f], True)
            _copy(k, "dve", ncol.ap[:], p0.ap[0:32, 0:2], [p0], [ncol])
            _ts(k, "dve", pcol.ap[:], ncol.ap[:], 511.0, 1.0 / 512.0, ALU.add, ALU.mult, [ncol], [pcol])
            _ts(k, "dve", pcol.ap[:], pcol.ap[:], -0.5 + 1.0 / 1024.0, MAGIC, ALU.add, ALU.add, [pcol], [pcol])
            _ts(k, "dve", pcol.ap[:], pcol.ap[:], -MAGIC, 512.0, ALU.add, ALU.mult, [pcol], [pcol])
            _ts(k, "dve", Up.ap[:], ltri.ap[0:32, 0:32], pcol.ap[:, 0:1], None, ALU.mult, None, [ltri, pcol], [Up])
            _tt(k, "dve", Ui.ap[:], ltri.ap[0:32, 0:32], ident.ap[0:32, 0:32], ALU.add, [ltri, ident], [Ui])
            p1 = next_ps(P)
            _mm(k, p1, p1.ap[:, 0:32], P.ones_f.ap[0:32, :], Up.ap[:], True, True, [P.ones_f, Up], True)
            _copy(k, "dve", offs.ap[:], p1.ap[:, 0:32], [p1], [offs])
            p2 = next_ps(P)
            _mm(k, p2, p2.ap[0:32, 0:2], Ui.ap[:], pcol.ap[:], True, True, [Ui, pcol], True)
            _copy(k, "dve", endc.ap[:], p2.ap[0:32, 0:2], [p2], [endc])
            _ts(k, "dve", cmpm.ap[:], tst.ap[:], endc.ap[:, 0:1], None, ALU.is_ge, None, [tst, endc], [cmpm])
            p3 = next_ps(P)
            _mm(k, p3, p3.ap[:, 0:NT_TILES], P.ones_f.ap[0:32, :], cmpm.ap[:], True, True, [P.ones_f, cmpm], True)
            _ts(k, "dve", eidf.ap[:], p3.ap[:, 0:NT_TILES], float(N_EXP - 1), None, ALU.min, None, [p3], [eidf])
            _copy(k, "dve", eid_i.ap[:], eidf.ap[:], [eidf], [eid_i])
            _tt(k, "dve", tmp3.ap[:], rank.ap[:], offs.ap[:].unsqueeze(1).to_broadcast([128, 32, 32]), ALU.add, [rank, offs], [tmp3])
            for kk in range(2):
                _tt(k, "dve", prod.ap[:], tmp3.ap[:], Mst.ap[:, :, kk, :], ALU.mult, [tmp3, Mst], [prod])
                k.op("dve", lambda e, kk=kk: e.reduce_sum(out=posf.ap[:, :, kk], in_=prod.ap[:], axis=AX.X), reads=[prod], writes=[posf])
            _copy(k, "dve", posi.ap[:], posf.ap[:], [posf], [posi])
            for c in range(32):
                for kk in range(2):
                    k.dma_raw("pool", lambda e, c=c, kk=kk: e.indirect_dma_start(
                        out=IDX.ap[:, :], out_offset=bass.IndirectOffsetOnAxis(ap=posi.ap[:, c, kk:kk + 1], axis=0),
                        in_=sv.ap[:, c, kk, :], in_offset=None, bounds_check=NSLOT - 1, oob_is_err=False),
                        reads=[posi, sv, IDX], wfree=[IDX], sem="st_IDX%d" % L)
            k.barrier()
        with ExitStack() as es4:
            wgs = [P.sb(es4, "swg%d" % i, [128, 8, D_EXP], BF16) for i in range(2)]
            wus = [P.sb(es4, "swu%d" % i, [128, 8, D_EXP], BF16) for i in range(2)]
            wds = [P.sb(es4, "swd%d" % i, [128, 4, D], BF16) for i in range(2)]
            idt = [P.sb(es4, "sidt%d" % i, [128, 4, 2], I32) for i in range(2)]
            hg = [P.sb(es4, "shg%d" % i, [128, D], BF16) for i in range(4)]
            hT = [P.sb(es4, "shT%d" % i, [128, 8, 512], BF16) for i in range(2)]
            he = [P.sb(es4, "she%d" % i, [128, 4, 512], BF16) for i in range(2)]
            asb = [P.sb(es4, "sasb%d" % i, [128, 512]) for i in range(2)]
            ysl = [P.sb(es4, "sysl%d" % i, [128, D]) for i in range(2)]
            reg = nc.gpsimd.alloc_register("moe_eid%d" % L)
            tr_banks = [ps_all[0], ps_all[1]]
            P.ps_list = ps_all[2:8]
            hgi = 0
            yi = 0

            def prefetch(i):
                wi = i % 2
                k._hazards("pool", [eid_i], [])
                nc.gpsimd.reg_load(reg, eid_i.ap[0:1, i:i + 1])
                ev = nc.gpsimd.snap(reg, min_val=0, max_val=N_EXP - 1)
                k.dma_raw("pool", lambda e: e.dma_start(out=wgs[wi].ap[:], in_=wg_d[bass.ds(ev, 1), :, :].rearrange(
                    "e (c p) n -> p (e c) n", p=128)), reads=[eid_i], writes=[wgs[wi]], sem="swg%d" % wi)
                k.dma_raw("pool", lambda e: e.dma_start(out=wus[wi].ap[:], in_=wu_d[bass.ds(ev, 1), :, :].rearrange(
                    "e (c p) n -> p (e c) n", p=128)), reads=[eid_i], writes=[wus[wi]], sem="swu%d" % wi)
                k.dma_raw("pool", lambda e: e.dma_start(out=wds[wi].ap[:], in_=wd_d[bass.ds(ev, 1), :, :].rearrange(
                    "e (c p) n -> p (e c) n", p=128)), reads=[eid_i], writes=[wds[wi]], sem="swd%d" % wi)
                it = idt[wi]
                k.dma(it.ap[:], IDX.ap[i * 512:(i + 1) * 512, :].rearrange("(s p) c -> p s c", p=128), reads=[IDX], writes=[it],
                      sem="sidt%d" % wi)

            def gather(i):
                nonlocal hgi
                it = idt[i % 2]
                res = []
                for s4 in range(4):
                    g = hg[hgi % 4]
                    hgi += 1
                    k.dma_raw("pool", lambda e, g=g, s4=s4: e.indirect_dma_start(
                        out=g.ap[:], out_offset=None, in_=HT.ap[:, :],
                        in_offset=bass.IndirectOffsetOnAxis(ap=it.ap[:, s4, 0:1], axis=0)),
                        reads=[it, HT], writes=[g], sem="shg%d" % ((hgi - 1) % 4))
                    res.append(g)
                return res

            prefetch(0)
            for i in range(NT_TILES):
                wi = i % 2
                gs = gather(i)
                if i + 1 < NT_TILES:
                    prefetch(i + 1)
                Wg, Wu, Wd = wgs[wi], wus[wi], wds[wi]
                ht = hT[i % 2]
                for c in range(8):
                    tb = tr_banks[c % 2]
                    tbv = tb.ap[:].bitcast(BF16)
                    for s4 in range(4):
                        k.op("pe", lambda e, c=c, s4=s4, tbv=tbv: e.transpose(out=tbv[:, s4 * 128:(s4 + 1) * 128],
                                                                             in_=gs[s4].ap[:, c * 128:(c + 1) * 128],
                                                                             identity=identb.ap[:]),
                             reads=[gs[s4], identb], writes=[tb], signal=(s4 == 3))
                    if c % 2 == 0:
                        _copy(k, "act", ht.ap[:, c, :], tbv[:, 0:512], [tb], [ht])
                    else:
                        _copy(k, "dve", ht.ap[:, c, :], tbv[:, 0:512], [tb], [ht])
                hb = he[i % 2]
                for f in range(4):
                    pg = next_ps(P)
                    pu = next_ps(P)
                    for c in range(8):
                        _mm(k, pg, pg.ap[:], Wg.ap[:, c, f * 128:(f + 1) * 128], ht.ap[:, c, :], c == 0, c == 7, [Wg, ht], c == 7)
                    for c in range(8):
                        _mm(k, pu, pu.ap[:], Wu.ap[:, c, f * 128:(f + 1) * 128], ht.ap[:, c, :], c == 0, c == 7, [Wu, ht], c == 7)
                    a = asb[f % 2]
                    _act(k, a.ap[:], pg.ap[:], AF.Silu, [pg], [a])
                    _tt(k, "dve", hb.ap[:, f, :], pu.ap[:], a.ap[:], ALU.mult, [pu, a], [hb])
                it = idt[wi]
                for s4 in range(4):
                    y = ysl[yi % 2]
                    yi += 1
                    for dh in range(2):
                        pd = next_ps(P)
                        for f in range(4):
                            _mm(k, pd, pd.ap[:], hb.ap[:, f, s4 * 128:(s4 + 1) * 128], Wd.ap[:, f, dh * 512:(dh + 1) * 512], f == 0,
                                f == 3, [Wd, hb], f == 3)
                        if dh == 0:
                            _copy(k, "act", y.ap[:, 0:512], pd.ap[:], [pd], [y])
                        else:
                            _copy(k, "dve", y.ap[:, 512:1024], pd.ap[:], [pd], [y])
                    k.dma_raw("pool", lambda e, y=y, s4=s4, it=it: e.indirect_dma_start(
                        out=Y2.ap[:, :], out_offset=bass.IndirectOffsetOnAxis(ap=it.ap[:, s4, 1:2], axis=0),
                        in_=y.ap[:], in_offset=None, bounds_check=2 * S - 1, oob_is_err=False),
                        reads=[y, it], wfree=[Y2], sem="st_Y2_%d" % L)
            k.barrier()
        with ExitStack() as es5:
            xt = [P.sb(es5, "dx%d" % i, [128, 8, 512]) for i in range(2)]
            y0 = [P.sb(es5, "dy0%d" % i, [128, D]) for i in range(2)]
            y1 = [P.sb(es5, "dy1%d" % i, [128, D]) for i in range(2)]
            sq = P.sb(es5, "dsq", [128, 8, 512])
            rstd = P.sb(es5, "drstd", [128, 512])
            if final:
                fg = P.sb(es5, "dfg", [128, 8])
                k.dma(fg.ap[:], fg_d[:, :], writes=[fg], sem="misc")
            P.ps_list = ps_all
            for tt in range(8):
                xb = xt[tt % 2]
                k.dma(xb.ap[:], Xin.ap[:, tt * 512:(tt + 1) * 512].rearrange("(c p) t -> p c t", p=128), reads=[Xin],
                      writes=[xb], sem="dx%d" % (tt % 2))
                for s4 in range(4):
                    cg = tt * 4 + s4
                    a0, a1 = y0[cg % 2], y1[cg % 2]
                    k.dma(a0.ap[:], Y2.ap[cg * 128:(cg + 1) * 128, :], reads=[Y2], writes=[a0], sem="dy0%d" % (cg % 2))
                    k.dma(a1.ap[:], Y2.ap[S + cg * 128:S + (cg + 1) * 128, :], reads=[Y2], writes=[a1], sem="dy1%d" % (cg % 2))
                    _ts(k, "dve", a0.ap[:], a0.ap[:], wts.ap[:, cg, 0:1], None, ALU.mult, None, [a0, wts], [a0])
                    _stt(k, "dve", a0.ap[:], a1.ap[:], wts.ap[:, cg, 1:2], a0.ap[:], ALU.mult, ALU.add, [a1, wts, a0], [a0])
                    pa, pb = next_ps(P), next_ps(P)
                    for c in range(8):
                        pp = pa if c < 4 else pb
                        k.op("pe", lambda e, c=c, pp=pp, a0=a0: e.transpose(out=pp.ap[:, (c % 4) * 128:(c % 4 + 1) * 128],
                                                                            in_=a0.ap[:, c * 128:(c + 1) * 128],
                                                                            identity=ident.ap[:]),
                             reads=[a0, ident], writes=[pp], signal=(c % 4 == 3))
                    for c in range(8):
                        pp = pa if c < 4 else pb
                        _stt(k, "dve", xb.ap[:, c, s4 * 128:(s4 + 1) * 128], pp.ap[:, (c % 4) * 128:(c % 4 + 1) * 128],
                             mv.ap[:, L, 5, c:c + 1], xb.ap[:, c, s4 * 128:(s4 + 1) * 128], ALU.mult, ALU.add, [pp, mv, xb], [xb])
                if final:
                    rms_rstd(P, xb.ap[:], xb, 512, sq, rstd)
                    _tt(k, "dve", sq.ap[:], xb.ap[:], rstd.ap[:].unsqueeze(1).to_broadcast([128, 8, 512]), ALU.mult, [xb, rstd], [sq])
                    for c in range(8):
                        _act(k, xb.ap[:, c, :], sq.ap[:, c, :], AF.Identity, [sq, fg], [xb], scale=fg.ap[:, c:c + 1])
                k.dma(Xout.ap[:, tt * 512:(tt + 1) * 512].rearrange("(c p) t -> p c t", p=128), xb.ap[:], reads=[xb], wfree=[Xout],
                      sem="st_" + Xout.name)
            k.barrier()
        P.ps_list = None
```

```python
import math
from contextlib import ExitStack

import numpy as np
import ml_dtypes

import concourse.bass as bass
import concourse.mybir as mybir
from concourse.bass_utils import run_bass_kernel_spmd

F32 = mybir.dt.float32
BF16 = mybir.dt.bfloat16
I32 = mybir.dt.int32
AF = mybir.ActivationFunctionType
ALU = mybir.AluOpType
AX = mybir.AxisListType

D = 1024
S = 4096
CTX = 256
LK = S + CTX
NCH = D // 128
EPS = 1e-6
N_EXP = 32
D_EXP = 512
N2 = 2 * S


class Tok:
    __slots__ = ("sem", "val", "eng", "dsem", "epoch")
    EPOCH = 0

    def __init__(self, sem, val, eng, dsem=None):
        self.sem = sem
        self.val = val
        self.eng = eng
        self.dsem = dsem
        self.epoch = Tok.EPOCH


class Buf:
    __slots__ = ("ap", "w", "r", "name")

    def __init__(self, ap, name=""):
        self.ap = ap
        self.w = None
        self.r = {}
        self.name = name

    def __getitem__(self, key):
        return self.ap[key]


class DSem:
    def __init__(self, sem):
        self.sem = sem
        self.count = 0
        self.persistent = False


class K:
    def __init__(self, nc):
        self.nc = nc
        self.E = {"pe": nc.tensor, "act": nc.scalar, "dve": nc.vector, "pool": nc.gpsimd, "sp": nc.sync}
        self.sem = {}
        self.cnt = {}
        for e in ("pe", "act", "dve", "pool"):
            self.sem[e] = nc.alloc_semaphore("s_" + e)
            self.cnt[e] = 0
        self.seen = {}
        self.dsems = {}
        self.free_dsems = []
        Tok.EPOCH = 0
        self.pe_pending = None
        self.n_instr = 0
        self.old_last = {}

    def dsem(self, name, persistent=False):
        if name not in self.dsems:
            if self.free_dsems and not persistent:
                self.dsems[name] = self.free_dsems.pop()
            else:
                self.dsems[name] = DSem(self.nc.alloc_semaphore("d_" + name.replace("#", "_")))
                self.dsems[name].persistent = persistent
        return self.dsems[name]

    def _wait(self, eng, tok):
        if tok is None:
            return
        if tok.epoch < Tok.EPOCH and not (tok.dsem is not None and tok.dsem.persistent):
            return
        if tok.eng == "pe" and eng == "pe":
            return
        if tok.val is None:
            raise RuntimeError("waiting on an un-signalled PE group")
        val = tok.val
        if tok.dsem is not None:
            val = tok.dsem.count
        key = (eng, tok.sem.num)
        if self.seen.get(key, 0) >= val:
            return
        self.E[eng].wait_ge(tok.sem, val)
        self.seen[key] = val

    def _hazards(self, eng, reads, writes):
        for b in reads:
            self._wait(eng, b.w)
        for b in writes:
            self._wait(eng, b.w)
            for t in b.r.values():
                self._wait(eng, t)

    def _update(self, tok, reads, writes):
        key = tok.eng if tok.dsem is None else ("d", tok.sem.num)
        for b in reads:
            b.r[key] = tok
        for b in writes:
            b.w = tok
            b.r = {}

    def op(self, eng, fn, reads=(), writes=(), signal=True):
        self._hazards(eng, reads, writes)
        ins = fn(self.E[eng])
        self.n_instr += 1
        if eng == "pe" and not signal:
            if self.pe_pending is None:
                self.pe_pending = Tok(self.sem["pe"], None, "pe")
            tok = self.pe_pending
        else:
            if self.cnt[eng] >= 15000:
                self.old_last[eng] = Tok(self.sem[eng], self.cnt[eng], eng)
                self.sem[eng] = self.nc.alloc_semaphore("s_%s_%d" % (eng, self.n_instr))
                self.cnt[eng] = 0
            self.cnt[eng] += 1
            ins.then_inc(self.sem[eng], 1)
            if eng == "pe" and self.pe_pending is not None:
                self.pe_pending.val = self.cnt[eng]
                tok = self.pe_pending
                self.pe_pending = None
            else:
                tok = Tok(self.sem[eng], self.cnt[eng], eng)
        self._update(tok, reads, writes)
        return tok

    def dma(self, out, in_, reads=(), writes=(), sem="ld", q="sp", wfree=(), rot=None):
        if rot is not None:
            sem = "%s#%d" % (sem, rot)
        ds = self.dsem(sem) if isinstance(sem, str) else sem
        self._hazards(q, reads, writes)
        for b in wfree:
            for t in b.r.values():
                self._wait(q, t)
        writes = list(writes) + list(wfree)
        ins = self.E[q].dma_start(out=out, in_=in_)
        ins.then_inc(ds.sem, 16)
        ds.count += 16
        self.n_instr += 1
        tok = Tok(ds.sem, ds.count, "dma", ds)
        self._update(tok, reads, writes)
        return tok

    def dma_raw(self, q, fn, reads=(), writes=(), wfree=(), sem="ld", rot=None):
        if rot is not None:
            sem = "%s#%d" % (sem, rot)
        ds = self.dsem(sem) if isinstance(sem, str) else sem
        self._hazards(q, reads, writes)
        for b in wfree:
            for t in b.r.values():
                self._wait(q, t)
        writes = list(writes) + list(wfree)
        ins = fn(self.E[q])
        ins.then_inc(ds.sem, 16)
        ds.count += 16
        self.n_instr += 1
        tok = Tok(ds.sem, ds.count, "dma", ds)
        self._update(tok, reads, writes)
        return tok

    def barrier(self):
        toks = [Tok(self.sem[e], self.cnt[e], e) if self.cnt[e] > 0 else self.old_last[e]
                for e in ("pe", "act", "dve", "pool") if self.cnt[e] > 0 or e in self.old_last]
        dtoks = [Tok(d.sem, d.count, "dma", d) for d in self.dsems.values() if d.count > 0 and not d.persistent]
        if self.pe_pending is not None:
            raise RuntimeError("barrier with un-signalled PE group")
        for e in ("pe", "act", "dve", "pool", "sp"):
            for t in toks:
                if t.eng != e:
                    self._wait(e, t)
            for t in dtoks:
                self._wait(e, t)
        keep = {}
        for name, d in self.dsems.items():
            if d.persistent:
                keep[name] = d
            elif d.count < 20000:
                self.free_dsems.append(d)
        self.dsems = keep
        Tok.EPOCH += 1


class Prog:
    def __init__(self, dbg=(), feed=()):
        self.nc = bass.Bass("TRN2", target_bir_lowering=False)
        self.k = K(self.nc)
        self.dbg = set(dbg)
        self.feed = set(feed)
        self.inputs = {}
        self.outputs = []
        self.es = ExitStack()
        nc = self.nc
        self.psum = [Buf(self.es.enter_context(nc.psum_tensor("ps%d" % i, [128, 512], F32)), "ps%d" % i)
                     for i in range(8)]

    def inp(self, name, shape, dtype=F32):
        t = self.nc.dram_tensor(name, list(shape), dtype, kind="ExternalInput").ap()
        self.inputs[name] = (tuple(shape), dtype)
        return t

    def out(self, name, shape, dtype=F32):
        t = self.nc.dram_tensor(name, list(shape), dtype, kind="ExternalOutput").ap()
        self.outputs.append(name)
        return t

    def scratch(self, name, shape, dtype=F32):
        if name in self.feed:
            return Buf(self.inp(name, shape, dtype), name)
        if name in self.dbg:
            return Buf(self.out(name, shape, dtype), name)
        return Buf(self.nc.dram_tensor(name, list(shape), dtype).ap(), name)

    def sb(self, es, name, shape, dtype=F32):
        self._uid = getattr(self, "_uid", 0) + 1
        return Buf(es.enter_context(self.nc.sbuf_tensor("%s_%d" % (name, self._uid), list(shape), dtype)), name)


def _mm(k, ps, out_ap, lhsT, rhs, start, stop, reads, signal):
    return k.op("pe", lambda e: e.matmul(out_ap, lhsT, rhs, start=start, stop=stop),
                reads=reads, writes=[ps], signal=signal)


def _act(k, out_ap, in_ap, func, reads, writes, bias=None, scale=None):
    kw = {}
    if bias is not None:
        kw["bias"] = bias
    if scale is not None:
        kw["scale"] = scale
    return k.op("act", lambda e: e.activation(out=out_ap, in_=in_ap, func=func, **kw), reads=reads, writes=writes)


def _tt(k, eng, out_ap, a, b, op, reads, writes):
    return k.op(eng, lambda e: e.tensor_tensor(out=out_ap, in0=a, in1=b, op=op), reads=reads, writes=writes)


def _ts(k, eng, out_ap, a, s1, s2, op0, op1, reads, writes):
    if op1 is None:
        return k.op(eng, lambda e: e.tensor_scalar(out=out_ap, in0=a, scalar1=s1, scalar2=None, op0=op0),
                    reads=reads, writes=writes)
    return k.op(eng, lambda e: e.tensor_scalar(out=out_ap, in0=a, scalar1=s1, scalar2=s2, op0=op0, op1=op1),
                reads=reads, writes=writes)


def _stt(k, eng, out_ap, a, s, b, op0, op1, reads, writes):
    return k.op(eng, lambda e: e.scalar_tensor_tensor(out=out_ap, in0=a, scalar=s, in1=b, op0=op0, op1=op1),
                reads=reads, writes=writes)


def _copy(k, eng, out_ap, in_ap, reads, writes):
    if eng == "act":
        return k.op("act", lambda e: e.copy(out=out_ap, in_=in_ap), reads=reads, writes=writes)
    return k.op(eng, lambda e: e.tensor_copy(out=out_ap, in_=in_ap), reads=reads, writes=writes)


def phase_adaln(P):
    k, nc = P.k, P.nc
    ada_w = P.inp("ada_w", [2, D, 6 * D])
    ada_b = P.inp("ada_bT", [128, 2, 48])
    ccol_d = P.inp("ccol", [128, 8, 2])
    n1g_d = P.inp("norm1_gT", [128, 2, 8])
    n2g_d = P.inp("norm2_gT", [128, 2, 8])
    P.mv = P.sb(P.es, "mv", [128, 2, 8, 8])
    mv = P.mv
    with ExitStack() as es:
        ccol = P.sb(es, "ccol_s", [128, 8, 2])
        silc = P.sb(es, "silc", [128, 8, 2])
        ab = P.sb(es, "adab", [128, 2, 48])
        ng = P.sb(es, "ng", [128, 2, 2, 8])
        acc = P.sb(es, "adacc", [128, 48, 2])
        wb = [P.sb(es, "adaw%d" % i, [128, 6 * D]) for i in range(2)]
        k.dma(ccol.ap[:], ccol_d[:, :, :], writes=[ccol], sem="misc")
        k.dma(ab.ap[:], ada_b[:, :, :], writes=[ab], sem="misc")
        k.dma(ng.ap[:, 0], n1g_d[:, :, :], writes=[ng], sem="misc")
        k.dma(ng.ap[:, 1], n2g_d[:, :, :], writes=[ng], sem="misc")
        _act(k, silc.ap[:], ccol.ap[:], AF.Silu, [ccol], [silc])
        it = 0
        for layer in range(2):
            for kc in range(8):
                w = wb[it % 2]
                k.dma(w.ap[:], ada_w[layer, kc * 128:(kc + 1) * 128, :], writes=[w], sem="adaw%d" % (it % 2))
                ps = P.psum[it % 2]
                for n in range(48):
                    _mm(k, ps, ps.ap[:, 2 * n:2 * n + 2], w.ap[:, n * 128:(n + 1) * 128], silc.ap[:, kc, :],
                        True, True, [w, silc], n == 47)
                pv = ps.ap[:, 0:96].rearrange("p (n c) -> p n c", c=2)
                if kc == 0:
                    _tt(k, "dve", acc.ap[:], pv, ab.ap[:, layer, :].unsqueeze(2).to_broadcast([128, 48, 2]), ALU.add,
                        [ps, ab], [acc])
                else:
                    _tt(k, "dve", acc.ap[:], pv, acc.ap[:], ALU.add, [ps, acc], [acc])
                it += 1
            def m(j, col):
                return acc.ap[:, j * 8:(j + 1) * 8, col]
            _stt(k, "dve", mv.ap[:, layer, 0, :], m(1, 0), 1.0, ng.ap[:, 0, layer, :], ALU.add, ALU.mult, [acc, ng], [mv])
            _copy(k, "dve", mv.ap[:, layer, 1, :], m(0, 0), [acc], [mv])
            _copy(k, "dve", mv.ap[:, layer, 2, :], m(2, 0), [acc], [mv])
            _stt(k, "dve", mv.ap[:, layer, 3, :], m(4, 0), 1.0, ng.ap[:, 1, layer, :], ALU.add, ALU.mult, [acc, ng], [mv])
            _copy(k, "dve", mv.ap[:, layer, 4, :], m(3, 0), [acc], [mv])
            _copy(k, "dve", mv.ap[:, layer, 5, :], m(5, 0), [acc], [mv])
            _stt(k, "dve", mv.ap[:, layer, 6, :], m(1, 1), 1.0, ng.ap[:, 0, layer, :], ALU.add, ALU.mult, [acc, ng], [mv])
            _copy(k, "dve", mv.ap[:, layer, 7, :], m(0, 1), [acc], [mv])
        if "mv" in P.dbg:
            o = P.out("mv_o", [128, 2, 8, 8])
            k.dma(o[:, :, :, :], mv.ap[:], reads=[mv], sem="misc")
        k.barrier()


def rope_tables():
    p = np.arange(128)
    j = p % 64
    blk = j // 32
    r = j % 32
    i = r % 16
    inv = (10000.0 ** (-(np.arange(0, 32, 2, dtype=np.float32)) / np.float32(32))).astype(np.float32)
    tok = np.arange(S)
    row = (tok // 64).astype(np.float32)
    col = (tok % 64).astype(np.float32)
    pos = np.where(blk[:, None] == 0, row[None, :], col[None, :]).astype(np.float32)
    ang = (pos * inv[i][:, None]).astype(np.float32)
    cos = np.cos(ang).astype(np.float32)
    sin = np.sin(ang).astype(np.float32)
    sgn = np.where(r < 16, -1.0, 1.0).astype(np.float32)
    rt = np.zeros((128, 128), np.float32)
    for m in range(128):
        partner = m + 16 if (m % 32) < 16 else m - 16
        rt[partner, m] = 1.0
    return cos, (sin * sgn[:, None]).astype(np.float32), rt


def setup_consts(P):
    k = P.k
    P.ones_f = P.sb(P.es, "ones_f", [128, 128])
    k.op("dve", lambda e: e.memset(P.ones_f.ap[:], 1.0), writes=[P.ones_f])
    P.ones_b = P.sb(P.es, "ones_b", [128, 128], BF16)
    k.op("dve", lambda e: e.memset(P.ones_b.ap[:], 1.0), writes=[P.ones_b])
    P._psi = 0
    P.ident_in = P.inp("ident", [128, 128])


def next_ps(P):
    lst = getattr(P, "ps_list", None) or P.psum
    b = lst[P._psi % len(lst)]
    P._psi += 1
    return b


def phase_front0(P):
    k, nc = P.k, P.nc
    xT = P.inp("xT", [D, S])
    P.xT_d = xT
    ctxT = P.inp("ctxT", [D, CTX])
    w_in = P.inp("w_in0", [D, 3 * D])
    cos_d = P.inp("rope_cos", [128, S])
    sin_d = P.inp("rope_sin", [128, S])
    rt_d = P.inp("rope_rt", [128, 128])
    P.QT = P.scratch("QT", [512, S], BF16)
    P.KT = P.scratch("KT", [512, LK], BF16)
    P.V = P.scratch("V", [LK, 512], BF16)
    P.U = P.scratch("U", [1536, S], F32)
    mv = P.mv
    xTv = xT.rearrange("(c p) t -> p c t", p=128)
    ctxv = ctxT.rearrange("(c p) t -> p c t", p=128)
    with ExitStack() as es:
        W = P.sb(es, "w_in_s", [128, 8, 3 * D], BF16)
        cos = P.sb(es, "cos_s", [128, S])
        sin = P.sb(es, "sin_s", [128, S])
        rt = P.sb(es, "rt_s", [128, 128])
        xt = [P.sb(es, "xt%d" % i, [128, 8, 512]) for i in range(2)]
        sq = P.sb(es, "sq", [128, 8, 512])
        rstd = P.sb(es, "rstd", [128, 512])
        hx = [P.sb(es, "hx%d" % i, [128, 8, 512], BF16) for i in range(2)]
        qf = [P.sb(es, "qf%d" % i, [128, 512]) for i in range(2)]
        t1 = [P.sb(es, "t1_%d" % i, [128, 512]) for i in range(2)]
        qb = [P.sb(es, "qb%d" % i, [128, 512], BF16) for i in range(3)]
        uf = [P.sb(es, "uf%d" % i, [128, 512]) for i in range(3)]
        vb = [P.sb(es, "vb%d" % i, [128, 512], BF16) for i in range(2)]
        w_v = w_in.rearrange("(c p) n -> p c n", p=128)
        for c in range(8):
            k.dma(W.ap[:, c, :], w_v[:, c, :], writes=[], wfree=[W], sem="w_in", q="pool")
        k.dma(cos.ap[:], cos_d[:, :], writes=[cos], sem="misc")
        k.dma(sin.ap[:], sin_d[:, :], writes=[sin], sem="misc")
        k.dma(rt.ap[:], rt_d[:, :], writes=[rt], sem="misc")
        cnt = {"q": 0, "u": 0, "v": 0, "f": 0}
        tiles = [("ctx", 0, CTX)] + [("x", t * 512, 512) for t in range(8)]
        def load(i):
            kind, t0, T = tiles[i]
            buf = xt[i % 2]
            src = ctxv[:, :, 0:T] if kind == "ctx" else xTv[:, :, t0:t0 + T]
            k.dma(buf.ap[:, :, 0:T], src, writes=[buf], sem="xt%d" % (i % 2))
        def norm0(i):
            kind, t0, T = tiles[i]
            xb = xt[i % 2]
            h = hx[i % 2]
            ia, ib = (6, 7) if kind == "ctx" else (0, 1)
            _act(k, sq.ap[:, :, 0:T], xb.ap[:, :, 0:T], AF.Square, [xb], [sq])
            ps = next_ps(P)
            for c in range(8):
                _mm(k, ps, ps.ap[:, 0:T], P.ones_f.ap[:], sq.ap[:, c, 0:T], c == 0, c == 7, [P.ones_f, sq], c == 7)
            _act(k, rstd.ap[:, 0:T], ps.ap[:, 0:T], AF.Sqrt, [ps], [rstd], bias=EPS, scale=1.0 / D)
            k.op("dve", lambda e: e.reciprocal(out=rstd.ap[:, 0:T], in_=rstd.ap[:, 0:T]), reads=[rstd], writes=[rstd])
            _tt(k, "dve", sq.ap[:, :, 0:T], xb.ap[:, :, 0:T], rstd.ap[:, 0:T].unsqueeze(1).to_broadcast([128, 8, T]),
                ALU.mult, [xb, rstd], [sq])
            for c in range(8):
                _act(k, h.ap[:, c, 0:T], sq.ap[:, c, 0:T], AF.Identity, [sq, mv], [h],
                     bias=mv.ap[:, 0, ib, c:c + 1], scale=mv.ap[:, 0, ia, c:c + 1])

        load(0)
        load(1)
        norm0(0)
        for i, (kind, t0, T) in enumerate(tiles):
            if i + 1 < len(tiles):
                norm0(i + 1)
            if i + 2 < len(tiles):
                load(i + 2)
            h = hx[i % 2]
            nlist = list(range(4, 8)) if kind == "ctx" else list(range(0, 8)) + list(range(12, 24))
            for n in nlist:
                ps = next_ps(P)
                for c in range(8):
                    _mm(k, ps, ps.ap[:, 0:T], W.ap[:, c, n * 128:(n + 1) * 128], h.ap[:, c, 0:T], c == 0, c == 7,
                        [W, h], c == 7)
                if n >= 12:
                    u = uf[cnt["u"] % 3]
                    cnt["u"] += 1
                    _copy(k, "act", u.ap[:, 0:T], ps.ap[:, 0:T], [ps], [u])
                    k.dma(P.U.ap[(n - 12) * 128:(n - 11) * 128, t0:t0 + T], u.ap[:, 0:T], reads=[u], wfree=[P.U], sem="st_U", rot=(cnt["u"] - 1) % 3, q="pool")
                elif kind == "ctx":
                    q = qb[cnt["q"] % 3]
                    cnt["q"] += 1
                    _copy(k, "act", q.ap[:, 0:T], ps.ap[:, 0:T], [ps], [q])
                    k.dma(P.KT.ap[(n - 4) * 128:(n - 3) * 128, 0:T], q.ap[:, 0:T], reads=[q], wfree=[P.KT], sem="st_Q", rot=(cnt["q"] - 1) % 3, q="pool")
                else:
                    f = qf[cnt["f"] % 2]
                    tt1 = t1[cnt["f"] % 2]
                    cnt["f"] += 1
                    q = qb[cnt["q"] % 3]
                    cnt["q"] += 1
                    _copy(k, "act", f.ap[:], ps.ap[:], [ps], [f])
                    ps2 = next_ps(P)
                    _mm(k, ps2, ps2.ap[:], rt.ap[:], f.ap[:], True, True, [rt, f], True)
                    _tt(k, "dve", tt1.ap[:], f.ap[:], cos.ap[:, t0:t0 + T], ALU.mult, [f, cos], [tt1])
                    _tt(k, "dve", f.ap[:], ps2.ap[:], sin.ap[:, t0:t0 + T], ALU.mult, [ps2, sin], [f])
                    _tt(k, "dve", q.ap[:], tt1.ap[:], f.ap[:], ALU.add, [tt1, f], [q])
                    if n < 4:
                        k.dma(P.QT.ap[n * 128:(n + 1) * 128, t0:t0 + T], q.ap[:], reads=[q], wfree=[P.QT], sem="st_Q", rot=(cnt["q"] - 1) % 3, q="pool")
                    else:
                        k.dma(P.KT.ap[(n - 4) * 128:(n - 3) * 128, CTX + t0:CTX + t0 + T], q.ap[:], reads=[q],
                              wfree=[P.KT], sem="st_Q", rot=(cnt["q"] - 1) % 3, q="pool")
            for s4 in range(T // 128):
                ps = next_ps(P)
                for c in range(8):
                    _mm(k, ps, ps.ap[:], h.ap[:, c, s4 * 128:(s4 + 1) * 128], W.ap[:, c, 1024:1536], c == 0, c == 7,
                        [W, h], c == 7)
                v = vb[cnt["v"] % 2]
                cnt["v"] += 1
                _copy(k, "dve", v.ap[:], ps.ap[:], [ps], [v])
                r0 = (0 if kind == "ctx" else CTX + t0) + s4 * 128
                k.dma(P.V.ap[r0:r0 + 128, :], v.ap[:], reads=[v], wfree=[P.V], sem="st_V", rot=(cnt["v"] - 1) % 2, q="pool")
        k.barrier()


LAM_INIT0 = 0.8 - 0.6 * math.exp(-0.3 * 0)


def phase_attn(P):
    k, nc = P.k, P.nc
    lam_d = P.inp("lamv", [4, 64])
    sg_d = P.inp("subln_gT", [128, 1])
    if not hasattr(P, "OAB"):
        P.OAB = P.scratch("OAB", [D, S], BF16)
    with ExitStack() as es:
        lamt = P.sb(es, "lamt", [128, 4, 64])
        lw = P.sb(es, "lamw", [128, 8])
        sg = P.sb(es, "sublng", [128, 1])
        KTs = [P.sb(es, "KTs%d" % i, [128, LK], BF16) for i in range(2)]
        QTs = [P.sb(es, "QTs%d" % i, [128, S], BF16) for i in range(2)]
        Vs = [P.sb(es, "Vs%d" % i, [128, LK // 128, 128], BF16) for i in range(2)]
        eT = [P.sb(es, "eT%d" % i, [128, 512], BF16) for i in range(4)]
        r = [P.sb(es, "rs%d" % i, [128, 512]) for i in range(2)]
        tA = P.sb(es, "tA", [128, 512])
        tB = P.sb(es, "tB", [128, 512])
        tC = P.sb(es, "tC", [128, 512])
        ob = [P.sb(es, "ob%d" % i, [128, 512], BF16) for i in range(2)]
        esum = [[P.sb(es, "esum%d_%d" % (e_, i), [128, 512]) for i in range(2)] for e_ in range(2)]
        SUM_ENG = getattr(P, "attn_sum_eng", ("dve", "dve"))
        for i in range(4):
            k.dma(lamt.ap[:, i, :], lam_d[i:i + 1, :].partition_broadcast(128), writes=[], wfree=[lamt], sem="misc")
        k.dma(sg.ap[:], sg_d[:, :], writes=[sg], sem="misc")
        _tt(k, "dve", lamt.ap[:, 0, :], lamt.ap[:, 0, :], lamt.ap[:, 1, :], ALU.mult, [lamt], [lamt])
        _tt(k, "dve", lamt.ap[:, 2, :], lamt.ap[:, 2, :], lamt.ap[:, 3, :], ALU.mult, [lamt], [lamt])
        k.op("dve", lambda e: e.reduce_sum(out=lw.ap[:, 0:1], in_=lamt.ap[:, 0, :], axis=AX.X), reads=[lamt], writes=[lw])
        k.op("dve", lambda e: e.reduce_sum(out=lw.ap[:, 1:2], in_=lamt.ap[:, 2, :], axis=AX.X), reads=[lamt], writes=[lw])
        _act(k, lw.ap[:, 2:4], lw.ap[:, 0:2], AF.Exp, [lw], [lw])
        _tt(k, "dve", lw.ap[:, 4:5], lw.ap[:, 3:4], lw.ap[:, 2:3], ALU.subtract, [lw], [lw])
        _ts(k, "dve", lw.ap[:, 4:5], lw.ap[:, 4:5], -LAM_INIT0, None, ALU.add, None, [lw], [lw])
        _ts(k, "dve", lw.ap[:, 5:6], sg.ap[:, 0:1], 1.0 - LAM_INIT0, None, ALU.mult, None, [sg], [lw])
        neglam = lw.ap[:, 4:5]
        gsc = lw.ap[:, 5:6]

        sc_banks = [P.psum[0], P.psum[1], P.psum[2], P.psum[3]]
        acc_o = [P.psum[4], P.psum[5]]
        acc_s = [P.psum[6], P.psum[7]]
        ms_b = P.psum[6]
        NKC = LK // 128

        def load_hp(hp):
            i = hp % 2
            k.dma(KTs[i].ap[:], P.KT.ap[hp * 128:(hp + 1) * 128, :], reads=[P.KT], writes=[KTs[i]], sem="ld_kt%d" % i)
            k.dma(QTs[i].ap[:], P.QT.ap[hp * 128:(hp + 1) * 128, :], reads=[P.QT], writes=[QTs[i]], sem="ld_qt%d" % i)
            k.dma(Vs[i].ap[:], P.V.ap[:, hp * 128:(hp + 1) * 128].rearrange("(c p) v -> p c v", p=128),
                  reads=[P.V], writes=[Vs[i]], sem="ld_v%d" % i)

        pairs = [(hp, qt, kc) for hp in range(4) for qt in range(8) for kc in range(NKC)]
        PE_SUM_EVERY = getattr(P, "pe_sum_every", 5)
        PE_SUM_LAST = ((NKC - 2) // PE_SUM_EVERY) * PE_SUM_EVERY
        assert PE_SUM_LAST < NKC - 1

        def score(j):
            hp, qt, kc = pairs[j]
            Kt, Qt = KTs[hp % 2], QTs[hp % 2]
            for e in range(2):
                ps = sc_banks[(j % 2) * 2 + e]
                _mm(k, ps, ps.ap[:], Kt.ap[e * 64:(e + 1) * 64, kc * 128:(kc + 1) * 128],
                    Qt.ap[e * 64:(e + 1) * 64, qt * 512:(qt + 1) * 512], True, True, [Kt, Qt], True)

        load_hp(0)
        score(0)
        for j, (hp, qt, kc) in enumerate(pairs):
            if qt == 0 and kc == 0 and hp + 1 < 4:
                load_hp(hp + 1)
            ets = []
            for e in range(2):
                ps = sc_banks[(j % 2) * 2 + e]
                et = eT[(j % 2) * 2 + e]
                etok = _act(k, et.ap[:], ps.ap[:], AF.Exp, [ps], [et], scale=0.125)
                ets.append(et)
            if j % 8 == 4:
                moe_precast_emit(P, 1, after=etok)
            if j + 1 < len(pairs):
                score(j + 1)
            Vt = Vs[hp % 2]
            for e in range(2):
                et = ets[e]
                _mm(k, acc_o[e], acc_o[e].ap[:], Vt.ap[:, kc, :], et.ap[:], kc == 0, kc == NKC - 1, [Vt, et], True)
                es_ = esum[e][qt % 2]
                seng = SUM_ENG[e]
                if kc % PE_SUM_EVERY == 0 and kc <= PE_SUM_LAST:
                    _mm(k, acc_s[e], acc_s[e].ap[:], P.ones_b.ap[:], et.ap[:], kc == 0, kc == PE_SUM_LAST, [P.ones_b, et], True)
                elif kc == 1:
                    _copy(k, seng, es_.ap[:], et.ap[:], [et], [es_])
                else:
                    _tt(k, seng, es_.ap[:], es_.ap[:], et.ap[:], ALU.add, [es_, et], [es_])
                if kc == NKC - 1:
                    _stt(k, "dve", es_.ap[:], acc_s[e].ap[:], 1.0 / 128.0, es_.ap[:], ALU.mult, ALU.add, [acc_s[e], es_], [es_])
                    _mm(k, acc_s[e], acc_s[e].ap[:], P.ones_f.ap[:], es_.ap[:], True, True, [P.ones_f, es_], True)
            e = 1
            if e == 1 and kc == NKC - 1:
                for ee in range(2):
                    k.op("dve", lambda e_, ee=ee: e_.reciprocal(out=r[ee].ap[:], in_=acc_s[ee].ap[:]),
                         reads=[acc_s[ee]], writes=[r[ee]])
                _tt(k, "dve", tA.ap[:], acc_o[0].ap[:], r[0].ap[:], ALU.mult, [acc_o[0], r[0]], [tA])
                _tt(k, "dve", tB.ap[:], acc_o[1].ap[:], r[1].ap[:], ALU.mult, [acc_o[1], r[1]], [tB])
                _stt(k, "dve", tA.ap[:], tB.ap[:], neglam, tA.ap[:], ALU.mult, ALU.add, [tB, tA, lw], [tA])
                _act(k, tB.ap[:], tA.ap[:], AF.Square, [tA], [tB])
                _mm(k, ms_b, ms_b.ap[:], P.ones_f.ap[:], tB.ap[:], True, True, [P.ones_f, tB], True)
                _act(k, tC.ap[:], ms_b.ap[:], AF.Sqrt, [ms_b], [tC], bias=EPS, scale=1.0 / 128)
                k.op("dve", lambda e_: e_.reciprocal(out=tC.ap[:], in_=tC.ap[:]), reads=[tC], writes=[tC])
                o = ob[qt % 2]
                _stt(k, "dve", o.ap[:], tA.ap[:], gsc, tC.ap[:], ALU.mult, ALU.mult, [tA, tC, lw], [o])
                k.dma(P.OAB.ap[hp * 128:(hp + 1) * 128, qt * 512:(qt + 1) * 512], o.ap[:], reads=[o], wfree=[P.OAB],
                      sem="st_OAB", rot=qt % 2, q="pool")
        k.barrier()


HY_W = 512
HY_MIN_DECAY = math.log(1e-2) / 0.3
HY_MAX_DECAY = math.log(1e-2) / 1.5


def hyena_consts():
    n = S
    j = np.arange(n + 1, dtype=np.float64)
    t = j / (n - 1)
    bands = np.linspace(1e-4, 15.0, 16)
    ang = (2.0 * math.pi / n) * j[:, None] * bands[None, :]
    z = np.concatenate([t[:, None], np.cos(ang), -np.sin(ang)], axis=1)
    zpos = np.ascontiguousarray(z.T).astype(np.float32)
    delta = np.abs(np.linspace(HY_MIN_DECAY, HY_MAX_DECAY, HY_W)).astype(np.float32).reshape(1, HY_W)
    p = np.arange(128)[:, None]
    jc = np.arange(32)[None, :]
    jj = jc * 128 + p
    tf = -(jj / (n - 1.0))
    tb = -((jj + 1) / (n - 1.0))
    tb[jj == n - 1] = -1.0e4
    tcol = np.stack([tf, tb], axis=1).astype(np.float32)
    alpha = math.pi * (freq_order()[jj] + 0.5) / N2
    rot = np.stack([np.cos(alpha), np.sin(alpha)], axis=1).astype(np.float32)
    return zpos, delta, tcol, rot


HALF = S // 2


def freq_order():
    return np.concatenate([np.arange(HALF), S - 1 - np.arange(HALF)])


def dft_mats():
    fg = freq_order()
    m = np.outer((2 * np.arange(S) + 1), (2 * fg + 1)) % (4 * N2)
    ang = (2.0 * math.pi / (4 * N2)) * m
    cg = np.cos(ang).astype(np.float32).astype(ml_dtypes.bfloat16)
    sg = np.sin(ang).astype(np.float32).astype(ml_dtypes.bfloat16)
    fa = np.arange(HALF)
    nat = np.zeros((2, 2, HALF, HALF), ml_dtypes.bfloat16)
    ft = np.zeros((2, 2, HALF, HALF), ml_dtypes.bfloat16)
    for b in range(2):
        mb = np.outer(2 * fa + 1, 2 * (2 * fa + b) + 1) % (4 * N2)
        ab = (2.0 * math.pi / (4 * N2)) * mb
        for cs, fn in enumerate((np.cos, np.sin)):
            v = fn(ab).astype(np.float32).astype(ml_dtypes.bfloat16)
            nat[cs, b] = v
            ft[cs, b] = v.T
    return cg, sg, nat, ft


def _sin_reduced(k, out_ap, arg, tmp, reads_arg, writes_out):
    MAGIC = 12582912.0
    TWO_PI = 2.0 * math.pi
    a_ap, a_buf = arg
    t_ap, t_buf = tmp
    _ts(k, "dve", t_ap, a_ap, 1.0 / TWO_PI, MAGIC, ALU.mult, ALU.add, [a_buf], [t_buf])
    _ts(k, "dve", t_ap, t_ap, -MAGIC, -TWO_PI, ALU.add, ALU.mult, [t_buf], [t_buf])
    _tt(k, "dve", t_ap, t_ap, a_ap, ALU.add, [t_buf, a_buf], [t_buf])
    _act(k, out_ap, t_ap, AF.Sin, [t_buf], writes_out)


def dft_stream(P, es, n_groups=16):
    gw = S // n_groups
    P.dft_gw = gw
    P.dftC = [P.sb(es, "dftC%d" % i, [128, 32, gw], BF16) for i in range(2)]
    P.dftS = [P.sb(es, "dftS%d" % i, [128, 32, gw], BF16) for i in range(2)]
    P.dft_it = 0


def dft_load(P, g):
    k = P.k
    gw = P.dft_gw
    i = P.dft_it % 2
    P.dft_it += 1
    cv = P.dftC_d[:, g * gw:(g + 1) * gw].rearrange("(jc p) f -> p jc f", p=128)
    sv = P.dftS_d[:, g * gw:(g + 1) * gw].rearrange("(jc p) f -> p jc f", p=128)
    k.dma(P.dftC[i].ap[:], cv, writes=[P.dftC[i]], sem="dftC%d" % i)
    k.dma(P.dftS[i].ap[:], sv, writes=[P.dftS[i]], sem="dftS%d" % i)
    return P.dftC[i], P.dftS[i]


def phase_hy_filters(P):
    k, nc = P.k, P.nc
    zpos_d = P.inp("hy_zpos", [33, S + 1])
    delta_d = P.inp("hy_delta", [1, HY_W])
    tcol_d = P.inp("hy_tcol", [128, 2, 32])
    rot_d = P.inp("hy_rot", [128, 2, 32])
    w1_d = P.inp("hy_w1", [33, 64])
    w2_d = P.inp("hy_w2", [64, 64])
    w3_d = P.inp("hy_w3", [64, 2048])
    b3_d = P.inp("hy_b3", [1, 2048])
    vec_d = P.inp("hy_vec", [64, 4])
    bias_d = P.inp("hy_bias", [2, HY_W])
    P.dftC_d = P.inp("dft_cg", [S, S], BF16)
    P.dftS_d = P.inp("dft_sg", [S, S], BF16)
    P.KS = P.scratch("KS", [2, 2, S, HY_W], F32)
    NT = S + 1
    with ExitStack() as es:
        h2 = P.sb(es, "hyh2", [64, NT])
        es1 = ExitStack()
        zpos = P.sb(es1, "zpos", [33, NT])
        h1 = P.sb(es1, "hyh1", [64, NT])
        arg = P.sb(es1, "hyarg", [64, 512])
        tmp = P.sb(es1, "hytmp", [64, 512])
        w1 = P.sb(es1, "hyw1", [33, 64])
        w2 = P.sb(es1, "hyw2", [64, 64])
        vec = P.sb(es1, "hyvec", [64, 8])
        k.dma(zpos.ap[:], zpos_d[:, :], writes=[zpos], sem="misc")
        k.dma(w1.ap[:], w1_d[:, :], writes=[w1], sem="misc")
        k.dma(w2.ap[:], w2_d[:, :], writes=[w2], sem="misc")
        k.dma(vec.ap[:, 0:4], vec_d[:, :], writes=[vec], sem="misc")
        _tt(k, "dve", vec.ap[:, 4:5], vec.ap[:, 0:1], vec.ap[:, 1:2], ALU.mult, [vec], [vec])
        _tt(k, "dve", vec.ap[:, 5:6], vec.ap[:, 2:3], vec.ap[:, 3:4], ALU.mult, [vec], [vec])
        ntile = (NT + 511) // 512
        for layer, (wt, src, dst, kdim, fcol, bcol) in enumerate(((w1, zpos, h1, 33, 1, 4), (w2, h1, h2, 64, 3, 5))):
            for ti in range(ntile):
                c0 = ti * 512
                T = min(512, NT - c0)
                ps = next_ps(P)
                _mm(k, ps, ps.ap[0:64, 0:T], wt.ap[0:kdim, :], src.ap[0:kdim, c0:c0 + T], True, True, [wt, src], True)
                _ts(k, "dve", arg.ap[:, 0:T], ps.ap[0:64, 0:T], vec.ap[:, fcol:fcol + 1], vec.ap[:, bcol:bcol + 1],
                    ALU.mult, ALU.add, [ps, vec], [arg])
                _sin_reduced(k, dst.ap[:, c0:c0 + T], (arg.ap[:, 0:T], arg), (tmp.ap[:, 0:T], tmp), None, [dst])
        k.barrier()
        es1.close()
        w3 = P.sb(es, "hyw3", [64, 2048])
        b3 = P.sb(es, "hyb3", [128, 2048])
        delt = P.sb(es, "hydelta", [128, HY_W])
        tcol = P.sb(es, "hytcol", [128, 2, 32])
        rot = P.sb(es, "hyrot", [128, 4, 32])
        biasb = P.sb(es, "hybias", [128, 2, HY_W])
        inv = P.sb(es, "hyinv", [128, HY_W])
        A = P.sb(es, "hyA", [128, 32, HY_W], BF16)
        Bm = P.sb(es, "hyBm", [128, 32, HY_W], BF16)
        decs = [[P.sb(es, "hydec%d_%d" % (i, j), [128, HY_W]) for j in range(2)] for i in range(2)]
        hfs = [P.sb(es, "hyhf%d" % i, [128, HY_W]) for i in range(2)]
        hbs = [P.sb(es, "hyhb%d" % i, [128, HY_W]) for i in range(2)]
        ab = [P.sb(es, "hyab%d" % i, [128, HY_W], BF16) for i in range(2)]
        t1 = P.sb(es, "hyt1", [128, HY_W])
        t2 = P.sb(es, "hyt2", [128, HY_W])
        ko = [P.sb(es, "hyko%d" % i, [128, 2, HY_W]) for i in range(2)]
        dft_stream(P, es)
        k.dma(w3.ap[:], w3_d[:, :], writes=[w3], sem="misc")
        k.dma(b3.ap[:], b3_d[0:1, :].partition_broadcast(128), writes=[b3], sem="misc")
        k.dma(delt.ap[:], delta_d[0:1, :].partition_broadcast(128), writes=[delt], sem="misc")
        k.dma(tcol.ap[:], tcol_d[:, :, :], writes=[tcol], sem="misc")
        k.dma(rot.ap[:, 0:2, :], rot_d[:, :, :], writes=[rot], sem="misc")
        for o in range(2):
            k.dma(biasb.ap[:, o, :], bias_d[o:o + 1, :].partition_broadcast(128), writes=[], wfree=[biasb], sem="misc")
        _ts(k, "dve", rot.ap[:, 2, :], rot.ap[:, 1, :], -1.0, None, ALU.mult, None, [rot], [rot])
        _ts(k, "dve", biasb.ap[:], biasb.ap[:], 2.0 / N2, None, ALU.mult, None, [biasb], [biasb])
        for o in range(2):
            nb = next_ps(P)

            def tap_mm(jc):
                psf = next_ps(P)
                if psf is nb:
                    psf = next_ps(P)
                psb = next_ps(P)
                if psb is nb:
                    psb = next_ps(P)
                _mm(k, psf, psf.ap[:], h2.ap[:, jc * 128:jc * 128 + 128], w3.ap[:, o * 1024:o * 1024 + 512], True, True,
                    [h2, w3], True)
                _mm(k, psb, psb.ap[:], h2.ap[:, jc * 128 + 1:jc * 128 + 129], w3.ap[:, o * 1024 + 512:o * 1024 + 1024],
                    True, True, [h2, w3], True)
                return psf, psb

            nxt_mm = tap_mm(0)
            for jc in range(32):
                psf, psb = nxt_mm
                if jc + 1 < 32:
                    nxt_mm = tap_mm(jc + 1)
                d0, d1 = decs[jc % 2]
                f_, b_ = hfs[jc % 2], hbs[jc % 2]
                _act(k, d0.ap[:], delt.ap[:], AF.Exp, [delt, tcol], [d0], scale=tcol.ap[:, 0, jc:jc + 1])
                _act(k, d1.ap[:], delt.ap[:], AF.Exp, [delt, tcol], [d1], scale=tcol.ap[:, 1, jc:jc + 1])
                _tt(k, "dve", f_.ap[:], psf.ap[:], b3.ap[:, o * 1024:o * 1024 + 512], ALU.add, [psf, b3], [f_])
                _tt(k, "dve", b_.ap[:], psb.ap[:], b3.ap[:, o * 1024 + 512:o * 1024 + 1024], ALU.add, [psb, b3], [b_])
                _tt(k, "dve", f_.ap[:], f_.ap[:], d0.ap[:], ALU.mult, [f_, d0], [f_])
                _tt(k, "dve", b_.ap[:], b_.ap[:], d1.ap[:], ALU.mult, [b_, d1], [b_])
                _tt(k, "dve", A.ap[:, jc, :], f_.ap[:], b_.ap[:], ALU.add, [f_, b_], [A])
                _tt(k, "dve", Bm.ap[:, jc, :], b_.ap[:], f_.ap[:], ALU.subtract, [f_, b_], [Bm])
                a = ab[jc % 2]
                _stt(k, "dve", d0.ap[:], f_.ap[:], -1.0, f_.ap[:], ALU.mult, ALU.max, [f_], [d0])
                _stt(k, "dve", d1.ap[:], b_.ap[:], -1.0, b_.ap[:], ALU.mult, ALU.max, [b_], [d1])
                _tt(k, "dve", a.ap[:], d0.ap[:], d1.ap[:], ALU.add, [d0, d1], [a])
                _mm(k, nb, nb.ap[:], P.ones_b.ap[:], a.ap[:], jc == 0, jc == 31, [P.ones_b, a], True)
            k.op("dve", lambda e: e.reciprocal(out=inv.ap[:], in_=nb.ap[:]), reads=[nb], writes=[inv])
            _ts(k, "dve", inv.ap[:], inv.ap[:], 2.0 / N2, None, ALU.mult, None, [inv], [inv])
            ng = S // P.dft_gw
            cpg = P.dft_gw // 128
            nxt = dft_load(P, 0)
            for g in range(ng):
                Cb, Sb = nxt
                if g + 1 < ng:
                    nxt = dft_load(P, g + 1)
                for ci in range(cpg):
                    fc = g * cpg + ci
                    pc = next_ps(P)
                    pss = next_ps(P)
                    for jc in range(32):
                        _mm(k, pc, pc.ap[:], Cb.ap[:, jc, ci * 128:(ci + 1) * 128], A.ap[:, jc, :], jc == 0, jc == 31,
                            [Cb, A], jc == 31)
                    for jc in range(32):
                        _mm(k, pss, pss.ap[:], Sb.ap[:, jc, ci * 128:(ci + 1) * 128], Bm.ap[:, jc, :], jc == 0, jc == 31,
                            [Sb, Bm], jc == 31)
                    kk = ko[fc % 2]
                    cosa = rot.ap[:, 0, fc:fc + 1]
                    sina = rot.ap[:, 1, fc:fc + 1]
                    nsina = rot.ap[:, 2, fc:fc + 1]
                    _ts(k, "dve", t1.ap[:], pss.ap[:], nsina, None, ALU.mult, None, [pss, rot], [t1])
                    _stt(k, "dve", t1.ap[:], pc.ap[:], cosa, t1.ap[:], ALU.mult, ALU.add, [pc, rot, t1], [t1])
                    _tt(k, "pool", t1.ap[:], t1.ap[:], inv.ap[:], ALU.mult, [t1, inv], [t1])
                    _tt(k, "pool", kk.ap[:, 0, :], t1.ap[:], biasb.ap[:, o, :], ALU.add, [t1, biasb], [kk])
                    _ts(k, "dve", t2.ap[:], pss.ap[:], cosa, None, ALU.mult, None, [pss, rot], [t2])
                    _stt(k, "dve", t2.ap[:], pc.ap[:], sina, t2.ap[:], ALU.mult, ALU.add, [pc, rot, t2], [t2])
                    _tt(k, "pool", kk.ap[:, 1, :], t2.ap[:], inv.ap[:], ALU.mult, [t2, inv], [kk])
                    k.dma(P.KS.ap[o, :, fc * 128:(fc + 1) * 128, :].rearrange("r p c -> p r c"), kk.ap[:], reads=[kk],
                          wfree=[P.KS], sem="st_KS", rot=fc % 2, q="pool")
        k.barrier()


def dft4_stream(P, es, gw=256):
    P.d4_gw = gw
    P.d4 = [[P.sb(es, "d4_%d_%d" % (i, q), [128, 16, gw], BF16) for q in range(4)] for i in range(2)]
    P.d4_it = 0


def dft4_load(P, src, g):
    k = P.k
    gw = P.d4_gw
    i = P.d4_it % 2
    P.d4_it += 1
    for cs in range(2):
        for b in range(2):
            q = cs * 2 + b
            v = src[cs, b, :, g * gw:(g + 1) * gw].rearrange("(kc p) f -> p kc f", p=128)
            k.dma(P.d4[i][q].ap[:], v, writes=[P.d4[i][q]], sem="d4_%d_%d" % (i, q))
    return P.d4[i]


def phase_hy_conv(P):
    k, nc = P.k, P.nc
    sw_d = P.inp("hy_swT", [128, 3, 12])
    sb_d = P.inp("hy_sbT", [128, 12])
    ft_d = P.inp("dft_ft", [2, 2, HALF, HALF], BF16)
    nat_d = P.inp("dft_nat", [2, 2, HALF, HALF], BF16)
    id_d = P.ident_in
    if not hasattr(P, "OAB"):
        P.OAB = P.scratch("OAB", [D, S], BF16)
    X1T = P.scratch("X1T", [S, HY_W], BF16)
    X2C = P.scratch("X2C", [HY_W, S], F32)
    with ExitStack() as es:
        sw = P.sb(es, "hysw", [128, 3, 12])
        sbb = P.sb(es, "hysb", [128, 12])
        ident = P.sb(es, "ident_s", [128, 128])
        vT = P.sb(es, "hyvT", [128, 32, HY_W], BF16)
        k.dma(sw.ap[:], sw_d[:, :, :], writes=[sw], sem="misc")
        k.dma(sbb.ap[:], sb_d[:, :], writes=[sbb], sem="misc")
        k.dma(ident.ap[:], id_d[:, :], writes=[ident], sem="misc")
        es1 = ExitStack()
        ucv = P.sb(es1, "hyucv", [128, 4, S])
        ub = [P.sb(es1, "hyub%d" % i, [128, S]) for i in range(2)]
        x1s = [P.sb(es1, "hyx1s%d" % i, [128, HY_W], BF16) for i in range(2)]

        def load_u(ch):
            k.dma(ub[ch % 2].ap[:], P.U.ap[ch * 128:(ch + 1) * 128, :], reads=[P.U], writes=[ub[ch % 2]], sem="ld_u%d" % (ch % 2))

        load_u(0)
        for ch in range(12):
            if ch + 1 < 12:
                load_u(ch + 1)
            u = ub[ch % 2]
            dst = ucv.ap[:, ch % 4, :]
            _ts(k, "dve", dst, u.ap[:], sw.ap[:, 1, ch:ch + 1], sbb.ap[:, ch:ch + 1], ALU.mult, ALU.add, [u, sw, sbb], [ucv])
            _stt(k, "dve", ucv.ap[:, ch % 4, 1:S], u.ap[:, 0:S - 1], sw.ap[:, 0, ch:ch + 1], ucv.ap[:, ch % 4, 1:S],
                 ALU.mult, ALU.add, [u, sw, ucv], [ucv])
            _stt(k, "dve", ucv.ap[:, ch % 4, 0:S - 1], u.ap[:, 1:S], sw.ap[:, 2, ch:ch + 1], ucv.ap[:, ch % 4, 0:S - 1],
                 ALU.mult, ALU.add, [u, sw, ucv], [ucv])
            if ch >= 8:
                k.dma(X2C.ap[(ch - 8) * 128:(ch - 7) * 128, :], ucv.ap[:, ch % 4, :], reads=[ucv], wfree=[X2C], sem="st_X2C")
            if ch == 3 or ch == 7:
                for sc in range(32):
                    pb, rc = sc // 16, sc % 16
                    ps = next_ps(P)
                    for c4 in range(4):
                        src = ucv.ap[:, c4, :].rearrange("p (r two) -> p two r", two=2)[:, pb, rc * 128:(rc + 1) * 128]
                        k.op("pe", lambda e, c4=c4, ps=ps, src=src: e.transpose(out=ps.ap[:, c4 * 128:(c4 + 1) * 128],
                                                                               in_=src, identity=ident.ap[:]),
                             reads=[ucv, ident], writes=[ps], signal=(c4 == 3))
                    if ch == 3:
                        _copy(k, "act", vT.ap[:, sc, :], ps.ap[:], [ps], [vT])
                    else:
                        xs = x1s[sc % 2]
                        _copy(k, "act", xs.ap[:], ps.ap[:], [ps], [xs])
                        k.dma(X1T.ap[sc * 128:(sc + 1) * 128, :], xs.ap[:], reads=[xs], wfree=[X1T], sem="st_X1T", rot=sc % 2, q="pool")
        k.barrier()
        es1.close()
        UU = [[P.sb(es, "hyU%d%d" % (pb, w), [128, 16, HY_W], BF16) for w in range(2)] for pb in range(2)]
        dft4_stream(P, es)
        gw = P.d4_gw
        ng = HALF // gw
        cpg = gw // 128

        def forward(o):
            with ExitStack() as esf:
                kt = [P.sb(esf, "hykt%d" % i, [128, 2, 2, HY_W]) for i in range(2)]
                Pc = P.sb(esf, "hyPc", [128, HY_W])
                Ps_ = P.sb(esf, "hyPs", [128, HY_W])
                PcM = P.sb(esf, "hyPcM", [128, HY_W])
                PsM = P.sb(esf, "hyPsM", [128, HY_W])
                t1 = P.sb(esf, "hyt1", [128, HY_W])
                t2 = P.sb(esf, "hyt2", [128, HY_W])
                t3 = P.sb(esf, "hyt3", [128, HY_W])
                t4 = P.sb(esf, "hyt4", [128, HY_W])

                def load_k(fx):
                    kk = kt[fx % 2]
                    for hf_ in range(2):
                        g0 = (hf_ * 16 + fx) * 128
                        k.dma(kk.ap[:, hf_], P.KS.ap[o, :, g0:g0 + 128, :].rearrange("r p c -> p r c"), reads=[P.KS],
                              writes=[], wfree=[kk], sem="ld_kt%d" % (fx % 2))

                load_k(0)
                nxt = dft4_load(P, ft_d, 0)
                for g in range(ng):
                    T4 = nxt
                    if g + 1 < ng:
                        nxt = dft4_load(P, ft_d, g + 1)
                    for ci in range(cpg):
                        fx = g * cpg + ci
                        if fx + 1 < 16:
                            load_k(fx + 1)
                        kk = kt[fx % 2]
                        pa, pb_, pc_, pd = next_ps(P), next_ps(P), next_ps(P), next_ps(P)
                        for (pp, q, zoff) in ((pa, 0, 0), (pb_, 1, 16), (pc_, 2, 0), (pd, 3, 16)):
                            for rc in range(16):
                                _mm(k, pp, pp.ap[:], T4[q].ap[:, rc, ci * 128:(ci + 1) * 128], vT.ap[:, zoff + rc, :], rc == 0, rc == 15,
                                    [T4[q], vT], rc == 15)
                        _copy(k, "act", t1.ap[:], pb_.ap[:], [pb_], [t1])
                        _copy(k, "act", t2.ap[:], pd.ap[:], [pd], [t2])
                        _tt(k, "dve", Pc.ap[:], pa.ap[:], t1.ap[:], ALU.add, [pa, t1], [Pc])
                        _tt(k, "dve", PsM.ap[:], pa.ap[:], t1.ap[:], ALU.subtract, [pa, t1], [PsM])
                        _tt(k, "dve", Ps_.ap[:], pc_.ap[:], t2.ap[:], ALU.add, [pc_, t2], [Ps_])
                        _tt(k, "dve", PcM.ap[:], pc_.ap[:], t2.ap[:], ALU.subtract, [pc_, t2], [PcM])
                        _tt(k, "dve", t1.ap[:], Pc.ap[:], kk.ap[:, 0, 0, :], ALU.mult, [Pc, kk], [t1])
                        _tt(k, "dve", t3.ap[:], Ps_.ap[:], kk.ap[:, 0, 1, :], ALU.mult, [Ps_, kk], [t3])
                        _tt(k, "dve", t1.ap[:], t1.ap[:], t3.ap[:], ALU.add, [t1, t3], [t1])
                        _tt(k, "dve", t3.ap[:], Ps_.ap[:], kk.ap[:, 0, 0, :], ALU.mult, [Ps_, kk], [t3])
                        _tt(k, "dve", Ps_.ap[:], Pc.ap[:], kk.ap[:, 0, 1, :], ALU.mult, [Pc, kk], [Ps_])
                        _tt(k, "dve", t3.ap[:], t3.ap[:], Ps_.ap[:], ALU.subtract, [t3, Ps_], [t3])
                        _tt(k, "dve", t2.ap[:], PcM.ap[:], kk.ap[:, 1, 0, :], ALU.mult, [PcM, kk], [t2])
                        _tt(k, "dve", t4.ap[:], PsM.ap[:], kk.ap[:, 1, 1, :], ALU.mult, [PsM, kk], [t4])
                        _tt(k, "dve", t2.ap[:], t2.ap[:], t4.ap[:], ALU.add, [t2, t4], [t2])
                        _tt(k, "dve", t4.ap[:], PsM.ap[:], kk.ap[:, 1, 0, :], ALU.mult, [PsM, kk], [t4])
                        _tt(k, "dve", PsM.ap[:], PcM.ap[:], kk.ap[:, 1, 1, :], ALU.mult, [PcM, kk], [PsM])
                        _tt(k, "dve", t4.ap[:], t4.ap[:], PsM.ap[:], ALU.subtract, [t4, PsM], [t4])
                        _tt(k, "dve", UU[0][0].ap[:, fx, :], t1.ap[:], t4.ap[:], ALU.add, [t1, t4], [UU[0][0]])
                        _tt(k, "dve", UU[1][0].ap[:, fx, :], t1.ap[:], t4.ap[:], ALU.subtract, [t1, t4], [UU[1][0]])
                        _tt(k, "dve", UU[0][1].ap[:, fx, :], t3.ap[:], t2.ap[:], ALU.add, [t3, t2], [UU[0][1]])
                        _tt(k, "dve", UU[1][1].ap[:, fx, :], t3.ap[:], t2.ap[:], ALU.subtract, [t3, t2], [UU[1][1]])
                k.barrier()

        forward(0)
        with ExitStack() as esi:
            xg = [P.sb(esi, "hyxg%d" % i, [128, HY_W], BF16) for i in range(2)]
            nxt = dft4_load(P, nat_d, 0)
            it = 0
            for g in range(ng):
                T4 = nxt
                if g + 1 < ng:
                    nxt = dft4_load(P, nat_d, g + 1)
                for ci in range(cpg):
                    rc = g * cpg + ci
                    for pb in range(2):
                        sc = pb * 16 + rc
                        x1t = xg[it % 2]
                        k.dma(x1t.ap[:], X1T.ap[sc * 128:(sc + 1) * 128, :], reads=[X1T], writes=[x1t], sem="ld_xg%d" % (it % 2))
                        it += 1
                        ps = next_ps(P)
                        for fc in range(16):
                            _mm(k, ps, ps.ap[:], T4[pb].ap[:, fc, ci * 128:(ci + 1) * 128], UU[pb][0].ap[:, fc, :], fc == 0, False,
                                [T4[pb], UU[pb][0]], False)
                        for fc in range(16):
                            _mm(k, ps, ps.ap[:], T4[2 + pb].ap[:, fc, ci * 128:(ci + 1) * 128], UU[pb][1].ap[:, fc, :], False, fc == 15,
                                [T4[2 + pb], UU[pb][1]], fc == 15)
                        _tt(k, "dve", vT.ap[:, sc, :], ps.ap[:], x1t.ap[:], ALU.mult, [ps, x1t], [vT])
            k.barrier()
        forward(1)
        with ExitStack() as esi:
            x2g = [P.sb(esi, "hyx2g%d" % i, [128, 4, 2 * gw]) for i in range(2)]
            obs = [P.sb(esi, "hyob%d" % i, [128, 2 * gw], BF16) for i in range(3)]
            nxt = dft4_load(P, nat_d, 0)
            oc = 0
            for g in range(ng):
                T4 = nxt
                if g + 1 < ng:
                    nxt = dft4_load(P, nat_d, g + 1)
                x2t = x2g[g % 2]
                tok0 = g * 2 * gw
                k.dma(x2t.ap[:], X2C.ap[:, tok0:tok0 + 2 * gw].rearrange("(c p) t -> p c t", p=128), reads=[X2C],
                      writes=[x2t], sem="ld_x2g%d" % (g % 2))
                for cc in range(4):
                    ob = obs[oc % 3]
                    oc += 1
                    for pb in range(2):
                        ps = next_ps(P)
                        for fc in range(16):
                            _mm(k, ps, ps.ap[:, 0:gw], UU[pb][0].ap[:, fc, cc * 128:(cc + 1) * 128], T4[pb].ap[:, fc, :], fc == 0, False,
                                [T4[pb], UU[pb][0]], False)
                        for fc in range(16):
                            _mm(k, ps, ps.ap[:, 0:gw], UU[pb][1].ap[:, fc, cc * 128:(cc + 1) * 128], T4[2 + pb].ap[:, fc, :], False,
                                fc == 15, [T4[2 + pb], UU[pb][1]], fc == 15)
                        obv = ob.ap[:, :].rearrange("p (r two) -> p two r", two=2)[:, pb, :]
                        x2v = x2t.ap[:, cc, :].rearrange("p (r two) -> p two r", two=2)[:, pb, :]
                        _tt(k, "dve", obv, ps.ap[:, 0:gw], x2v, ALU.mult, [ps, x2t], [ob])
                    k.dma(P.OAB.ap[512 + cc * 128:512 + (cc + 1) * 128, tok0:tok0 + 2 * gw], ob.ap[:, :], reads=[ob],
                          wfree=[P.OAB], sem="st_OAB", rot=(oc - 1) % 3, q="pool")
            k.barrier()


def rms_rstd(P, xap, xbuf, T, sq, rstd, pre=None):
    k = P.k
    _act(k, sq.ap[:, :, 0:T], xap, AF.Square, [xbuf], [sq])
    ps = next_ps(P)
    if pre is None:
        for c in range(8):
            _mm(k, ps, ps.ap[:, 0:T], P.ones_f.ap[:], sq.ap[:, c, 0:T], c == 0, c == 7, [P.ones_f, sq], c == 7)
    else:
        k.op("dve", lambda e: e.reduce_sum(out=pre.ap[:, 0:T], in_=sq.ap[:, :, 0:T].rearrange("p c t -> p t c"), axis=AX.X),
             reads=[sq], writes=[pre])
        _mm(k, ps, ps.ap[:, 0:T], P.ones_f.ap[:], pre.ap[:, 0:T], True, True, [P.ones_f, pre], True)
    _act(k, rstd.ap[:, 0:T], ps.ap[:, 0:T], AF.Sqrt, [ps], [rstd], bias=EPS, scale=1.0 / D)
    k.op("dve", lambda e: e.reciprocal(out=rstd.ap[:, 0:T], in_=rstd.ap[:, 0:T]), reads=[rstd], writes=[rstd])


def phase_outproj0(P):
    k, nc = P.k, P.nc
    w_out = P.inp("w_out0", [D, D])
    xT = P.xT_d
    P.X1 = P.scratch("X1", [D, S], F32)
    mv = P.mv
    xTv = xT.rearrange("(c p) t -> p c t", p=128)
    with ExitStack() as es:
        W = P.sb(es, "w_out_s", [128, 8, D], BF16)
        xt = [P.sb(es, "opx%d" % i, [128, 8, 512]) for i in range(2)]
        ot = [P.sb(es, "opo%d" % i, [128, 8, 512], BF16) for i in range(2)]
        k.dma(W.ap[:], w_out.rearrange("(c p) n -> p c n", p=128), writes=[W], sem="w_out", q="pool")
        for t in range(8):
            xb, ob = xt[t % 2], ot[t % 2]
            k.dma(xb.ap[:], xTv[:, :, t * 512:(t + 1) * 512], writes=[xb], sem="opx%d" % (t % 2))
            k.dma(ob.ap[:], P.OAB.ap[:, t * 512:(t + 1) * 512].rearrange("(c p) t -> p c t", p=128), reads=[P.OAB],
                  writes=[ob], sem="opo%d" % (t % 2))
            for dc in range(8):
                ps = next_ps(P)
                for c in range(8):
                    _mm(k, ps, ps.ap[:], W.ap[:, c, dc * 128:(dc + 1) * 128], ob.ap[:, c, :], c == 0, c == 7, [W, ob], c == 7)
                _stt(k, "dve", xb.ap[:, dc, :], ps.ap[:], mv.ap[:, 0, 2, dc:dc + 1], xb.ap[:, dc, :], ALU.mult, ALU.add,
                     [ps, mv, xb], [xb])
            k.dma(P.X1.ap[:, t * 512:(t + 1) * 512].rearrange("(c p) t -> p c t", p=128), xb.ap[:], reads=[xb], wfree=[P.X1],
                  sem="st_X1", rot=t % 2, q="pool")
        k.barrier()


def phase_moe(P, layer, Xin, Xout, final=False, experts=range(N_EXP)):
    k, nc = P.k, P.nc
    L = layer
    wr_d = P.inp("moe_wr%d" % L, [D, 36])
    br_d = P.inp("moe_br%d" % L, [1, 36])
    wg_d = P.inp("moe_w_gate%d" % L, [N_EXP, D, D_EXP])
    wu_d = P.inp("moe_w_up%d" % L, [N_EXP, D, D_EXP])
    wd_d = P.inp("moe_w_down%d" % L, [N_EXP, D_EXP, D])
    if final:
        fg_d = P.inp("final_gT", [128, 8])
    CT = P.scratch("CT%d" % L, [N_EXP, S], F32)
    mv = P.mv
    TH = 2048
    with ExitStack() as es:
        xh = P.sb(es, "mxh", [128, 8, TH])
        h = P.sb(es, "mh", [128, 8, TH], BF16)
        wgs = [P.sb(es, "mwg%d" % i, [128, 8, D_EXP], BF16) for i in range(2)]
        wus = [P.sb(es, "mwu%d" % i, [128, 8, D_EXP], BF16) for i in range(2)]
        wds = [P.sb(es, "mwd%d" % i, [128, 4, D], BF16) for i in range(2)]
        ident = P.sb(es, "mident", [128, 128])
        wr = P.sb(es, "mwr", [128, 8, 36])
        brb = P.sb(es, "mbr", [128, 36])
        k.dma(ident.ap[:], P.ident_in[:, :], writes=[ident], sem="misc")
        k.dma(wr.ap[:], wr_d.rearrange("(c p) n -> p c n", p=128), writes=[wr], sem="misc")
        k.dma(brb.ap[:], br_d[0:1, :].partition_broadcast(128), writes=[brb], sem="misc")
        if final:
            fg = P.sb(es, "mfg", [128, 8])
            k.dma(fg.ap[:], fg_d[:, :], writes=[fg], sem="misc")

        def load_w(e, i):
            k.dma(wgs[i].ap[:], wg_d[e].rearrange("(c p) n -> p c n", p=128), writes=[wgs[i]], sem="mwg%d" % i, q="pool")
            k.dma(wus[i].ap[:], wu_d[e].rearrange("(c p) n -> p c n", p=128), writes=[wus[i]], sem="mwu%d" % i, q="pool")
            k.dma(wds[i].ap[:], wd_d[e].rearrange("(c p) n -> p c n", p=128), writes=[wds[i]], sem="mwd%d" % i, q="pool")

        elist = list(experts)
        witer = 0
        for hh in range(S // TH):
            tok0 = hh * TH
            k.dma(xh.ap[:], Xin.ap[:, tok0:tok0 + TH].rearrange("(c p) t -> p c t", p=128), reads=[Xin], writes=[xh],
                  sem="mxh")
            load_w(elist[0], witer % 2)
            with ExitStack() as es2:
                sq = P.sb(es2, "msq", [128, 8, 512])
                hf = P.sb(es2, "mhf", [128, 8, 512])
                rstd = P.sb(es2, "mrstd", [128, 512])
                lg = P.sb(es2, "mlg", [128, 36])
                sm = P.sb(es2, "msm", [128, 16])
                gm = P.sb(es2, "mgm", [128, 4])
                e1 = P.sb(es2, "me1", [128, 8])
                e2 = P.sb(es2, "me2", [128, 8])
                k1 = P.sb(es2, "mk1", [128, 8])
                k2 = P.sb(es2, "mk2", [128, 8])
                c8 = P.sb(es2, "mc8", [128, 8])
                cmb = P.sb(es2, "mcmb", [128, 32])
                cT = P.sb(es2, "mcT", [32, 512])
                for tt in range(TH // 512):
                    c0 = tt * 512
                    rms_rstd(P, xh.ap[:, :, c0:c0 + 512], xh, 512, sq, rstd)
                    _tt(k, "dve", sq.ap[:], xh.ap[:, :, c0:c0 + 512], rstd.ap[:].unsqueeze(1).to_broadcast([128, 8, 512]),
                        ALU.mult, [xh, rstd], [sq])
                    for c in range(8):
                        _act(k, hf.ap[:, c, :], sq.ap[:, c, :], AF.Identity, [sq, mv], [hf],
                             bias=mv.ap[:, L, 4, c:c + 1], scale=mv.ap[:, L, 3, c:c + 1])
                    _copy(k, "dve", h.ap[:, :, c0:c0 + 512], hf.ap[:], [hf], [h])
                    pT = next_ps(P)
                    for s4 in range(4):
                        ps = next_ps(P)
                        if ps is pT:
                            ps = next_ps(P)
                        for c in range(8):
                            _mm(k, ps, ps.ap[:, 0:36], hf.ap[:, c, s4 * 128:(s4 + 1) * 128], wr.ap[:, c, :], c == 0, c == 7,
                                [hf, wr], c == 7)
                        _tt(k, "dve", lg.ap[:], ps.ap[:, 0:36], brb.ap[:], ALU.add, [ps, brb], [lg])
                        lge = lg.ap[:, 4:36].rearrange("p (g e) -> p g e", e=8)
                        k.op("dve", lambda e: e.reduce_max(out=sm.ap[:, 0:1], in_=lg.ap[:, 0:4], axis=AX.X), reads=[lg], writes=[sm])
                        _ts(k, "dve", gm.ap[:], lg.ap[:, 0:4], sm.ap[:, 0:1], None, ALU.is_ge, None, [lg, sm], [gm])
                        _ts(k, "dve", sm.ap[:, 1:2], sm.ap[:, 0:1], -1.0, None, ALU.mult, None, [sm], [sm])
                        k.op("act", lambda e: e.activation(out=e2.ap[:, 0:4], in_=lg.ap[:, 0:4], func=AF.Exp, bias=sm.ap[:, 1:2],
                                                           scale=1.0, accum_out=sm.ap[:, 2:3]), reads=[lg, sm], writes=[e2, sm])
                        k.op("dve", lambda e: e.reciprocal(out=sm.ap[:, 3:4], in_=sm.ap[:, 2:3]), reads=[sm], writes=[sm])
                        _ts(k, "dve", e1.ap[:], lge[:, 0, :], gm.ap[:, 0:1], None, ALU.mult, None, [lg, gm], [e1])
                        for g in range(1, 4):
                            _stt(k, "dve", e1.ap[:], lge[:, g, :], gm.ap[:, g:g + 1], e1.ap[:], ALU.mult, ALU.add, [lg, gm, e1], [e1])
                        k.op("dve", lambda e: e.reduce_max(out=sm.ap[:, 4:5], in_=e1.ap[:], axis=AX.X), reads=[e1], writes=[sm])
                        _ts(k, "dve", k1.ap[:], e1.ap[:], sm.ap[:, 4:5], None, ALU.is_ge, None, [e1, sm], [k1])
                        _stt(k, "dve", e2.ap[:], k1.ap[:], -1.0e30, e1.ap[:], ALU.mult, ALU.add, [k1, e1], [e2])
                        k.op("dve", lambda e: e.reduce_max(out=sm.ap[:, 5:6], in_=e2.ap[:], axis=AX.X), reads=[e2], writes=[sm])
                        _ts(k, "dve", k2.ap[:], e2.ap[:], sm.ap[:, 5:6], None, ALU.is_ge, None, [e2, sm], [k2])
                        _ts(k, "dve", sm.ap[:, 6:7], sm.ap[:, 4:5], -1.0, None, ALU.mult, None, [sm], [sm])
                        _act(k, sm.ap[:, 7:8], sm.ap[:, 5:6], AF.Exp, [sm], [sm], bias=sm.ap[:, 6:7], scale=1.0)
                        _ts(k, "dve", sm.ap[:, 7:8], sm.ap[:, 7:8], 1.0, None, ALU.add, None, [sm], [sm])
                        k.op("dve", lambda e: e.reciprocal(out=sm.ap[:, 8:9], in_=sm.ap[:, 7:8]), reads=[sm], writes=[sm])
                        _ts(k, "dve", sm.ap[:, 9:10], sm.ap[:, 8:9], -1.0, 1.0, ALU.mult, ALU.add, [sm], [sm])
                        _ts(k, "dve", c8.ap[:], k1.ap[:], sm.ap[:, 8:9], None, ALU.mult, None, [k1, sm], [c8])
                        _stt(k, "dve", c8.ap[:], k2.ap[:], sm.ap[:, 9:10], c8.ap[:], ALU.mult, ALU.add, [k2, sm, c8], [c8])
                        _ts(k, "dve", c8.ap[:], c8.ap[:], sm.ap[:, 3:4], None, ALU.mult, None, [c8, sm], [c8])
                        for g in range(4):
                            _ts(k, "dve", cmb.ap[:, g * 8:(g + 1) * 8], c8.ap[:], gm.ap[:, g:g + 1], None, ALU.mult, None,
                                [c8, gm], [cmb])
                        k.op("pe", lambda e, s4=s4: e.transpose(out=pT.ap[0:32, s4 * 128:(s4 + 1) * 128], in_=cmb.ap[:, :],
                                                          identity=ident.ap[:]), reads=[cmb, ident], writes=[pT], signal=True)
                    _copy(k, "dve", cT.ap[:], pT.ap[0:32, :], [pT], [cT])
                    k.dma(CT.ap[:, tok0 + c0:tok0 + c0 + 512], cT.ap[:], reads=[cT], wfree=[CT], sem="st_CT%d" % L)
                k.barrier()
            es3 = ExitStack()
            cB = [P.sb(es3, "mcb%d" % i, [128, TH]) for i in range(2)]
            he = [P.sb(es3, "mhe%d" % i, [128, 4, 512], BF16) for i in range(2)]
            asb = [P.sb(es3, "masb%d" % i, [128, 512]) for i in range(2)]
            bsb = [P.sb(es3, "mbsb%d" % i, [128, 512]) for i in range(2)]
            for ei, e in enumerate(elist):
                wi = witer % 2
                witer += 1
                if ei + 1 < len(elist):
                    load_w(elist[ei + 1], witer % 2)
                cb = cB[ei % 2]
                k.dma(cb.ap[:], CT.ap[e:e + 1, tok0:tok0 + TH].partition_broadcast(128), reads=[CT], writes=[cb],
                      sem="mcb%d" % (ei % 2))
                Wg, Wu, Wd = wgs[wi], wus[wi], wds[wi]
                for tt in range(TH // 512):
                    c0 = tt * 512
                    hb = he[tt % 2]
                    for f in range(4):
                        pg = next_ps(P)
                        pu = next_ps(P)
                        for c in range(8):
                            _mm(k, pg, pg.ap[:], Wg.ap[:, c, f * 128:(f + 1) * 128], h.ap[:, c, c0:c0 + 512], c == 0, c == 7,
                                [Wg, h], c == 7)
                        for c in range(8):
                            _mm(k, pu, pu.ap[:], Wu.ap[:, c, f * 128:(f + 1) * 128], h.ap[:, c, c0:c0 + 512], c == 0, c == 7,
                                [Wu, h], c == 7)
                        a = asb[f % 2]
                        b = bsb[f % 2]
                        _act(k, a.ap[:], pg.ap[:], AF.Silu, [pg], [a])
                        _tt(k, "dve", b.ap[:], pu.ap[:], a.ap[:], ALU.mult, [pu, a], [b])
                        _tt(k, "dve", hb.ap[:, f, :], b.ap[:], cb.ap[:, c0:c0 + 512], ALU.mult, [b, cb], [hb])
                    for dc in range(8):
                        pd = next_ps(P)
                        for f in range(4):
                            _mm(k, pd, pd.ap[:], Wd.ap[:, f, dc * 128:(dc + 1) * 128], hb.ap[:, f, :], f == 0, f == 3, [Wd, hb],
                                f == 3)
                        _stt(k, "dve", xh.ap[:, dc, c0:c0 + 512], pd.ap[:], mv.ap[:, L, 5, dc:dc + 1], xh.ap[:, dc, c0:c0 + 512],
                             ALU.mult, ALU.add, [pd, mv, xh], [xh])
            k.barrier()
            es3.close()
            if not final:
                k.dma(Xout.ap[:, tok0:tok0 + TH].rearrange("(c p) t -> p c t", p=128), xh.ap[:], reads=[xh], wfree=[Xout],
                      sem="st_" + Xout.name)
            else:
                with ExitStack() as es2:
                    sq = P.sb(es2, "fsq", [128, 8, 512])
                    rstd = P.sb(es2, "frstd", [128, 512])
                    for tt in range(TH // 512):
                        c0 = tt * 512
                        rms_rstd(P, xh.ap[:, :, c0:c0 + 512], xh, 512, sq, rstd)
                        _tt(k, "dve", sq.ap[:], xh.ap[:, :, c0:c0 + 512], rstd.ap[:].unsqueeze(1).to_broadcast([128, 8, 512]),
                            ALU.mult, [xh, rstd], [sq])
                        for c in range(8):
                            _act(k, xh.ap[:, c, c0:c0 + 512], sq.ap[:, c, :], AF.Identity, [sq, fg], [xh], scale=fg.ap[:, c:c + 1])
                    k.dma(Xout.ap[:, tok0:tok0 + TH].rearrange("(c p) t -> p c t", p=128), xh.ap[:], reads=[xh], wfree=[Xout],
                          sem="st_" + Xout.name)
                    k.barrier()
        k.barrier()


CV_K = 31
CV_PAD = 15


def phase_conformer(P, Xin, Xout):
    k, nc = P.k, P.nc
    L = 1
    w1_d = P.inp("cv_w1", [D, 2 * D])
    w2_d = P.inp("cv_w2", [D, D])
    b1_d = P.inp("cv_b1T", [128, 16])
    dww_d = P.inp("cv_dw_wT", [128, CV_K, 8])
    vec_d = P.inp("cv_vecT", [128, 4, 8])
    mv = P.mv
    with ExitStack() as es:
        GLU = P.scratch("GLU", [D, S + 2 * CV_PAD], BF16)
        GLUv = GLU.ap.rearrange("(c p) t -> p c t", p=128)
        zpad = P.sb(es, "cvzpad", [128, 8, CV_PAD], BF16)
        b1 = P.sb(es, "cvb1", [128, 16])
        dww = P.sb(es, "cvdww", [128, CV_K, 8])
        vec = P.sb(es, "cvvec", [128, 5, 8])
        identb = P.sb(es, "cvidb", [128, 128], BF16)
        identf = P.sb(es, "cvidf", [128, 128])
        k.dma(b1.ap[:], b1_d[:, :], writes=[b1], sem="misc")
        k.dma(dww.ap[:], dww_d[:, :, :], writes=[dww], sem="misc")
        k.dma(vec.ap[:, 0:4, :], vec_d[:, :, :], writes=[vec], sem="misc")
        k.dma(identf.ap[:], P.ident_in[:, :], writes=[identf], sem="misc")
        _copy(k, "dve", identb.ap[:], identf.ap[:], [identf], [identb])
        _tt(k, "dve", vec.ap[:, 4, :], vec.ap[:, 3, :], mv.ap[:, L, 2, :], ALU.mult, [vec, mv], [vec])
        k.op("dve", lambda e: e.memset(zpad.ap[:], 0.0), writes=[zpad])
        k.dma(GLUv[:, :, 0:CV_PAD], zpad.ap[:], reads=[zpad], wfree=[GLU], sem="st_GLU")
        k.dma(GLUv[:, :, S + CV_PAD:S + 2 * CV_PAD], zpad.ap[:], reads=[zpad], wfree=[GLU], sem="st_GLU")
        dg = [P.sb(es, "cvdg%d" % c, [128, CV_K, 128], BF16) for c in range(8)]
        with ExitStack() as es1:
            W1 = P.sb(es1, "cvw1", [128, 8, 2 * D], BF16)
            xt = [P.sb(es1, "cvx%d" % i, [128, 8, 512]) for i in range(2)]
            sq = P.sb(es1, "cvsq", [128, 8, 512])
            rstd = P.sb(es1, "cvrstd", [128, 512])
            hx = [P.sb(es1, "cvhx%d" % i, [128, 8, 512], BF16) for i in range(2)]
            sg = [P.sb(es1, "cvsg%d" % i, [128, 512]) for i in range(2)]
            gt = [P.sb(es1, "cvgt%d" % i, [128, 8, 512], BF16) for i in range(2)]
            w1v = w1_d.rearrange("(c p) n -> p c n", p=128)
            for c in range(8):
                k.dma(W1.ap[:, c, :], w1v[:, c, :], writes=[], wfree=[W1], sem="cvw1", q="pool")
            for c in range(8):
                _tt(k, "pool", dg[c].ap[:], identf.ap[:].unsqueeze(1).to_broadcast([128, CV_K, 128]),
                    dww.ap[:, :, c:c + 1].to_broadcast([128, CV_K, 128]), ALU.mult, [identf, dww], [dg[c]])
            def norm1(t):
                xb, h = xt[t % 2], hx[t % 2]
                k.dma(xb.ap[:], Xin.ap[:, t * 512:(t + 1) * 512].rearrange("(c p) t -> p c t", p=128), reads=[Xin], writes=[xb],
                      sem="cvx%d" % (t % 2))
                rms_rstd(P, xb.ap[:], xb, 512, sq, rstd)
                _tt(k, "dve", sq.ap[:], xb.ap[:], rstd.ap[:].unsqueeze(1).to_broadcast([128, 8, 512]), ALU.mult, [xb, rstd], [sq])
                for c in range(8):
                    _act(k, h.ap[:, c, :], sq.ap[:, c, :], AF.Identity, [sq, mv], [h], bias=mv.ap[:, L, 1, c:c + 1],
                         scale=mv.ap[:, L, 0, c:c + 1])

            def glu1(t):
                h = hx[t % 2]
                for c in range(8):
                    pa = next_ps(P)
                    pg = next_ps(P)
                    for kc in range(8):
                        _mm(k, pa, pa.ap[:], W1.ap[:, kc, c * 128:(c + 1) * 128], h.ap[:, kc, :], kc == 0, kc == 7, [W1, h], kc == 7)
                    for kc in range(8):
                        _mm(k, pg, pg.ap[:], W1.ap[:, kc, D + c * 128:D + (c + 1) * 128], h.ap[:, kc, :], kc == 0, kc == 7, [W1, h],
                            kc == 7)
                    s_ = sg[c % 2]
                    _act(k, s_.ap[:], pg.ap[:], AF.Sigmoid, [pg, b1], [s_], bias=b1.ap[:, 8 + c:9 + c])
                    _stt(k, "dve", gt[t % 2].ap[:, c, :], pa.ap[:], b1.ap[:, c:c + 1], s_.ap[:],
                         ALU.add, ALU.mult, [pa, b1, s_], [gt[t % 2]])
                k.dma(GLUv[:, :, CV_PAD + t * 512:CV_PAD + (t + 1) * 512], gt[t % 2].ap[:], reads=[gt[t % 2]], wfree=[GLU],
                      sem="st_GLU", rot=t % 2, q="pool")

            norm1(0)
            for t in range(8):
                if t + 1 < 8:
                    norm1(t + 1)
                glu1(t)
            k.barrier()
        with ExitStack() as es2:
            W2 = P.sb(es2, "cvw2", [128, 8, D], BF16)
            xt = [P.sb(es2, "cvx2%d" % i, [128, 8, 512]) for i in range(1)]
            gl = [P.sb(es2, "cvgl%d" % i, [128, 8, 512 + 2 * CV_PAD], BF16) for i in range(2)]
            cvts = [P.sb(es2, "cvcvt%d" % i, [128, 8, 512]) for i in range(2)]
            sq = P.sb(es2, "cvsq2", [128, 8, 512])
            s1 = P.sb(es2, "cvs1", [128, 512])
            s2 = P.sb(es2, "cvs2", [128, 512])
            mean = P.sb(es2, "cvmean", [128, 512])
            rstd = P.sb(es2, "cvrstd2", [128, 512])
            act = [P.sb(es2, "cvact%d" % i, [128, 8, 512], BF16) for i in range(2)]
            k.dma(W2.ap[:], w2_d.rearrange("(c p) n -> p c n", p=128), writes=[W2], sem="cvw2", q="pool")

            def load_gl(t):
                k.dma(gl[t % 2].ap[:], GLUv[:, :, t * 512:t * 512 + 512 + 2 * CV_PAD], reads=[GLU], writes=[gl[t % 2]],
                      sem="cvgl%d" % (t % 2))

            def load_x(t):
                k.dma(xt[0].ap[:], Xin.ap[:, t * 512:(t + 1) * 512].rearrange("(c p) t -> p c t", p=128), reads=[Xin],
                      writes=[xt[0]], sem="cvx2")

            def conv(t):
                glu = gl[t % 2]
                cvt = cvts[t % 2]
                for c in range(8):
                    ps = next_ps(P)
                    for j in range(CV_K):
                        _mm(k, ps, ps.ap[:], dg[c].ap[:, j, :], glu.ap[:, c, j:j + 512], j == 0, j == CV_K - 1,
                            [dg[c], glu], j == CV_K - 1)
                    _act(k, cvt.ap[:, c, :], ps.ap[:], AF.Identity, [ps, vec], [cvt], bias=vec.ap[:, 0, c:c + 1])

            def chunk_sums(t):
                cvt = cvts[t % 2]
                _act(k, sq.ap[:], cvt.ap[:], AF.Square, [cvt], [sq])
                _tt(k, "dve", sq.ap[:, 0:4, :], sq.ap[:, 0:4, :], sq.ap[:, 4:8, :], ALU.add, [sq], [sq])
                _tt(k, "dve", sq.ap[:, 0:2, :], sq.ap[:, 0:2, :], sq.ap[:, 2:4, :], ALU.add, [sq], [sq])
                _tt(k, "dve", s2.ap[:], sq.ap[:, 0, :], sq.ap[:, 1, :], ALU.add, [sq], [s2])
                _tt(k, "dve", sq.ap[:, 0:4, :], cvt.ap[:, 0:4, :], cvt.ap[:, 4:8, :], ALU.add, [cvt, sq], [sq])
                _tt(k, "dve", sq.ap[:, 0:2, :], sq.ap[:, 0:2, :], sq.ap[:, 2:4, :], ALU.add, [sq], [sq])
                _tt(k, "dve", s1.ap[:], sq.ap[:, 0, :], sq.ap[:, 1, :], ALU.add, [sq], [s1])

            def ln_silu(t):
                cvt = cvts[t % 2]
                ab = act[t % 2]
                p1 = next_ps(P)
                p2 = next_ps(P)
                _mm(k, p1, p1.ap[:], P.ones_f.ap[:], s1.ap[:], True, True, [P.ones_f, s1], True)
                _mm(k, p2, p2.ap[:], P.ones_f.ap[:], s2.ap[:], True, True, [P.ones_f, s2], True)
                _ts(k, "dve", mean.ap[:], p1.ap[:], 1.0 / D, None, ALU.mult, None, [p1], [mean])
                _tt(k, "dve", rstd.ap[:], mean.ap[:], mean.ap[:], ALU.mult, [mean], [rstd])
                _stt(k, "dve", rstd.ap[:], p2.ap[:], 1.0 / D, rstd.ap[:], ALU.mult, ALU.subtract, [p2, rstd], [rstd])
                _act(k, rstd.ap[:], rstd.ap[:], AF.Sqrt, [rstd], [rstd], bias=EPS)
                k.op("dve", lambda e: e.reciprocal(out=rstd.ap[:], in_=rstd.ap[:]), reads=[rstd], writes=[rstd])
                _tt(k, "dve", cvt.ap[:], cvt.ap[:], mean.ap[:].unsqueeze(1).to_broadcast([128, 8, 512]), ALU.subtract,
                    [cvt, mean], [cvt])
                _tt(k, "pool", cvt.ap[:], cvt.ap[:], rstd.ap[:].unsqueeze(1).to_broadcast([128, 8, 512]), ALU.mult,
                    [cvt, rstd], [cvt])
                for c in range(8):
                    _act(k, ab.ap[:, c, :], cvt.ap[:, c, :], AF.Silu, [cvt, vec], [ab], bias=vec.ap[:, 2, c:c + 1],
                         scale=vec.ap[:, 1, c:c + 1])

            def w2_res(t):
                xb, ab = xt[0], act[t % 2]
                for dc in range(8):
                    ps = next_ps(P)
                    for kc in range(8):
                        _mm(k, ps, ps.ap[:], W2.ap[:, kc, dc * 128:(dc + 1) * 128], ab.ap[:, kc, :], kc == 0, kc == 7, [W2, ab], kc == 7)
                    _act(k, xb.ap[:, dc, :], xb.ap[:, dc, :], AF.Identity, [xb, vec], [xb], bias=vec.ap[:, 4, dc:dc + 1])
                    _stt(k, "dve", xb.ap[:, dc, :], ps.ap[:], mv.ap[:, L, 2, dc:dc + 1], xb.ap[:, dc, :], ALU.mult, ALU.add,
                         [ps, mv, xb], [xb])
                k.dma(Xout.ap[:, t * 512:(t + 1) * 512].rearrange("(c p) t -> p c t", p=128), xb.ap[:], reads=[xb], wfree=[Xout],
                      sem="st_" + Xout.name)

            load_gl(0)
            load_gl(1)
            conv(0)
            chunk_sums(0)
            ln_silu(0)
            for t in range(8):
                load_x(t)
                if t + 2 < 8:
                    pass
                if t + 1 < 8:
                    conv(t + 1)
                    chunk_sums(t + 1)
                    if t + 2 < 8:
                        load_gl(t + 2)
                w2_res(t)
                if t + 1 < 8:
                    ln_silu(t + 1)
            k.barrier()


def build_program():
    P = Prog()
    setup_consts(P)
    phase_adaln(P)
    phase_front0(P)
    moe_precast_setup(P, 0)
    moe_precast_setup(P, 1)
    phase_attn(P)
    moe_precast_emit(P)
    phase_hy_filters(P)
    phase_hy_conv(P)
    phase_outproj0(P)
    X2 = P.scratch("X2", [D, S], F32)
    phase_moe_sparse(P, 0, P.X1, X2)
    X3 = P.scratch("X3", [D, S], F32)
    phase_conformer(P, X2, X3)
    outT = Buf(P.out("outT", [D, S]), "outT")
    phase_moe_sparse(P, 1, X3, outT, final=True)
    return P


def _colT(v):
    return np.ascontiguousarray(np.asarray(v, np.float32).reshape(-1, 128).T)


_CONST_CACHE = {}


def _consts():
    if not _CONST_CACHE:
        cos, sin, rt = rope_tables()
        zpos, delta, tcol, rot = hyena_consts()
        cg, sg, nat, ft = dft_mats()
        _CONST_CACHE.update(dict(rope_cos=cos, rope_sin=sin, rope_rt=rt, hy_zpos=zpos, hy_delta=delta, hy_tcol=tcol,
                                 hy_rot=rot, dft_cg=cg, dft_sg=sg, dft_nat=nat, dft_ft=ft, ident=np.eye(128, dtype=np.float32)))
    return _CONST_CACHE


def kernel(x, c, ctx, c_ctx, ada_w, ada_b, norm1_g, norm2_g, final_g,
           w_in0, w_out0, lam_q1, lam_k1, lam_q2, lam_k2, subln_g,
           hy_short_w, hy_short_b, hy_w1, hy_b1, hy_fr1, hy_w2, hy_b2, hy_fr2, hy_w3, hy_b3, hy_bias,
           cv_w1, cv_b1, cv_dw_w, cv_dw_b, cv_ln_g, cv_ln_b, cv_w2, cv_b2,
           moe_wg, moe_bg, moe_we, moe_be, moe_w_gate, moe_w_up, moe_w_down):
    f32 = np.float32
    A = lambda v: np.ascontiguousarray(np.asarray(v, f32))
    P = build_program()
    shared = dict(_consts())
    shared.update({
        "ada_w": A(ada_w),
        "ada_bT": np.ascontiguousarray(np.stack([_colT(ada_b[l]) for l in range(2)], axis=1)),
        "norm1_gT": np.ascontiguousarray(np.stack([_colT(norm1_g[l]) for l in range(2)], axis=1)),
        "norm2_gT": np.ascontiguousarray(np.stack([_colT(norm2_g[l]) for l in range(2)], axis=1)),
        "final_gT": _colT(final_g),
        "w_in0": A(w_in0[0]), "w_out0": A(w_out0[0]),
        "lamv": A(np.stack([lam_q1[0], lam_k1[0], lam_q2[0], lam_k2[0]])),
        "subln_gT": A(subln_g[0]).reshape(128, 1),
        "hy_swT": np.ascontiguousarray(np.stack([_colT(hy_short_w[0][j]) for j in range(3)], axis=1)),
        "hy_sbT": _colT(hy_short_b[0]),
        "hy_w1": A(hy_w1[0]), "hy_w2": A(hy_w2[0]), "hy_w3": A(hy_w3[0]), "hy_b3": A(hy_b3[0]).reshape(1, 2048),
        "hy_vec": np.ascontiguousarray(np.stack([A(hy_b1[0]), A(hy_fr1[0]), A(hy_b2[0]), A(hy_fr2[0])], axis=1)),
        "hy_bias": A(hy_bias[0]),
        "cv_w1": A(cv_w1[0]), "cv_w2": A(cv_w2[0]), "cv_b1T": _colT(cv_b1[0]),
        "cv_dw_wT": np.ascontiguousarray(np.stack([_colT(cv_dw_w[0][j]) for j in range(CV_K)], axis=1)),
        "cv_vecT": np.ascontiguousarray(np.stack([_colT(cv_dw_b[0]), _colT(cv_ln_g[0]), _colT(cv_ln_b[0]), _colT(cv_b2[0])], axis=1)),
    })
    for L in range(2):
        shared["moe_wr%d" % L] = np.ascontiguousarray(np.concatenate([A(moe_wg[L]), A(moe_we[L])], axis=1))
        shared["moe_br%d" % L] = np.concatenate([A(moe_bg[L]), A(moe_be[L])]).reshape(1, 36)
        shared["moe_wt%d" % L] = moe_weight_table(moe_w_gate[L], moe_w_up[L], moe_w_down[L])
    lt, tstart, slotvals, init, pio = moe_consts()
    shared.update({"moe_lt": lt, "moe_tstart": tstart, "moe_slotvals": slotvals, "moe_idxinit": init, "moe_pio": pio})
    x = np.asarray(x, f32)
    ctx = np.asarray(ctx, f32)
    c = np.asarray(c, f32)
    c_ctx = np.asarray(c_ctx, f32)
    in_maps = []
    for b in range(8):
        m = dict(shared)
        m["xT"] = np.ascontiguousarray(x[b].T)
        m["ctxT"] = np.ascontiguousarray(ctx[b].T)
        m["ccol"] = np.ascontiguousarray(np.stack([_colT(c[b]), _colT(c_ctx)], axis=-1))
        in_maps.append({k_: m[k_] for k_ in P.inputs})
    res = run_bass_kernel_spmd(P.nc, in_maps, core_ids=list(range(8)))
    out = np.stack([np.ascontiguousarray(res.results[b]["outT"].T) for b in range(8)], axis=0)
    return out.astype(f32)


NT_TILES = (2 * S) // 512 + N_EXP
NSLOT = NT_TILES * 512
WROW = 2 * 8 * D_EXP + 4 * D


def moe_consts():
    lt = np.triu(np.ones((128, 128), np.float32), 1)
    tstart = np.tile((512.0 * np.arange(NT_TILES, dtype=np.float32))[None, :], (32, 1)).astype(np.float32)
    p = np.arange(128)[:, None, None]
    c = np.arange(32)[None, :, None]
    kk = np.arange(2)[None, None, :]
    src = np.broadcast_to(c * 128 + p, (128, 32, 2))
    dst = kk * S + c * 128 + p
    slotvals = np.stack([src, np.broadcast_to(dst, (128, 32, 2))], axis=-1).astype(np.int32)
    init = np.zeros((128, NSLOT // 128, 2), np.int32)
    init[..., 0] = S
    init[..., 1] = 1 << 30
    pio = np.arange(128, dtype=np.float32).reshape(128, 1)
    return lt, tstart, slotvals, init, pio


def moe_weight_table(wg, wu, wd):
    g = np.asarray(wg, np.float32).reshape(N_EXP, 8, 128, D_EXP).transpose(0, 2, 1, 3).reshape(N_EXP * 128, 8 * D_EXP)
    u = np.asarray(wu, np.float32).reshape(N_EXP, 8, 128, D_EXP).transpose(0, 2, 1, 3).reshape(N_EXP * 128, 8 * D_EXP)
    d = np.asarray(wd, np.float32).reshape(N_EXP, 4, 128, D).transpose(0, 2, 1, 3).reshape(N_EXP * 128, 4 * D)
    return np.ascontiguousarray(np.concatenate([g, u, d], axis=1))


def moe_precast_setup(P, L):
    if not hasattr(P, "moe_wtb"):
        P.moe_wtb = {}
        P.precast_q = []
    if L in P.moe_wtb:
        return
    src = P.inp("moe_wt%d" % L, [N_EXP * 128, WROW])
    dst = P.scratch("WTB%d" % L, [N_EXP * 128, WROW], BF16)
    P.moe_wtb[L] = dst
    ds = P.k.dsem("precast%d" % L, persistent=True)
    for e in range(N_EXP):
        for hh in range(2):
            r0 = e * 128 + hh * 64
            P.precast_q.append((dst, src, r0, ds))


def moe_precast_emit(P, n=None, after=None):
    k = P.k
    q = getattr(P, "precast_q", [])
    cnt = len(q) if n is None else min(n, len(q))
    for _ in range(cnt):
        dst, src, r0, ds = q.pop(0)
        if after is not None:
            k._wait("pool", after)
        k.dma(dst.ap[r0:r0 + 64, :], src[r0:r0 + 64, :], writes=[], wfree=[dst], sem=ds, q="pool")


def moe_precast(P, L):
    moe_precast_setup(P, L)
    moe_precast_emit(P)


def phase_moe_sparse(P, layer, Xin, Xout, final=False):
    k, nc = P.k, P.nc
    L = layer
    wr_d = P.inp("moe_wr%d" % L, [D, 36])
    br_d = P.inp("moe_br%d" % L, [1, 36])
    moe_precast(P, L)
    wt_d = P.moe_wtb[L].ap
    WTB = P.moe_wtb[L]
    if not hasattr(P, "moe_c"):
        P.moe_c = (P.inp("moe_lt", [128, 128]), P.inp("moe_tstart", [32, NT_TILES]),
                   P.inp("moe_slotvals", [128, 32, 2, 2], I32), P.inp("moe_idxinit", [128, NSLOT // 128, 2], I32),
                   P.inp("moe_pio", [128, 1]))
    lt_d, ts_d, sv_d, ii_d, pio_d = P.moe_c
    if final:
        fg_d = P.inp("final_gT", [128, 8])
    HT = P.scratch("HT%d" % L, [S + 128, D], BF16)
    IDX = P.scratch("IDX%d" % L, [NSLOT, 2], I32)
    Y2 = P.scratch("Y2_%d" % L, [2 * S, D], F32)
    mv = P.mv
    ps_all = P.psum
    with ExitStack() as es:
        ident = P.sb(es, "sident", [128, 128])
        identb = P.sb(es, "sidentb", [128, 128], BF16)
        ltri = P.sb(es, "sltri", [128, 128])
        wr = P.sb(es, "swr", [128, 8, 36])
        brb = P.sb(es, "sbr", [128, 36])
        Mst = P.sb(es, "sMst", [128, 32, 2, 32])
        rank = P.sb(es, "srank", [128, 32, 32])
        cum = P.sb(es, "scum", [128, 32])
        wts = P.sb(es, "swts", [128, 32, 2])
        widx = P.sb(es, "swidx", [128, NT_TILES], I32)
        pio = P.sb(es, "spio", [128, 1])
        k.dma(pio.ap[:], pio_d[:, :], writes=[pio], sem="misc")
        k.dma(ident.ap[:], P.ident_in[:, :], writes=[ident], sem="misc")
        k.dma(ltri.ap[:], lt_d[:, :], writes=[ltri], sem="misc")
        k.dma(wr.ap[:], wr_d.rearrange("(c p) n -> p c n", p=128), writes=[wr], sem="misc")
        k.dma(brb.ap[:], br_d[0:1, :].partition_broadcast(128), writes=[brb], sem="misc")
        _copy(k, "dve", identb.ap[:], ident.ap[:], [ident], [identb])
        with ExitStack() as es0:
            ii = P.sb(es0, "sii", [128, NSLOT // 128, 2], I32)
            zr = P.sb(es0, "szr", [128, D], BF16)
            k.dma(ii.ap[:], ii_d[:, :, :], writes=[ii], sem="misc")
            k.dma(IDX.ap.rearrange("(p r) c -> p r c", p=128), ii.ap[:], reads=[ii], writes=[IDX], sem="st_IDX%d" % L)
            k.op("dve", lambda e: e.memset(zr.ap[:], 0.0), writes=[zr])
            k.dma(HT.ap[S:S + 128, :], zr.ap[:], reads=[zr], wfree=[HT], sem="st_HT%d" % L)
            k.barrier()
        with ExitStack() as es2:
            xt = [P.sb(es2, "sx%d" % i, [128, 8, 512]) for i in range(2)]
            sq = P.sb(es2, "ssq", [128, 8, 512])
            hfs = [P.sb(es2, "shf%d" % i, [128, 8, 512]) for i in range(2)]
            rstd = P.sb(es2, "srstd", [128, 512])
            pre = P.sb(es2, "spre", [128, 512])
            hbs = [P.sb(es2, "shb%d" % i, [128, 8, 512], BF16) for i in range(2)]
            lg = P.sb(es2, "slg", [128, 32, 36])
            htk = [P.sb(es2, "shtk%d" % i, [128, D], BF16) for i in range(3)]
            P.ps_list = ps_all

            def load_x(tt):
                k.dma(xt[tt % 2].ap[:], Xin.ap[:, tt * 512:(tt + 1) * 512].rearrange("(c p) t -> p c t", p=128), reads=[Xin],
                      writes=[xt[tt % 2]], sem="sx%d" % (tt % 2))

            def normA(tt):
                xb = xt[tt % 2]
                hf = hfs[tt % 2]
                hb16 = hbs[tt % 2]
                rms_rstd(P, xb.ap[:], xb, 512, sq, rstd, pre=pre)
                _tt(k, "dve", sq.ap[:], xb.ap[:], rstd.ap[:].unsqueeze(1).to_broadcast([128, 8, 512]), ALU.mult, [xb, rstd], [sq])
                for c in range(8):
                    _act(k, hf.ap[:, c, :], sq.ap[:, c, :], AF.Identity, [sq, mv], [hf],
                         bias=mv.ap[:, L, 4, c:c + 1], scale=mv.ap[:, L, 3, c:c + 1])
                for c in range(8):
                    _act(k, hb16.ap[:, c, :], sq.ap[:, c, :], AF.Identity, [sq, mv], [hb16],
                         bias=mv.ap[:, L, 4, c:c + 1], scale=mv.ap[:, L, 3, c:c + 1])

            def projA(tt):
                hf = hfs[tt % 2]
                hb16 = hbs[tt % 2]
                for s4 in range(4):
                    cg = tt * 4 + s4
                    pa = next_ps(P)
                    pav = pa.ap[:].bitcast(BF16)
                    for c in range(8):
                        k.op("pe", lambda e, c=c, pav=pav, s4=s4, hb16=hb16: e.transpose(out=pav[:, c * 128:(c + 1) * 128],
                                                                                     in_=hb16.ap[:, c, s4 * 128:(s4 + 1) * 128],
                                                                                     identity=identb.ap[:]),
                             reads=[hb16, identb], writes=[pa], signal=(c == 7))
                    hk = htk[cg % 3]
                    _copy(k, "act", hk.ap[:, :], pav[:, 0:1024], [pa], [hk])
                    k.dma(HT.ap[cg * 128:(cg + 1) * 128, :], hk.ap[:], reads=[hk], wfree=[HT], sem="st_HT%d" % L, rot=cg % 3, q="pool")
                    ps = next_ps(P)
                    for c in range(8):
                        _mm(k, ps, ps.ap[:, 0:36], hf.ap[:, c, s4 * 128:(s4 + 1) * 128], wr.ap[:, c, :], c == 0, c == 7,
                            [hf, wr], c == 7)
                    _tt(k, "dve", lg.ap[:, cg, :], ps.ap[:, 0:36], brb.ap[:], ALU.add, [ps, brb], [lg])

            load_x(0)
            load_x(1)
            normA(0)
            for tt in range(8):
                if tt + 1 < 8:
                    normA(tt + 1)
                if tt + 2 < 8:
                    load_x(tt + 2)
                projA(tt)
            G = 32
            gmax = P.sb(es2, "sgmax", [128, G])
            gm = P.sb(es2, "sgm", [128, G, 4])
            t4 = P.sb(es2, "st4", [128, G, 4])
            gw = P.sb(es2, "sgw", [128, G])
            p48 = P.sb(es2, "sp48", [128, G, 4, 8])
            e1 = P.sb(es2, "se1", [128, G, 8])
            e2 = P.sb(es2, "se2", [128, G, 8])
            k1 = P.sb(es2, "sk1", [128, G, 8])
            k2 = P.sb(es2, "sk2", [128, G, 8])
            m1 = P.sb(es2, "sm1", [128, G])
            m2 = P.sb(es2, "sm2", [128, G])
            w1 = P.sb(es2, "sw1", [128, G])
            w2 = P.sb(es2, "sw2", [128, G])
            msum = P.sb(es2, "smsum", [128, G, 32])
            tot = P.sb(es2, "stot", [128, G, 32])
            cumx = P.sb(es2, "scumx", [128, G, 32])

            def bc2(ap, n):
                return ap.unsqueeze(2).to_broadcast([128, G, n])

            lge = lg.ap[:, :, 4:36].rearrange("p c (g e) -> p c g e", e=8)
            k.op("dve", lambda e: e.reduce_max(out=gmax.ap[:], in_=lg.ap[:, :, 0:4], axis=AX.X), reads=[lg], writes=[gmax])
            _tt(k, "dve", gm.ap[:], lg.ap[:, :, 0:4], bc2(gmax.ap[:], 4), ALU.is_ge, [lg, gmax], [gm])
            _tt(k, "dve", t4.ap[:], lg.ap[:, :, 0:4], bc2(gmax.ap[:], 4), ALU.subtract, [lg, gmax], [t4])
            _act(k, t4.ap[:], t4.ap[:], AF.Exp, [t4], [t4])
            k.op("dve", lambda e: e.reduce_sum(out=gw.ap[:], in_=t4.ap[:], axis=AX.X), reads=[t4], writes=[gw])
            k.op("dve", lambda e: e.reciprocal(out=gw.ap[:], in_=gw.ap[:]), reads=[gw], writes=[gw])
            _tt(k, "dve", p48.ap[:], lge, gm.ap[:].unsqueeze(3).to_broadcast([128, G, 4, 8]), ALU.mult, [lg, gm], [p48])
            k.op("dve", lambda e: e.reduce_sum(out=e1.ap[:], in_=p48.ap[:].rearrange("p c g e -> p c e g"), axis=AX.X),
                 reads=[p48], writes=[e1])
            k.op("dve", lambda e: e.reduce_max(out=m1.ap[:], in_=e1.ap[:], axis=AX.X), reads=[e1], writes=[m1])
            _tt(k, "dve", k1.ap[:], e1.ap[:], bc2(m1.ap[:], 8), ALU.is_ge, [e1, m1], [k1])
            _stt(k, "dve", e2.ap[:], k1.ap[:], -1.0e30, e1.ap[:], ALU.mult, ALU.add, [k1, e1], [e2])
            k.op("dve", lambda e: e.reduce_max(out=m2.ap[:], in_=e2.ap[:], axis=AX.X), reads=[e2], writes=[m2])
            _tt(k, "dve", k2.ap[:], e2.ap[:], bc2(m2.ap[:], 8), ALU.is_ge, [e2, m2], [k2])
            _tt(k, "dve", w1.ap[:], m2.ap[:], m1.ap[:], ALU.subtract, [m2, m1], [w1])
            _act(k, w1.ap[:], w1.ap[:], AF.Exp, [w1], [w1])
            _ts(k, "dve", w1.ap[:], w1.ap[:], 1.0, None, ALU.add, None, [w1], [w1])
            k.op("dve", lambda e: e.reciprocal(out=w1.ap[:], in_=w1.ap[:]), reads=[w1], writes=[w1])
            _ts(k, "dve", w2.ap[:], w1.ap[:], -1.0, 1.0, ALU.mult, ALU.add, [w1], [w2])
            _tt(k, "dve", wts.ap[:, :, 0], w1.ap[:], gw.ap[:], ALU.mult, [w1, gw], [wts])
            _tt(k, "dve", wts.ap[:, :, 1], w2.ap[:], gw.ap[:], ALU.mult, [w2, gw], [wts])
            for kk, kx in enumerate((k1, k2)):
                _tt(k, "dve", Mst.ap[:, :, kk, :].rearrange("p c (g e) -> p c g e", e=8),
                    gm.ap[:].unsqueeze(3).to_broadcast([128, G, 4, 8]), kx.ap[:].unsqueeze(2).to_broadcast([128, G, 4, 8]),
                    ALU.mult, [gm, kx], [Mst])
            _tt(k, "dve", msum.ap[:], Mst.ap[:, :, 0, :], Mst.ap[:, :, 1, :], ALU.add, [Mst], [msum])
            msf = msum.ap[:].rearrange("p c e -> p (c e)")
            prb = [next_ps(P), next_ps(P)]
            ptb = [next_ps(P), next_ps(P)]
            for hh in range(2):
                _mm(k, prb[hh], prb[hh].ap[:], ltri.ap[:], msf[:, hh * 512:(hh + 1) * 512], True, True, [ltri, msum], True)
                _mm(k, ptb[hh], ptb[hh].ap[:], P.ones_f.ap[:], msf[:, hh * 512:(hh + 1) * 512], True, True, [P.ones_f, msum], True)
            for hh in range(2):
                _copy(k, "act", tot.ap[:, hh * 16:(hh + 1) * 16, :].rearrange("p c e -> p (c e)"), ptb[hh].ap[:], [ptb[hh]], [tot])
            k.op("dve", lambda e: e.memset(cumx.ap[:, 0, :], 0.0), writes=[cumx])
            for c in range(1, G):
                _tt(k, "dve", cumx.ap[:, c, :], cumx.ap[:, c - 1, :], tot.ap[:, c - 1, :], ALU.add, [cumx, tot], [cumx])
            _tt(k, "dve", cum.ap[:], cumx.ap[:, G - 1, :], tot.ap[:, G - 1, :], ALU.add, [cumx, tot], [cum])
            for hh in range(2):
                _tt(k, "dve", rank.ap[:, hh * 16:(hh + 1) * 16, :].rearrange("p c e -> p (c e)"), prb[hh].ap[:],
                    cumx.ap[:, hh * 16:(hh + 1) * 16, :].rearrange("p c e -> p (c e)"), ALU.add, [prb[hh], cumx], [rank])
            k.barrier()
        with ExitStack() as es3:
            ncol = P.sb(es3, "sncol", [32, 2])
            pcol = P.sb(es3, "spcol", [32, 2])
            Up = P.sb(es3, "sUp", [32, 32])
            Ui = P.sb(es3, "sUi", [32, 32])
            offs = P.sb(es3, "soffs", [128, 32])
            endc = P.sb(es3, "sendc", [32, 2])
            tst = P.sb(es3, "ststart", [32, NT_TILES])
            cmpm = P.sb(es3, "scmp", [32, NT_TILES])
            eidf = P.sb(es3, "seidf", [128, NT_TILES])
            tmp3 = P.sb(es3, "stmp3", [128, 32, 32])
            prod = P.sb(es3, "sprod", [128, 32, 32])
            posf = P.sb(es3, "sposf", [128, 32, 2])
            posi = P.sb(es3, "sposi", [128, 32, 2], I32)
            sv = P.sb(es3, "ssv", [128, 32, 2, 2], I32)
            k.dma(tst.ap[:], ts_d[:, :], writes=[tst], sem="misc")
            k.dma(sv.ap[:], sv_d[:, :, :, :], writes=[sv], sem="misc")
            MAGIC = 12582912.0
            p0 = next_ps(P)
            _mm(k, p0, p0.ap[0:32, 0:2], cum.ap[0:1, :], P.ones_f.ap[0:1, 0:2], True, True, [cum, P.ones_f], True)
            _copy(k, "dve", ncol.ap[:], p0.ap[0:32, 0:2], [p0], [ncol])
            _ts(k, "dve", pcol.ap[:], ncol.ap[:], 511.0, 1.0 / 512.0, ALU.add, ALU.mult, [ncol], [pcol])
            _ts(k, "dve", pcol.ap[:], pcol.ap[:], -0.5 + 1.0 / 1024.0, MAGIC, ALU.add, ALU.add, [pcol], [pcol])
            _ts(k, "dve", pcol.ap[:], pcol.ap[:], -MAGIC, 512.0, ALU.add, ALU.mult, [pcol], [pcol])
            _ts(k, "dve", Up.ap[:], ltri.ap[0:32, 0:32], pcol.ap[:, 0:1], None, ALU.mult, None, [ltri, pcol], [Up])
            _tt(k, "dve", Ui.ap[:], ltri.ap[0:32, 0:32], ident.ap[0:32, 0:32], ALU.add, [ltri, ident], [Ui])
            p1 = next_ps(P)
            _mm(k, p1, p1.ap[:, 0:32], P.ones_f.ap[0:32, :], Up.ap[:], True, True, [P.ones_f, Up], True)
            _copy(k, "dve", offs.ap[:], p1.ap[:, 0:32], [p1], [offs])
            p2 = next_ps(P)
            _mm(k, p2, p2.ap[0:32, 0:2], Ui.ap[:], pcol.ap[:], True, True, [Ui, pcol], True)
            _copy(k, "dve", endc.ap[:], p2.ap[0:32, 0:2], [p2], [endc])
            _ts(k, "dve", cmpm.ap[:], tst.ap[:], endc.ap[:, 0:1], None, ALU.is_ge, None, [tst, endc], [cmpm])
            p3 = next_ps(P)
            _mm(k, p3, p3.ap[:, 0:NT_TILES], P.ones_f.ap[0:32, :], cmpm.ap[:], True, True, [P.ones_f, cmpm], True)
            _ts(k, "dve", eidf.ap[:], p3.ap[:, 0:NT_TILES], float(N_EXP - 1), None, ALU.min, None, [p3], [eidf])
            _ts(k, "dve", eidf.ap[:], eidf.ap[:], 128.0, pio.ap[:, 0:1], ALU.mult, ALU.add, [eidf, pio], [eidf])
            _copy(k, "dve", widx.ap[:], eidf.ap[:], [eidf], [widx])
            _tt(k, "dve", tmp3.ap[:], rank.ap[:], offs.ap[:].unsqueeze(1).to_broadcast([128, 32, 32]), ALU.add, [rank, offs], [tmp3])
            for kk in range(2):
                _tt(k, "dve", prod.ap[:], tmp3.ap[:], Mst.ap[:, :, kk, :], ALU.mult, [tmp3, Mst], [prod])
                k.op("dve", lambda e, kk=kk: e.reduce_sum(out=posf.ap[:, :, kk], in_=prod.ap[:], axis=AX.X), reads=[prod], writes=[posf])
            _copy(k, "dve", posi.ap[:], posf.ap[:], [posf], [posi])
            bc_idx = nc.gpsimd.alloc_register("moe_bci%d" % L)
            nc.gpsimd.reg_mov(bc_idx, NSLOT - 1)
            for c in range(32):
                for kk in range(2):
                    k.dma_raw("pool", lambda e, c=c, kk=kk: e.indirect_dma_start(
                        out=IDX.ap[:, :], out_offset=bass.IndirectOffsetOnAxis(ap=posi.ap[:, c, kk:kk + 1], axis=0),
                        in_=sv.ap[:, c, kk, :], in_offset=None, bounds_check=bc_idx, oob_is_err=False),
                        reads=[posi, sv], wfree=[IDX], sem="st_IDX%d" % L)
            k.barrier()
        with ExitStack() as es4:
            NWB = 3
            Wt = [P.sb(es4, "swt%d" % i, [128, WROW], BF16) for i in range(NWB)]
            idt = [P.sb(es4, "sidt%d" % i, [128, 4, 2], I32) for i in range(NWB)]
            hg = [P.sb(es4, "shg%d" % i, [128, D], BF16) for i in range(8)]
            hT = [P.sb(es4, "shT%d" % i, [128, 8, 512], BF16) for i in range(2)]
            he = [P.sb(es4, "she%d" % i, [128, 4, 512], BF16) for i in range(2)]
            asb = [P.sb(es4, "sasb%d" % i, [128, 512]) for i in range(2)]
            NY = 8
            ysl = [P.sb(es4, "sysl%d" % i, [128, D]) for i in range(NY)]
            bc_y = nc.gpsimd.alloc_register("moe_bcy%d" % L)
            nc.gpsimd.reg_mov(bc_y, 2 * S - 1)
            tr_banks = [ps_all[0], ps_all[1]]
            P.ps_list = ps_all[2:8]
            yi = 0

            def prefetch(i):
                wi = i % NWB
                k.dma_raw("pool", lambda e: e.indirect_dma_start(
                    out=Wt[wi].ap[:], out_offset=None, in_=wt_d[:, :],
                    in_offset=bass.IndirectOffsetOnAxis(ap=widx.ap[:, i:i + 1], axis=0)),
                    reads=[widx, WTB], writes=[Wt[wi]], sem="swt%d" % wi)
                it = idt[wi]
                k.dma(it.ap[:], IDX.ap[i * 512:(i + 1) * 512, :].rearrange("(s p) c -> p s c", p=128), reads=[IDX], writes=[it],
                      sem="sidt%d" % wi, q="pool")

            def gather(i):
                it = idt[i % NWB]
                res = []
                for s4 in range(4):
                    bi = (i % 2) * 4 + s4
                    g = hg[bi]
                    k.dma_raw("pool", lambda e, g=g, s4=s4: e.indirect_dma_start(
                        out=g.ap[:], out_offset=None, in_=HT.ap[:, :],
                        in_offset=bass.IndirectOffsetOnAxis(ap=it.ap[:, s4, 0:1], axis=0)),
                        reads=[it, HT], writes=[g], sem="shg%d" % bi)
                    res.append(g)
                return res

            prefetch(0)
            nxt = gather(0)
            prefetch(1)
            for i in range(NT_TILES):
                wi = i % NWB
                gs = nxt
                if i + 2 < NT_TILES:
                    prefetch(i + 2)
                if i + 1 < NT_TILES:
                    nxt = gather(i + 1)
                W = Wt[wi]
                ht = hT[i % 2]
                for c in range(8):
                    tb = tr_banks[c % 2]
                    tbv = tb.ap[:].bitcast(BF16)
                    for s4 in range(4):
                        k.op("pe", lambda e, c=c, s4=s4, tbv=tbv: e.transpose(out=tbv[:, s4 * 128:(s4 + 1) * 128],
                                                                             in_=gs[s4].ap[:, c * 128:(c + 1) * 128],
                                                                             identity=identb.ap[:]),
                             reads=[gs[s4], identb], writes=[tb], signal=(s4 == 3))
                    if c % 2 == 0:
                        _copy(k, "act", ht.ap[:, c, :], tbv[:, 0:512], [tb], [ht])
                    else:
                        _copy(k, "dve", ht.ap[:, c, :], tbv[:, 0:512], [tb], [ht])
                hb = he[i % 2]
                for f in range(4):
                    pg = next_ps(P)
                    pu = next_ps(P)
                    for c in range(8):
                        _mm(k, pg, pg.ap[:], W.ap[:, c * 512 + f * 128:c * 512 + (f + 1) * 128], ht.ap[:, c, :], c == 0, c == 7,
                            [W, ht], c == 7)
                    for c in range(8):
                        _mm(k, pu, pu.ap[:], W.ap[:, 4096 + c * 512 + f * 128:4096 + c * 512 + (f + 1) * 128], ht.ap[:, c, :],
                            c == 0, c == 7, [W, ht], c == 7)
                    a = asb[f % 2]
                    _act(k, a.ap[:], pg.ap[:], AF.Silu, [pg], [a])
                    _tt(k, "dve", hb.ap[:, f, :], pu.ap[:], a.ap[:], ALU.mult, [pu, a], [hb])
                it = idt[wi]
                for s4 in range(4):
                    y = ysl[yi % NY]
                    yi += 1
                    for dh in range(2):
                        pd = next_ps(P)
                        for f in range(4):
                            _mm(k, pd, pd.ap[:], hb.ap[:, f, s4 * 128:(s4 + 1) * 128],
                                W.ap[:, 8192 + f * 1024 + dh * 512:8192 + f * 1024 + (dh + 1) * 512], f == 0,
                                f == 3, [W, hb], f == 3)
                        if dh == 0:
                            _copy(k, "act", y.ap[:, 0:512], pd.ap[:], [pd], [y])
                        else:
                            _copy(k, "dve", y.ap[:, 512:1024], pd.ap[:], [pd], [y])
                    k.dma_raw("pool", lambda e, y=y, s4=s4, it=it: e.indirect_dma_start(
                        out=Y2.ap[:, :], out_offset=bass.IndirectOffsetOnAxis(ap=it.ap[:, s4, 1:2], axis=0),
                        in_=y.ap[:], in_offset=None, bounds_check=bc_y, oob_is_err=False),
                        reads=[y, it], wfree=[Y2], sem="st_Y2_%d" % L, rot=(yi - 1) % NY)
            k.barrier()
        with ExitStack() as es5:
            NX = 3
            xt = [P.sb(es5, "dx%d" % i, [128, 8, 512]) for i in range(NX)]
            ND = 4
            y0 = [P.sb(es5, "dy0%d" % i, [128, D]) for i in range(ND)]
            y1 = [P.sb(es5, "dy1%d" % i, [128, D]) for i in range(ND)]
            sq = P.sb(es5, "dsq", [128, 8, 512])
            rstd = P.sb(es5, "drstd", [128, 512])
            if final:
                fg = P.sb(es5, "dfg", [128, 8])
                k.dma(fg.ap[:], fg_d[:, :], writes=[fg], sem="misc")
            P.ps_list = ps_all

            def chunksD(tt):
                xb = xt[tt % NX]
                k.dma(xb.ap[:], Xin.ap[:, tt * 512:(tt + 1) * 512].rearrange("(c p) t -> p c t", p=128), reads=[Xin],
                      writes=[xb], sem="dx%d" % (tt % NX))
                for s4 in range(4):
                    cg = tt * 4 + s4
                    a0, a1 = y0[cg % ND], y1[cg % ND]
                    k.dma(a0.ap[:], Y2.ap[cg * 128:(cg + 1) * 128, :], reads=[Y2], writes=[a0], sem="dy0%d" % (cg % ND))
                    k.dma(a1.ap[:], Y2.ap[S + cg * 128:S + (cg + 1) * 128, :], reads=[Y2], writes=[a1], sem="dy1%d" % (cg % ND))
                    _ts(k, "dve", a0.ap[:], a0.ap[:], wts.ap[:, cg, 0:1], None, ALU.mult, None, [a0, wts], [a0])
                    _stt(k, "dve", a0.ap[:], a1.ap[:], wts.ap[:, cg, 1:2], a0.ap[:], ALU.mult, ALU.add, [a1, wts, a0], [a0])
                    pa, pb = next_ps(P), next_ps(P)
                    for c in range(8):
                        pp = pa if c < 4 else pb
                        k.op("pe", lambda e, c=c, pp=pp, a0=a0: e.transpose(out=pp.ap[:, (c % 4) * 128:(c % 4 + 1) * 128],
                                                                            in_=a0.ap[:, c * 128:(c + 1) * 128],
                                                                            identity=ident.ap[:]),
                             reads=[a0, ident], writes=[pp], signal=(c % 4 == 3))
                    for c in range(8):
                        pp = pa if c < 4 else pb
                        _stt(k, "dve", xb.ap[:, c, s4 * 128:(s4 + 1) * 128], pp.ap[:, (c % 4) * 128:(c % 4 + 1) * 128],
                             mv.ap[:, L, 5, c:c + 1], xb.ap[:, c, s4 * 128:(s4 + 1) * 128], ALU.mult, ALU.add, [pp, mv, xb], [xb])

            def finishD(tt):
                xb = xt[tt % NX]
                if final:
                    rms_rstd(P, xb.ap[:], xb, 512, sq, rstd)
                    _tt(k, "dve", sq.ap[:], xb.ap[:], rstd.ap[:].unsqueeze(1).to_broadcast([128, 8, 512]), ALU.mult, [xb, rstd], [sq])
                    for c in range(8):
                        _act(k, xb.ap[:, c, :], sq.ap[:, c, :], AF.Identity, [sq, fg], [xb], scale=fg.ap[:, c:c + 1])
                k.dma(Xout.ap[:, tt * 512:(tt + 1) * 512].rearrange("(c p) t -> p c t", p=128), xb.ap[:], reads=[xb], wfree=[Xout],
                      sem="st_" + Xout.name, rot=tt % NX, q="pool")

            chunksD(0)
            for tt in range(8):
                if tt + 1 < 8:
                    chunksD(tt + 1)
                finishD(tt)
            k.barrier()
        P.ps_list = None
```
